# Optimizing a Trainium2 kernel written in Bass

```python
import jax
import jax.numpy as jnp
from jax import lax
import numpy as np


D_MODEL = 1024
BATCH = 8
SEQ = 2048
DEPTH = 2

MEM_LEN = 256
N_BRANCH = 4
BRANCH_W = 512
BLOCK_Q = 128
A_GROUPS = 4
A_CHUNK = 128
A_GW = BRANCH_W // A_GROUPS
B_HEADS = 8
B_KV = 2
B_GQ = B_HEADS // B_KV
B_HD = 64
CMP_LEN = 32
CMP_STRIDE = 16
SEL_LEN = 64
SEL_N = 8
WIN = 256
C_HEADS = 8
C_HD = 64
M_HEADS = 4
M_HD = BRANCH_W // M_HEADS
ROPE_THETA = 10000.0
EPS = 1e-6
NEG = -1e30
BIG = 1e9

IN_SIZES = (
    BRANCH_W, BRANCH_W, BRANCH_W,
    B_HEADS * B_HD, B_KV * B_HD, B_KV * B_HD, B_KV * B_HD,
    B_KV * B_HD, B_KV * B_HD, B_KV * B_HD, B_HEADS * 3, BRANCH_W,
    C_HEADS * C_HD, C_HEADS * C_HD, C_HEADS * C_HD, C_HEADS, BRANCH_W,
    M_HEADS * M_HD, BRANCH_W,
)
D_IN = int(sum(IN_SIZES))
IN_SPLITS = tuple(int(c) for c in np.cumsum(IN_SIZES)[:-1])

kernel_name = 'hybrid_gmlp_nsa_fox_mem_block'


def rms_norm(x, g):
    xf = x.astype(jnp.float32)
    y = xf * lax.rsqrt(jnp.mean(xf * xf, axis=-1, keepdims=True) + EPS)
    return (y * g.astype(jnp.float32)).astype(x.dtype)


def layer_norm(x, g, b):
    xf = x.astype(jnp.float32)
    mu = jnp.mean(xf, axis=-1, keepdims=True)
    xc = xf - mu
    y = xc * lax.rsqrt(jnp.mean(xc * xc, axis=-1, keepdims=True) + EPS)
    return (y * g.astype(jnp.float32) + b.astype(jnp.float32)).astype(x.dtype)


def rope(x, pos):
    half = x.shape[-1] // 2
    freqs = ROPE_THETA ** (-jnp.arange(half, dtype=jnp.float32) / half)
    ang = pos.astype(jnp.float32)[:, None] * freqs[None, :]
    cos, sin = jnp.cos(ang), jnp.sin(ang)
    xf = x.astype(jnp.float32)
    x1, x2 = xf[..., :half], xf[..., half:]
    return jnp.concatenate([x1 * cos - x2 * sin, x1 * sin + x2 * cos], axis=-1).astype(x.dtype)


def masked_softmax(s, mask):
    p = jax.nn.softmax(jnp.where(mask, s, NEG), axis=-1)
    return jnp.where(mask, p, 0.0)


def chunked_gmlp(u, v, ln_g, ln_b, w_s, b_s):
    Bn, S, W = u.shape
    u = jax.nn.gelu(u)
    v = layer_norm(jax.nn.gelu(v), ln_g, ln_b)
    vc = v.reshape(Bn, S // A_CHUNK, A_CHUNK, A_GROUPS, A_GW)
    tri = np.tril(np.ones((A_CHUNK, A_CHUNK), dtype=bool))
    ws = jnp.where(tri, w_s, 0.0)
    s = jnp.einsum('gts,bcsgd->bctgd', ws, vc) + b_s.T[None, None, :, :, None]
    return u * s.reshape(Bn, S, W)


def compress(k, pos_emb, w1, w2):
    S = k.shape[2]
    nc = (S - CMP_LEN) // CMP_STRIDE + 1
    idx = np.arange(nc)[:, None] * CMP_STRIDE + np.arange(CMP_LEN)[None, :]
    blocks = k[:, :, idx] + pos_emb
    flat = blocks.reshape(blocks.shape[0], blocks.shape[1], nc, CMP_LEN * B_HD)
    return jax.nn.silu(flat @ w1) @ w2


def nsa_attention(q, kc, vc, ks, vs, kw, vw, gate_logits, pos_k, w1_k, w2_k, pos_v, w1_v, w2_v):
    Bn, S, _ = q.shape
    pos = jnp.arange(S)
    scale = B_HD ** -0.5
    q = rope(q.reshape(Bn, S, B_KV, B_GQ, B_HD).transpose(0, 2, 3, 1, 4), pos)
    kv_heads = lambda t: t.reshape(Bn, S, B_KV, B_HD).transpose(0, 2, 1, 3)
    kc, ks, kw = [rope(kv_heads(t), pos) for t in (kc, ks, kw)]
    vc, vs, vw = [kv_heads(t) for t in (vc, vs, vw)]

    kcmp = compress(kc, pos_k, w1_k, w2_k)
    vcmp = compress(vc, pos_v, w1_v, w2_v)
    nc = kcmp.shape[2]
    cmp_ok = (np.arange(nc) * CMP_STRIDE + CMP_LEN - 1)[None, :] <= np.arange(S)[:, None]
    s = jnp.einsum('bgrtd,bgcd->bgrtc', q, kcmp, preferred_element_type=jnp.float32) * scale
    p_cmp = masked_softmax(s, cmp_ok)
    o_cmp = jnp.einsum('bgrtc,bgcd->bgrtd', p_cmp.astype(vcmp.dtype), vcmp)

    nsel = S // SEL_LEN
    n_sel = min(SEL_N, nsel)
    ci = np.arange(nc) * CMP_STRIDE
    sj = np.arange(nsel) * SEL_LEN
    overlap = ((ci[:, None] <= sj[None, :] + SEL_LEN - 1) & (ci[:, None] + CMP_LEN - 1 >= sj[None, :])).astype(np.float32)
    imp = jnp.einsum('bgrtc,cj->bgtj', p_cmp, overlap)
    cur = np.arange(S) // SEL_LEN
    blk = np.arange(nsel)
    forced = (blk[None, :] == 0) | (blk[None, :] == cur[:, None]) | (blk[None, :] == cur[:, None] - 1)
    future = blk[None, :] > cur[:, None]
    imp = jnp.where(forced, BIG, jnp.where(future, NEG, imp))
    _, sel_idx = lax.top_k(imp, n_sel)

    nb = S // BLOCK_Q
    q_blk = q.reshape(Bn, B_KV, B_GQ, nb, BLOCK_Q, B_HD).transpose(3, 0, 1, 2, 4, 5)
    idx_blk = sel_idx.reshape(Bn, B_KV, nb, BLOCK_Q, n_sel).transpose(2, 0, 1, 3, 4)
    ks_b = ks.reshape(Bn, B_KV, nsel, SEL_LEN, B_HD)
    vs_b = vs.reshape(Bn, B_KV, nsel, SEL_LEN, B_HD)
    kpad = jnp.pad(kw, ((0, 0), (0, 0), (WIN, 0), (0, 0)))
    vpad = jnp.pad(vw, ((0, 0), (0, 0), (WIN, 0), (0, 0)))
    bi = jnp.arange(Bn)[:, None, None, None]
    gi = jnp.arange(B_KV)[None, :, None, None]
    n_keys = n_sel * SEL_LEN

    def block_fn(args):
        qb, idxb, i = args
        qs = i * BLOCK_Q
        tq = qs + jnp.arange(BLOCK_Q)
        kg = ks_b[bi, gi, idxb].reshape(Bn, B_KV, BLOCK_Q, n_keys, B_HD)
        vg = vs_b[bi, gi, idxb].reshape(Bn, B_KV, BLOCK_Q, n_keys, B_HD)
        kpos = (idxb[..., None] * SEL_LEN + jnp.arange(SEL_LEN)).reshape(Bn, B_KV, BLOCK_Q, n_keys)
        sel_ok = (kpos <= tq[:, None])[:, :, None]
        s_sel = jnp.einsum('bgrtd,bgtkd->bgrtk', qb, kg, preferred_element_type=jnp.float32) * scale
        o_sel = jnp.einsum('bgrtk,bgtkd->bgrtd', masked_softmax(s_sel, sel_ok).astype(vg.dtype), vg)
        kwin = lax.dynamic_slice_in_dim(kpad, qs, WIN + BLOCK_Q, axis=2)
        vwin = lax.dynamic_slice_in_dim(vpad, qs, WIN + BLOCK_Q, axis=2)
        wpos = qs - WIN + jnp.arange(WIN + BLOCK_Q)
        win_ok = (wpos[None, :] <= tq[:, None]) & (wpos[None, :] > tq[:, None] - WIN) & (wpos[None, :] >= 0)
        s_win = jnp.einsum('bgrtd,bgkd->bgrtk', qb, kwin, preferred_element_type=jnp.float32) * scale
        o_win = jnp.einsum('bgrtk,bgkd->bgrtd', masked_softmax(s_win, win_ok).astype(vwin.dtype), vwin)
        return o_sel, o_win

    o_sel, o_win = lax.map(block_fn, (q_blk, idx_blk, jnp.arange(nb)))
    o_sel = o_sel.transpose(1, 2, 3, 0, 4, 5).reshape(Bn, B_KV, B_GQ, S, B_HD)
    o_win = o_win.transpose(1, 2, 3, 0, 4, 5).reshape(Bn, B_KV, B_GQ, S, B_HD)

    g = jax.nn.sigmoid(gate_logits.astype(jnp.float32)).reshape(Bn, S, B_KV, B_GQ, 3).transpose(0, 2, 3, 1, 4).astype(q.dtype)
    o = g[..., 0:1] * o_cmp + g[..., 1:2] * o_sel + g[..., 2:3] * o_win
    return o.transpose(0, 3, 1, 2, 4).reshape(Bn, S, B_HEADS * B_HD)


def forgetting_attention(q, k, v, f_logit, f_bias):
    Bn, S, _ = q.shape
    heads = lambda t: t.reshape(Bn, S, C_HEADS, C_HD).transpose(0, 2, 1, 3)
    q, k, v = heads(q), heads(k), heads(v)
    log_f = jax.nn.log_sigmoid(f_logit.astype(jnp.float32) + f_bias.astype(jnp.float32))
    c = jnp.cumsum(log_f, axis=1).transpose(0, 2, 1)
    scale = C_HD ** -0.5
    outs = []
    for i in range(S // BLOCK_Q):
        qs, qe = i * BLOCK_Q, (i + 1) * BLOCK_Q
        s = jnp.einsum('bhtd,bhsd->bhts', q[:, :, qs:qe], k[:, :, :qe], preferred_element_type=jnp.float32) * scale
        s = s + c[:, :, qs:qe, None] - c[:, :, None, :qe]
        causal = np.arange(qe)[None, :] <= np.arange(qs, qe)[:, None]
        p = masked_softmax(s, causal)
        outs.append(jnp.einsum('bhts,bhsd->bhtd', p.astype(v.dtype), v[:, :, :qe]))
    o = jnp.concatenate(outs, axis=2)
    return o.transpose(0, 2, 1, 3).reshape(Bn, S, C_HEADS * C_HD)


def memory_attention(q, mem, g_mem, w_mem_kv):
    Bn, S, _ = q.shape
    mkv = rms_norm(mem, g_mem) @ w_mem_kv
    mk, mv = jnp.split(mkv, 2, axis=-1)
    heads = lambda t: t.reshape(t.shape[0], t.shape[1], M_HEADS, M_HD).transpose(0, 2, 1, 3)
    s = jnp.einsum('bhtd,bhmd->bhtm', heads(q), heads(mk), preferred_element_type=jnp.float32) * (M_HD ** -0.5)
    p = jax.nn.softmax(s, axis=-1)
    o = jnp.einsum('bhtm,bhmd->bhtd', p.astype(mv.dtype), heads(mv))
    return o.transpose(0, 2, 1, 3).reshape(Bn, S, M_HEADS * M_HD)


def hybrid_layer(x, mem, w_in, g_pre, g_post, g_mem, w_mem_kv, a_ln_g, a_ln_b, a_ws, a_bs,
                 b_cmp_pos_k, b_cmp_w1_k, b_cmp_w2_k, b_cmp_pos_v, b_cmp_w1_v, b_cmp_w2_v,
                 c_fbias, w_br, w_gate, w_o):
    h = rms_norm(x, g_pre)
    parts = jnp.split(h @ w_in, IN_SPLITS, axis=-1)
    (a_u, a_v, a_z,
     b_q, b_kc, b_vc, b_ks, b_vs, b_kw, b_vw, b_g, b_z,
     c_q, c_k, c_v, c_f, c_z,
     m_q, m_z) = parts
    y_a = chunked_gmlp(a_u, a_v, a_ln_g, a_ln_b, a_ws, a_bs) * jax.nn.silu(a_z)
    y_b = nsa_attention(b_q, b_kc, b_vc, b_ks, b_vs, b_kw, b_vw, b_g,
                        b_cmp_pos_k, b_cmp_w1_k, b_cmp_w2_k, b_cmp_pos_v, b_cmp_w1_v, b_cmp_w2_v) * jax.nn.silu(b_z)
    y_c = forgetting_attention(c_q, c_k, c_v, c_f, c_fbias) * jax.nn.silu(c_z)
    y_m = memory_attention(m_q, mem, g_mem, w_mem_kv) * jax.nn.silu(m_z)
    ys = jnp.stack([y_a, y_b, y_c, y_m], axis=2)
    up = jnp.einsum('bsnw,nwd->bsnd', ys, w_br)
    gates = jax.nn.sigmoid(jnp.einsum('bsd,dne->bsne', h, w_gate))
    merged = jnp.einsum('bsnd,bsnd->bsd', gates, up)
    out = merged @ w_o
    return x + rms_norm(out, g_post)


def setup_inputs(seed: int = 0) -> dict:
    key = jax.random.key(seed)
    k = jax.random.split(key, 21)
    f32 = jnp.float32
    nrm = lambda kk, shape, sc: jax.random.normal(kk, shape, f32) * sc
    L, D = DEPTH, D_MODEL
    return {
        'x': nrm(k[0], (BATCH, SEQ, D), 1.0),
        'mem': nrm(k[1], (BATCH, MEM_LEN, D), 1.0),
        'w_in': nrm(k[2], (L, D, D_IN), D ** -0.5),
        'g_pre': 1.0 + nrm(k[3], (L, D), 0.02),
        'g_post': 1.0 + nrm(k[4], (L, D), 0.02),
        'g_mem': 1.0 + nrm(k[5], (L, D), 0.02),
        'w_mem_kv': nrm(k[6], (L, D, 2 * BRANCH_W), D ** -0.5),
        'a_ln_g': 1.0 + nrm(k[7], (L, BRANCH_W), 0.02),
        'a_ln_b': nrm(k[8], (L, BRANCH_W), 0.02),
        'a_ws': nrm(k[9], (L, A_GROUPS, A_CHUNK, A_CHUNK), A_CHUNK ** -0.5),
        'a_bs': 1.0 + nrm(k[10], (L, A_GROUPS, A_CHUNK), 0.1),
        'b_cmp_pos_k': nrm(k[11], (L, CMP_LEN, B_HD), 0.02),
        'b_cmp_w1_k': nrm(k[12], (L, CMP_LEN * B_HD, B_HD), (CMP_LEN * B_HD) ** -0.5),
        'b_cmp_w2_k': nrm(k[13], (L, B_HD, B_HD), B_HD ** -0.5),
        'b_cmp_pos_v': nrm(k[14], (L, CMP_LEN, B_HD), 0.02),
        'b_cmp_w1_v': nrm(k[15], (L, CMP_LEN * B_HD, B_HD), (CMP_LEN * B_HD) ** -0.5),
        'b_cmp_w2_v': nrm(k[16], (L, B_HD, B_HD), B_HD ** -0.5),
        'c_fbias': 2.0 + nrm(k[17], (L, C_HEADS), 0.5),
        'w_br': nrm(k[18], (L, N_BRANCH, BRANCH_W, D), BRANCH_W ** -0.5),
        'w_gate': nrm(k[19], (L, D, N_BRANCH, D), D ** -0.5),
        'w_o': nrm(k[20], (L, D, D), D ** -0.5),
    }


def reference(x, mem, w_in, g_pre, g_post, g_mem, w_mem_kv, a_ln_g, a_ln_b, a_ws, a_bs,
              b_cmp_pos_k, b_cmp_w1_k, b_cmp_w2_k, b_cmp_pos_v, b_cmp_w1_v, b_cmp_w2_v,
              c_fbias, w_br, w_gate, w_o):
    for l in range(DEPTH):
        x = hybrid_layer(x, mem, w_in[l], g_pre[l], g_post[l], g_mem[l], w_mem_kv[l],
                         a_ln_g[l], a_ln_b[l], a_ws[l], a_bs[l],
                         b_cmp_pos_k[l], b_cmp_w1_k[l], b_cmp_w2_k[l],
                         b_cmp_pos_v[l], b_cmp_w1_v[l], b_cmp_w2_v[l],
                         c_fbias[l], w_br[l], w_gate[l], w_o[l])
    return x
```

```python
import math
from contextlib import ExitStack
import numpy as np
import ml_dtypes
import concourse.bass as bass
import concourse.mybir as mybir
from concourse.bass_utils import run_bass_kernel_spmd

F32 = mybir.dt.float32
BF16 = mybir.dt.bfloat16
AF = mybir.ActivationFunctionType
ALU = mybir.AluOpType
AX = mybir.AxisListType

S_LEN = 2048
D = 1024
NT = 16
NCH = 4
KC = 8
MEM = 256
NEG = -1.0e30
EPS = 1e-6
GC1 = math.sqrt(2.0 / math.pi)
GC2 = GC1 * 0.044715

ENGS = ['pe', 'act', 'dve', 'pool', 'sp']
SAME_ENG_SYNC = {'pe': False, 'act': True, 'dve': True, 'pool': True, 'sp': False}
N_DMA_SEMS = 12


class Sched:
    def __init__(self):
        self.ops = []
        self.per_eng = {e: [] for e in ENGS}
        self.last_w = {}
        self.readers = {}
        self.dma_count = {e: 0 for e in ENGS}

    def op(self, eng, fn, reads=(), writes=(), dma=False):
        oid = len(self.ops)
        deps = set()
        for k in reads:
            w = self.last_w.get(k)
            if w is not None:
                deps.add(w)
            if isinstance(k, str) and k.startswith('ps'):
                for r in self.readers.get(k, ()):
                    if self.ops[r]['eng'] != eng:
                        deps.add(r)
        for k in writes:
            w = self.last_w.get(k)
            if w is not None:
                deps.add(w)
            for r in self.readers.get(k, ()):
                deps.add(r)
        for k in reads:
            self.readers.setdefault(k, []).append(oid)
        for k in writes:
            self.last_w[k] = oid
            self.readers[k] = []
        deps.discard(oid)
        o = dict(id=oid, eng=eng, fn=fn, deps=sorted(deps), dma=dma)
        if dma:
            n = self.dma_count[eng]
            self.dma_count[eng] = n + 1
            o['dsem'] = (eng, n % N_DMA_SEMS)
            o['dval'] = 16 * (n // N_DMA_SEMS + 1)
        self.ops.append(o)
        self.per_eng[eng].append(oid)
        return oid

    def barrier(self, engs=('pe', 'act', 'dve', 'pool')):
        last = {}
        for e in engs:
            if self.per_eng[e]:
                last[e] = self.per_eng[e][-1]
        for e in engs:
            deps = [v for k, v in last.items() if k != e]
            oid = len(self.ops)
            self.ops.append(dict(id=oid, eng=e, fn=None, deps=sorted(deps), dma=False))
            self.per_eng[e].append(oid)

    def finish(self, eng, dep_ops):
        oid = len(self.ops)
        self.ops.append(dict(id=oid, eng=eng, fn=None, deps=sorted(dep_ops), dma=False))
        self.per_eng[eng].append(oid)

    def emit(self, block, sems, dsems):
        ops = self.ops
        for o in ops:
            o['sig'] = False
        for o in ops:
            for d in o['deps']:
                od = ops[d]
                if od['dma']:
                    continue
                if od['fn'] is None:
                    continue
                if od['eng'] != o['eng']:
                    od['sig'] = True
        cnt = {e: 0 for e in ENGS}
        for o in ops:
            if o['sig']:
                cnt[o['eng']] += 1
                o['sidx'] = cnt[o['eng']]
        known = {e: {} for e in ENGS}
        pos = {}
        for e in ENGS:
            for i_, oid_ in enumerate(self.per_eng[e]):
                pos[oid_] = i_
        last_drain = {e: -1 for e in ENGS}
        for o in ops:
            e = o['eng']
            kn = known[e]
            wd = {}
            o['drain'] = False
            if SAME_ENG_SYNC[e] and o['fn'] is not None:
                for d in o['deps']:
                    od = ops[d]
                    if od['eng'] == e and od['fn'] is not None and not od['dma'] and pos[d] > last_drain[e]:
                        o['drain'] = True
                if o['drain']:
                    last_drain[e] = pos[o['id']] - 1
            for d in o['deps']:
                od = ops[d]
                if od['fn'] is None:
                    for k2, v2 in od['snap'].items():
                        if od['eng'] == e and kn.get(k2, 0) < v2:
                            kn[k2] = v2
                    continue
                if od['dma']:
                    key = ('d',) + od['dsem']
                    val = od['dval']
                else:
                    if od['eng'] == e:
                        continue
                    key = od['eng']
                    val = od['sidx']
                if kn.get(key, 0) >= val:
                    continue
                wd[key] = max(wd.get(key, 0), val)
                kn[key] = val
                for k2, v2 in od['snap'].items():
                    if kn.get(k2, 0) < v2:
                        kn[k2] = v2
            o['waits'] = wd
            o['snap'] = dict(kn)
        engobj = {'pe': block.tensor, 'act': block.scalar, 'dve': block.vector,
                  'pool': block.gpsimd, 'sp': block.sync}

        def make(e):
            def body(eh):
                for oid in self.per_eng[e]:
                    o = ops[oid]
                    for key, val in o['waits'].items():
                        if isinstance(key, tuple):
                            eh.wait_ge(dsems[(key[1], key[2])], val)
                        else:
                            eh.wait_ge(sems[key], val)
                    if o['fn'] is None:
                        continue
                    if o['drain']:
                        eh.drain()
                    ins = o['fn'](eh)
                    if o['dma']:
                        ins.then_inc(dsems[o['dsem']], 16)
                    elif o['sig']:
                        ins.then_inc(sems[e], 1)
            return body

        for e in ENGS:
            if self.per_eng[e]:
                engobj[e](make(e))


O_AU, O_AV, O_AZ = 0, 512, 1024
O_BQ, O_BKC, O_BVC, O_BKS, O_BVS, O_BKW, O_BVW, O_BG, O_BZ = 1536, 2048, 2176, 2304, 2432, 2560, 2688, 2816, 2840
O_CQ, O_CK, O_CV, O_CF, O_CZ = 3352, 3864, 4376, 4888, 4896
O_MQ, O_MZ = 5408, 5920
WB_COLS = 1408


def _swap_halves(w, hd=64):
    sh = w.shape
    w4 = w.reshape(sh[:-1] + (sh[-1] // hd, 2, hd // 2))
    return w4[..., ::-1, :].reshape(sh)


def _host_consts():
    c = {}
    half = 32
    freqs = 10000.0 ** (-np.arange(half, dtype=np.float32) / half)
    ang = np.arange(S_LEN, dtype=np.float32)[:, None] * freqs[None, :]
    cos, sin = np.cos(ang).astype(np.float32).T, np.sin(ang).astype(np.float32).T
    cos64 = np.concatenate([cos, cos], 0)
    sin64 = np.concatenate([-sin, sin], 0)
    c['cosF'] = np.ascontiguousarray(np.concatenate([cos64, cos64], 0))
    c['sinF'] = np.ascontiguousarray(np.concatenate([sin64, sin64], 0))
    p = np.arange(128)
    bf = ml_dtypes.bfloat16
    ident = (p[:, None] == p[None, :]).astype(np.float32)
    causalneg = np.where(p[:, None] > p[None, :], NEG, 0.0).astype(np.float32)
    anticausalneg = np.where(p[:, None] <= p[None, :], NEG, 0.0).astype(np.float32)
    cb = np.zeros((128, 128 * 3), np.float32)
    cb[:, 0:128] = ident
    cb[:, 128:256] = causalneg
    cb[:, 256:384] = anticausalneg
    t = np.arange(S_LEN)
    cmpneg = np.where((p[:, None] * 16 + 31) > t[None, :], NEG, 0.0).astype(np.float32)
    c['cb'] = cb.astype(bf)
    c['cmpneg'] = cmpneg.astype(bf)
    onehot = ((t[None, :] // 64) == np.arange(32)[:, None]).astype(np.float32)
    c['onehot'] = onehot.astype(bf)
    ci = np.arange(127) * 16
    sj = np.arange(32) * 64
    ovl = ((ci[:, None] <= sj[None, :] + 63) & (ci[:, None] + 31 >= sj[None, :])).astype(np.float32)
    ov = np.zeros((128, 33), np.float32)
    ov[:127, 0] = 1.0
    ov[:127, 1:] = ovl
    c['ovl'] = ov.astype(bf)
    cur = t // 64
    blk = np.arange(32)
    forced = (blk[None, :] == 0) | (blk[None, :] == cur[:, None]) | (blk[None, :] == cur[:, None] - 1)
    future = blk[None, :] > cur[:, None]
    keep = (~(forced | future)).astype(np.float32)
    addc = np.where(forced, 1e9, np.where(future, NEG, 0.0)).astype(np.float32)
    ka = np.zeros((128, 2, NT, 32), np.float32)
    ka[:, 0] = keep.reshape(NT, 128, 32).transpose(1, 0, 2)
    ka[:, 1] = addc.reshape(NT, 128, 32).transpose(1, 0, 2)
    c['keepadd'] = ka.reshape(128, -1)
    cf = np.zeros((128, 4 * 128), np.float32)
    cf[:, 0:128] = ident
    cf[:, 128:256] = (p[:, None] <= p[None, :]).astype(np.float32)
    cf[:, 256:384] = 1.0
    cf[64, 384:512] = 1.0
    c['cf'] = cf
    return c


def _host_layer_blobs(inp):
    out = {}
    w_in = inp['w_in']
    L = w_in.shape[0]
    f32 = np.float32
    sl = lambda o, n: w_in[:, :, o:o + n]
    out['wM'] = np.ascontiguousarray(np.concatenate([sl(O_MQ, 512), sl(O_MZ, 512)], -1))
    out['wA'] = np.ascontiguousarray(np.concatenate([sl(O_AU, 512), sl(O_AZ, 512), sl(O_AV, 512)], -1))
    out['wC'] = np.ascontiguousarray(np.concatenate([sl(O_CQ, 512), sl(O_CK, 512), sl(O_CV, 512), sl(O_CZ, 512)], -1))
    out['wsm'] = np.ascontiguousarray(np.concatenate([sl(O_CF, 8), sl(O_BG, 24)], -1))
    wB = np.zeros((L, 2, D, WB_COLS), f32)
    for g in range(2):
        parts = []
        for pr in range(2):
            a = sl(O_BQ + g * 256 + pr * 128, 128)
            parts += [a, _swap_halves(a)]
        ksw = np.concatenate([sl(O_BKS + g * 64, 64), sl(O_BKW + g * 64, 64)], -1)
        parts += [ksw, _swap_halves(ksw)]
        kcvc = np.concatenate([sl(O_BKC + g * 64, 64), sl(O_BVC + g * 64, 64)], -1)
        parts += [kcvc, _swap_halves(kcvc)]
        parts += [np.concatenate([sl(O_BVS + g * 64, 64), sl(O_BVW + g * 64, 64)], -1)]
        parts += [sl(O_BZ + g * 256, 256)]
        wB[:, g] = np.concatenate(parts, -1)
    out['wB'] = wB
    out['wmem'] = np.ascontiguousarray(inp['w_mem_kv'])
    out['wbr'] = np.ascontiguousarray(inp['w_br'])
    out['wgate'] = np.ascontiguousarray(inp['w_gate'].reshape(L, D, 4 * D))
    out['wo'] = np.ascontiguousarray(inp['w_o'])
    fm = lambda v: np.ascontiguousarray(v.reshape(L, KC, 128).transpose(2, 0, 1))
    out['gpre'] = fm(inp['g_pre']).reshape(128, -1)
    out['gmem'] = fm(inp['g_mem']).reshape(128, -1)
    rep = lambda v: np.ascontiguousarray(np.broadcast_to(v[:, None, :], (L, 128, v.shape[-1])))
    out['gpost'] = rep(inp['g_post'])
    out['lng'] = rep(inp['a_ln_g'])
    out['lnb'] = rep(inp['a_ln_b'])
    out['fbias'] = rep(inp['c_fbias'])
    out['abs'] = np.ascontiguousarray(inp['a_bs'].reshape(L, 1, 512))
    out['awsT'] = np.ascontiguousarray(inp['a_ws'].transpose(0, 3, 1, 2))
    out['posk'] = np.ascontiguousarray(inp['b_cmp_pos_k'].transpose(0, 2, 1))
    out['posv'] = np.ascontiguousarray(inp['b_cmp_pos_v'].transpose(0, 2, 1))
    out['w1k'] = np.ascontiguousarray(inp['b_cmp_w1_k'].reshape(L, 32, 64, 64).transpose(0, 2, 1, 3))
    out['w1v'] = np.ascontiguousarray(inp['b_cmp_w1_v'].reshape(L, 32, 64, 64).transpose(0, 2, 1, 3))
    out['w2k'] = np.ascontiguousarray(inp['b_cmp_w2_k'])
    out['w2v'] = np.ascontiguousarray(inp['b_cmp_w2_v'])
    return out


import os
ARENA_EL = 29184


class Builder:
    def __init__(self, blob_shapes, const_shapes, debug=None, nlayers=2, branches="MACB"):
        self.debug = debug or []
        self.nlayers = nlayers
        self.branches = branches
        nc = self.nc = bass.Bass("TRN2", target_bir_lowering=False)
        self.S = Sched()
        self.dr = {}
        self.dr['x'] = nc.dram_tensor("x", [S_LEN, D], F32, kind="ExternalInput").ap()
        self.dr['mem'] = nc.dram_tensor("mem", [MEM, D], F32, kind="ExternalInput").ap()
        for k, (shp, dt) in {**blob_shapes, **const_shapes}.items():
            self.dr[k] = nc.dram_tensor(k, list(shp), dt, kind="ExternalInput").ap()
        self.dr['out'] = nc.dram_tensor("out", [S_LEN, D], F32, kind="ExternalOutput").ap()
        self.dbg_out = {}
        self.rr = {}

    def sb(self, name, shape, dt):
        return self.es.enter_context(self.nc.sbuf_tensor("sb_" + name, shape, dt))

    def mm(self, out, lhsT, rhs, start, stop, r, w, skip=False):
        if skip:
            return self.S.op('pe', lambda e: e.matmul(out, lhsT=lhsT, rhs=rhs, start=start, stop=stop, skip_group_check=True), r, w)
        return self.S.op('pe', lambda e: e.matmul(out, lhsT=lhsT, rhs=rhs, start=start, stop=stop), r, w)

    def tr(self, out, in_, r, w):
        ident = self.identb
        return self.S.op('pe', lambda e: e.transpose(out, in_, ident), list(r) + ['const'], w)

    def act(self, out, in_, func, r, w, **kw):
        return self.S.op('act', lambda e: e.activation(out=out, in_=in_, func=func, **kw), r, w)

    def ts(self, eng, out, in0, s1, s2, op0, op1, r, w):
        def f(e):
            if s2 is None:
                return e.tensor_scalar(out=out, in0=in0, scalar1=s1, scalar2=None, op0=op0)
            return e.tensor_scalar(out=out, in0=in0, scalar1=s1, scalar2=s2, op0=op0, op1=op1)
        return self.S.op(eng, f, r, w)

    def tt(self, eng, out, in0, in1, op, r, w):
        return self.S.op(eng, lambda e: e.tensor_tensor(out=out, in0=in0, in1=in1, op=op), r, w)

    def stt(self, eng, out, in0, scalar, in1, op0, op1, r, w):
        return self.S.op(eng, lambda e: e.scalar_tensor_tensor(out=out, in0=in0, scalar=scalar, in1=in1, op0=op0, op1=op1), r, w)

    def cp(self, eng, out, in_, r, w):
        if eng == 'act':
            return self.act(out, in_, AF.Copy, r, w)
        return self.S.op(eng, lambda e: e.tensor_copy(out=out, in_=in_), r, w)

    def recip(self, out, in_, r, w):
        return self.S.op('dve', lambda e: e.reciprocal(out=out, in_=in_), r, w)

    def memset(self, eng, ap, val, w):
        return self.S.op(eng, lambda e: e.memset(ap, val), (), w)

    def DMA(self, out, in_, r=(), w=(), q='sp'):
        return self.S.op(q, lambda e: e.dma_start(out=out, in_=in_), r, w, dma=True)

    def rot(self, pool):
        lst = self.pools[pool]
        i = self.rr.get(pool, 0)
        self.rr[pool] = i + 1
        return lst[i % len(lst)]

    def dbg(self, name, ap, shape, dt, keys):
        if name not in self.debug:
            return
        t = self.nc.dram_tensor("dbg_" + name, list(shape), dt, kind="ExternalOutput").ap()
        self.dbg_out[name] = t
        self.DMA(t, ap, r=keys)

    def load_w(self, dram2d, kc, n, scale=None, dst_fn=None, dstkey=None, mulc=None, rows=128):
        assert kc * n <= 2048
        stg, skey = self.rot('stage')
        sv = stg[0:rows, 0:kc * n]
        src = dram2d.rearrange("(k p) c -> p k c", p=rows)
        self.DMA(sv.rearrange("p (k c) -> p k c", k=kc), src, w=[skey])
        if dst_fn is None:
            wb, wkey = self.rot('wb')
            dv = wb[0:rows, 0:kc * n]
            dst_fn = lambda k: dv[:, k * n:(k + 1) * n]
        else:
            dv, wkey = None, dstkey
        if scale is not None:
            sc_t, sc_off, sc_key = scale
            for k in range(kc):
                if mulc is None:
                    self.ts('pool', dst_fn(k), sv[:, k * n:(k + 1) * n], sc_t[0:rows, sc_off + k:sc_off + k + 1], None, ALU.mult, None, [skey, sc_key], [wkey])
                else:
                    self.ts('pool', dst_fn(k), sv[:, k * n:(k + 1) * n], sc_t[0:rows, sc_off + k:sc_off + k + 1], float(mulc), ALU.mult, ALU.mult, [skey, sc_key], [wkey])
        elif mulc is not None:
            for k in range(kc):
                self.ts('pool', dst_fn(k), sv[:, k * n:(k + 1) * n], float(mulc), None, ALU.mult, None, [skey], [wkey])
        else:
            for k in range(kc):
                self.cp('pool', dst_fn(k), sv[:, k * n:(k + 1) * n], [skey], [wkey])
        return dv, wkey

    def proj_fm(self, wfn, wkey, ncol, src, srckey, kc, epi, tchunks=range(NCH), srclen=S_LEN):
        for T in tchunks:
            ps, pkey = self.rot('psA')
            for k in range(kc):
                self.mm(ps[0:ncol, 0:512], wfn(k), src[:, k * srclen + T * 512:k * srclen + (T + 1) * 512], k == 0, k == kc - 1,
                        [wkey, (srckey, T)], [pkey])
            epi(ps, pkey, T)

    def proj_tm(self, wfn, wkey, ncol, src, srckey, kc, epi, tiles=range(NT), srclen=S_LEN):
        for i in tiles:
            ps, pkey = self.rot('psA')
            for k in range(kc):
                self.mm(ps[:, 0:ncol], src[:, k * srclen + i * 128:k * srclen + (i + 1) * 128], wfn(k), k == 0, k == kc - 1,
                        [wkey, (srckey, i // 4)], [pkey])
            epi(ps, pkey, i)

    def norm_transpose(self, xt, xkey, dstT, dkey, dlen, col0):
        junk, jkey = self.rot('tmpb1k')
        ss, sskey = self.rot('small')
        self.act(junk[:, 0:1024], xt, AF.Square, [xkey], [jkey, sskey], accum_out=ss[:, 0:1])
        self.act(ss[:, 1:2], ss[:, 0:1], AF.Sqrt, [sskey, 'epsb'], [sskey], scale=1.0 / D, bias=self.epsb[:, 0:1])
        self.recip(ss[:, 2:3], ss[:, 1:2], [sskey], [sskey])
        xn, xnkey = self.rot('tmpb1k')
        self.ts('dve', xn[:, 0:1024], xt, ss[:, 2:3], None, ALU.mult, None, [xkey, sskey], [xnkey])
        pt, ptkey = self.rot('psT')
        for k in range(KC):
            self.tr(pt[:, k * 128:(k + 1) * 128], xn[:, k * 128:(k + 1) * 128], [xnkey], [ptkey])
        dv = dstT.rearrange("p (k c) -> p k c", k=KC)[:, :, col0:col0 + 128]
        self.act(dv, pt[:, 0:1024].rearrange("p (k c) -> p k c", k=KC), AF.Copy, [ptkey], [dkey])

    def silu2(self, ps, pkey, dst, dkey, n):
        th, thkey = self.rot('tmpf')
        self.act(th[:, 0:n], ps[:, 0:n], AF.Tanh, [pkey], [thkey], scale=0.5)
        self.stt('dve', dst, th[:, 0:n], 1.0, ps[:, 0:n], ALU.add, ALU.mult, [thkey, pkey], [dkey])

    def gelu2(self, ps, pkey, dst, dkey, n):
        sq, sqkey = self.rot('tmpf')
        self.act(sq[:, 0:n], ps[:, 0:n], AF.Square, [pkey], [sqkey])
        self.ts('dve', sq[:, 0:n], sq[:, 0:n], GC2, GC1, ALU.mult, ALU.add, [sqkey], [sqkey])
        self.tt('dve', sq[:, 0:n], sq[:, 0:n], ps[:, 0:n], ALU.mult, [sqkey, pkey], [sqkey])
        self.act(sq[:, 0:n], sq[:, 0:n], AF.Tanh, [sqkey], [sqkey])
        self.stt('dve', dst, sq[:, 0:n], 1.0, ps[:, 0:n], ALU.add, ALU.mult, [sqkey, pkey], [dkey])

    def finish_chunk_sub(self, T, yt, ykey, ystride, wcs, ycol0, wzfn, wzkey):
        for wi, wc in enumerate(wcs):
            zg, zgkey = self.rot('tmpb')

            def epi(ps, pkey, T_, zg=zg, zgkey=zgkey):
                self.silu2(ps, pkey, zg[:, 0:512], zgkey, 512)
            self.proj_fm(lambda k, wi=wi: wzfn(k, wi), wzkey, 128, self.hT, 'hT', KC, epi, tchunks=[T])
            pt, ptkey = self.rot('psT')
            for qt in range(4):
                c0 = qt * ystride + ycol0 + wi * 128
                self.tr(pt[:, qt * 128:(qt + 1) * 128], yt[:, c0:c0 + 128], [ykey], [ptkey])
            dst = self.yT[:, wc * S_LEN + T * 512:wc * S_LEN + (T + 1) * 512]
            self.tt('dve', dst, pt[:, 0:512], zg[:, 0:512], ALU.mult, [ptkey, zgkey], [('yT', T)])

    def pv_evac(self, pv, pvkey, nq, hd, ydst, ykey, stride=None):
        stride = stride or (hd + 1)
        rz, rzkey = self.rot('small')
        pv3 = pv[:, 0:nq * stride].rearrange("p (q c) -> p q c", c=stride)
        rz3 = lambda a: rz[:, a:a + nq].rearrange("p (q c) -> p q c", c=1)
        self.ts('dve', rz3(0), pv3[:, :, hd:hd + 1], 1e-30, None, ALU.max, None, [pvkey], [rzkey])
        self.recip(rz[:, 8:8 + nq], rz[:, 0:nq], [rzkey], [rzkey])
        self.tt('dve', ydst, pv3[:, :, 0:hd], rz3(8).broadcast_to([128, nq, hd]), ALU.mult, [pvkey, rzkey], [ykey])

    def branch_end(self, l, n, first):
        dr = self.dr
        for eb in range(4):
            wbr, wbrkey = self.load_w(dr['wbr'][l, n, :, eb * 256:(eb + 1) * 256], 4, 256, mulc=0.5)
            wg, wgkey = self.load_w(dr['wgate'][l, :, n * D + eb * 256:n * D + (eb + 1) * 256], KC, 256, scale=(self.gpre, l * KC, 'gvec'))
            for ec in range(2):
                e_idx = eb * 2 + ec
                for T in range(NCH):
                    pu, pukey = self.rot('psA')
                    for k in range(4):
                        self.mm(pu[:, 0:512], wbr[:, k * 256 + ec * 128:k * 256 + (ec + 1) * 128], self.yT[:, k * S_LEN + T * 512:k * S_LEN + (T + 1) * 512],
                                k == 0, k == 3, [wbrkey, ('yT', T)], [pukey])
                    pg, pgkey = self.rot('psA')
                    for k in range(KC):
                        self.mm(pg[:, 0:512], wg[:, k * 256 + ec * 128:k * 256 + (ec + 1) * 128], self.hT[:, k * S_LEN + T * 512:k * S_LEN + (T + 1) * 512],
                                k == 0, k == KC - 1, [wgkey, ('hT', T)], [pgkey])
                    th, thkey = self.rot('tmpf')
                    self.act(th[:, 0:512], pg[:, 0:512], AF.Tanh, [pgkey], [thkey], scale=0.5)
                    mdst = self.merged[:, e_idx * S_LEN + T * 512:e_idx * S_LEN + (T + 1) * 512]
                    mkey = ('merged', e_idx, T)
                    if first:
                        self.stt('dve', mdst, th[:, 0:512], 1.0, pu[:, 0:512], ALU.add, ALU.mult, [thkey, pukey], [mkey])
                    else:
                        self.stt('dve', th[:, 0:512], th[:, 0:512], 1.0, pu[:, 0:512], ALU.add, ALU.mult, [thkey, pukey], [thkey])
                        self.tt('pool', mdst, mdst, th[:, 0:512], ALU.add, [thkey, mkey], [mkey])

    def build(self):
        nc, S, dr = self.nc, self.S, self.dr
        with ExitStack() as es:
            self.es = es
            sb = self.sb
            self.hT = sb("hT", [128, KC * S_LEN], BF16)
            self.merged = sb("merged", [128, KC * S_LEN], BF16)
            self.yT = sb("yT", [128, 4 * S_LEN], BF16)
            self.cb = sb("cb", [128, 384], BF16)
            self.identb = self.cb[:, 0:128]
            self.causalneg = self.cb[:, 128:256]
            self.anticausalneg = self.cb[:, 256:384]
            self.cf = sb("cf", [128, 512], F32)
            self.identf = self.cf[:, 0:128]
            self.triu = self.cf[:, 128:256]
            self.onesf = self.cf[:, 256:384]
            self.e64 = self.cf[:, 384:512]
            self.ovl = sb("ovl", [128, 33], BF16)
            self.gpre = sb("gpre", [128, 2 * KC], F32)
            self.gmem = sb("gmem", [128, 2 * KC], F32)
            self.fbias = sb("fbias", [128, 8], F32)
            self.epsb = sb("epsb", [128, 2], F32)
            self.smallproj = sb("smallproj", [128, NT * 32], F32)
            self.cvecs = sb("cvecs", [128, 4 * 128], F32)
            self.arena = sb("arena", [128, ARENA_EL], BF16)
            stage = [(sb(f"stage{i}", [128, 2048], F32), f"stage{i}") for i in range(2)]
            wbs = [(sb(f"wb{i}", [128, 2048], BF16), f"wb{i}") for i in range(3)]
            xts = [(sb(f"xt{i}", [128, 1024], F32), f"xt{i}") for i in range(int(os.environ.get("K_XT", "2")))]
            tmpf = [(sb(f"tmpf{i}", [128, 512], F32), f"tmpf{i}") for i in range(3)]
            tmpb = [(sb(f"tmpb{i}", [128, 512], BF16), f"tmpb{i}") for i in range(6)]
            tmpb1k = [(sb(f"tmpb1k{i}", [128, 1024], BF16), f"tmpb1k{i}") for i in range(2)]
            small = [(sb(f"small{i}", [128, 32], F32), f"small{i}") for i in range(8)]
            biasp = [(sb(f"biasp{i}", [128, 128], F32), f"biasp{i}") for i in range(2)]
            psA = [(es.enter_context(nc.psum_tensor(f"psA{i}", [128, 512], F32)), f"psA{i}") for i in range(4)]
            psB = [(es.enter_context(nc.psum_tensor(f"psB{i}", [128, 512], F32)), f"psB{i}") for i in range(2)]
            psT = [(es.enter_context(nc.psum_tensor(f"psT{i}", [128, 1024], BF16)), f"psT{i}") for i in range(2)]
            self.pools = dict(stage=stage, wb=wbs, xt=xts, tmpf=tmpf, tmpb=tmpb, tmpb1k=tmpb1k, small=small, psA=psA, psB=psB, psT=psT, biasp=biasp)
            sems = {e: es.enter_context(nc.semaphore("s_" + e)) for e in ENGS}
            dsems = {(e, i): es.enter_context(nc.semaphore(f"d_{e}_{i}")) for e in ('sp',) for i in range(N_DMA_SEMS)}
            block = es.enter_context(nc.Block())

            self.memset('dve', self.epsb[:, 0:1], EPS, ['epsb'])
            self.memset('dve', self.epsb[:, 1:2], 4.0 * EPS, ['epsb'])
            for nm, t in [('cb', self.cb), ('cf', self.cf), ('ovl', self.ovl), ('gpre', self.gpre), ('gmem', self.gmem)]:
                self.DMA(t[:], dr[nm], w=['const' if nm not in ('gpre', 'gmem') else 'gvec'])

            for i in range(NT):
                xt, xkey = self.rot('xt')
                self.DMA(xt[:], dr['x'][i * 128:(i + 1) * 128, :], w=[xkey])
                self.norm_transpose(xt[:], xkey, self.hT[:], ('hT', i // 4), S_LEN, i * 128)
            self.dbg('hT0', self.hT[:], [128, KC * S_LEN], BF16, [('hT', T) for T in range(4)])
            self.stop = int(os.environ.get("K_STOP", "99"))

            for l in range(self.nlayers if self.stop > 0 else 0):
                first = True
                for br in self.branches:
                    S.barrier()
                    getattr(self, 'branch_' + br)(l)
                    self.dbg(f'yT_{br}{l}', self.yT[:], [128, 4 * S_LEN], BF16, [('yT', T) for T in range(4)])
                    if self.stop == 1:
                        break
                    self.branch_end(l, 'ABCM'.index(br), first)
                    first = False
                    if self.stop == 2:
                        break
                if self.stop < 3:
                    break
                self.dbg(f'merged{l}', self.merged[:], [128, KC * S_LEN], BF16, [('merged', e, T) for e in range(8) for T in range(4)])
                S.barrier()
                self.final_phase(l)

            S.finish('sp', [o['id'] for o in S.ops if o['dma']])
            S.emit(block, sems, dsems)
        return nc

    def final_phase(self, l):
        dr = self.dr
        last = (l == self.nlayers - 1)
        ar = self.arena
        wo = ar[:, 0:KC * 1024]
        gpost = ar[:, KC * 1024:KC * 1024 + 2048].bitcast(F32)
        self.DMA(gpost, dr['gpost'][l], w=['gpost'])
        wo3 = wo.rearrange("p (k c) -> p k c", k=KC)
        for cbk in range(4):
            self.load_w(dr['wo'][l, :, cbk * 256:(cbk + 1) * 256], KC, 256, dst_fn=lambda k, cbk=cbk: wo[:, k * 1024 + cbk * 256:k * 1024 + (cbk + 1) * 256], dstkey='wo')
        src_x = dr['x'] if l == 0 else dr['out']
        fstop = int(os.environ.get("K_FSTOP", "99"))
        for i in range(NT if fstop > 0 else 0):
            if fstop in (1, 2, 3) and i > 0:
                break
            if i >= int(os.environ.get("K_FTILES", "99")):
                break
            T = i // 4
            xt, xkey = self.rot('xt')
            self.DMA(xt[:], src_x[i * 128:(i + 1) * 128, :], r=[('outdram', i)] if l > 0 else [], w=[xkey])
            pss = []
            for hf in range(2):
                ps, pkey = self.rot('psA')
                for k in range(KC):
                    self.mm(ps[:, 0:512], self.merged[:, k * S_LEN + i * 128:k * S_LEN + (i + 1) * 128], wo[:, k * 1024 + hf * 512:k * 1024 + (hf + 1) * 512],
                            k == 0, k == KC - 1, ['wo', ('merged', k, T)], [pkey])
                pss.append((ps, pkey))
            if fstop == 1:
                break
            ss, sskey = self.rot('small')
            junk, jkey = self.rot('tmpb')
            for hf in range(2):
                self.act(junk[:, 0:512], pss[hf][0][:, 0:512], AF.Square, [pss[hf][1]], [jkey, sskey], accum_out=ss[:, hf:hf + 1])
            self.tt('dve', ss[:, 2:3], ss[:, 0:1], ss[:, 1:2], ALU.add, [sskey], [sskey])
            self.act(ss[:, 3:4], ss[:, 2:3], AF.Sqrt, [sskey, 'epsb'], [sskey], scale=0.25 / D, bias=self.epsb[:, 0:1])
            self.recip(ss[:, 5:6], ss[:, 3:4], [sskey], [sskey])
            self.ts('dve', ss[:, 4:5], ss[:, 5:6], 0.5, None, ALU.mult, None, [sskey], [sskey])
            if fstop == 2:
                break
            for hf in range(2):
                dl, dlkey = self.rot('tmpf')
                self.stt('dve', dl[:, 0:512], pss[hf][0][:, 0:512], ss[:, 4:5], gpost[:, hf * 512:(hf + 1) * 512], ALU.mult, ALU.mult, [pss[hf][1], sskey, 'gpost'], [dlkey])
                self.tt('pool', xt[:, hf * 512:(hf + 1) * 512], xt[:, hf * 512:(hf + 1) * 512], dl[:, 0:512], ALU.add, [dlkey, xkey], [xkey])
            self.DMA(dr['out'][i * 128:(i + 1) * 128, :], xt[:], r=[xkey], w=[('outdram', i)])
            if not last:
                self.norm_transpose(xt[:], xkey, self.hT[:], ('hT', T), S_LEN, i * 128)

    def branch_M(self, l):
        dr = self.dr
        ar = self.arena
        o = 0
        qmT = ar[:, o:o + 4 * S_LEN]; o += 4 * S_LEN
        memT = ar[:, o:o + KC * MEM]; o += KC * MEM
        mkT = ar[:, o:o + 4 * MEM]; o += 4 * MEM
        MVW = 130
        mv = ar[:, o:o + 2 * 4 * MVW]; o += 2 * 4 * MVW
        ytm = [ar[:, o + i * 2048:o + (i + 1) * 2048] for i in range(2)]; o += 4096
        for mt in range(2):
            xt, xkey = self.rot('xt')
            self.DMA(xt[:], dr['mem'][mt * 128:(mt + 1) * 128, :], w=[xkey])
            self.norm_transpose(xt[:], xkey, memT, 'memT', MEM, mt * 128)
        self.memset('pool', mv.rearrange("p (a c) -> p a c", c=MVW)[:, :, 128:129], 1.0, ['mv'])
        gm = (self.gmem, l * KC, 'gvec')
        gp = (self.gpre, l * KC, 'gvec')
        for cbk in range(4):
            wv, wkey = self.load_w(dr['wmem'][l, :, cbk * 256:(cbk + 1) * 256], KC, 256, scale=gm)
            if cbk < 2:
                for hh in range(2):
                    h = cbk * 2 + hh
                    ps, pkey = self.rot('psA')
                    for k in range(KC):
                        self.mm(ps[:, 0:MEM], wv[:, k * 256 + hh * 128:k * 256 + (hh + 1) * 128], memT[:, k * MEM:(k + 1) * MEM], k == 0, k == KC - 1, [wkey, 'memT'], [pkey])
                    self.cp('act', mkT[:, h * MEM:(h + 1) * MEM], ps[:, 0:MEM], [pkey], ['mkT'])
            else:
                for mt in range(2):
                    ps, pkey = self.rot('psA')
                    for k in range(KC):
                        self.mm(ps[:, 0:256], memT[:, k * MEM + mt * 128:k * MEM + (mt + 1) * 128], wv[:, k * 256:(k + 1) * 256], k == 0, k == KC - 1, [wkey, 'memT'], [pkey])
                    h0 = (cbk - 2) * 2
                    dst = mv[:, (mt * 4 + h0) * MVW:(mt * 4 + h0 + 2) * MVW].rearrange("p (h c) -> p h c", c=MVW)[:, :, 0:128]
                    self.cp('act', dst, ps[:, 0:256].rearrange("p (h c) -> p h c", c=128), [pkey], ['mv'])
        for cbk in range(2):
            wv, wkey = self.load_w(dr['wM'][l, :, cbk * 256:(cbk + 1) * 256], KC, 256, scale=gp)
            for hh in range(2):
                h = cbk * 2 + hh

                def epi(ps, pkey, T, h=h):
                    self.cp('act', qmT[:, h * S_LEN + T * 512:h * S_LEN + (T + 1) * 512], ps[:, 0:512], [pkey], [('qmT', T)])
                self.proj_fm(lambda k, wv=wv, hh=hh: wv[:, k * 256 + hh * 128:k * 256 + (hh + 1) * 128], wkey, 128, self.hT, 'hT', KC, epi)
        sc = 128.0 ** -0.5
        for T in range(NCH):
            yt = ytm[T % 2]
            ykey = f'ytm{T % 2}'
            for h in range(4):
                pts = []
                for mt in range(2):
                    ps, pkey = self.rot('psA')
                    self.mm(ps[:, 0:512], mkT[:, h * MEM + mt * 128:h * MEM + (mt + 1) * 128], qmT[:, h * S_LEN + T * 512:h * S_LEN + (T + 1) * 512], True, True,
                            ['mkT', ('qmT', T)], [pkey])
                    pt, ptkey = self.rot('tmpb')
                    self.act(pt[:, 0:512], ps[:, 0:512], AF.Exp, [pkey], [ptkey], scale=sc)
                    pts.append((pt, ptkey))
                for q2 in range(2):
                    pv, pvkey = self.rot('psB')
                    for qq in range(2):
                        qt = q2 * 2 + qq
                        for mt in range(2):
                            self.mm(pv[:, qq * 129:(qq + 1) * 129], pts[mt][0][:, qt * 128:(qt + 1) * 128], mv[:, (mt * 4 + h) * MVW:(mt * 4 + h) * MVW + 129],
                                    mt == 0, mt == 1, [pts[mt][1], 'mv'], [pvkey])
                    ydst = yt[:, q2 * 1024:(q2 + 1) * 1024].rearrange("p (q c) -> p q c", c=512)[:, :, h * 128:(h + 1) * 128]
                    self.pv_evac(pv, pvkey, 2, 128, ydst, ykey)
            for cbk in range(2):
                wzv, wzkey = self.load_w(dr['wM'][l, :, 512 + cbk * 256:512 + (cbk + 1) * 256], KC, 256, scale=gp)
                self.finish_chunk_sub(T, yt, ykey, 512, [cbk * 2, cbk * 2 + 1], cbk * 256,
                                      lambda k, wi, wzv=wzv: wzv[:, k * 256 + wi * 128:k * 256 + (wi + 1) * 128], wzkey)

    def branch_C(self, l):
        dr = self.dr
        ar = self.arena
        o = 0
        qT = ar[:, o:o + 4 * S_LEN]; o += 4 * S_LEN
        kT = ar[:, o:o + 4 * S_LEN]; o += 4 * S_LEN
        vC = ar[:, o:o + NT * 8 * 65]; o += NT * 8 * 65
        ytm = [ar[:, o + i * 2048:o + (i + 1) * 2048] for i in range(2)]; o += 4096
        assert o <= ARENA_EL
        gp = (self.gpre, l * KC, 'gvec')
        cv = self.cvecs
        lg = cv[:, 0:128]
        pre = cv[:, 128:256]
        cpos = cv[:, 256:384]
        cmid = cv[:, 384:512]
        wv, wkey = self.load_w(dr['wsm'][l], KC, 32, scale=gp)

        def epi_s(ps, pkey, i):
            self.cp('act', self.smallproj[:, i * 32:(i + 1) * 32], ps[:, 0:32], [pkey], ['smallproj'])
        self.proj_tm(lambda k: wv[:, k * 32:(k + 1) * 32], wkey, 32, self.hT, 'hT', KC, epi_s)
        self.DMA(self.fbias[:], dr['fbias'][l], w=['fbias'])
        sp3 = self.smallproj[:].rearrange("p (i c) -> p i c", c=32)
        lg3 = lg.rearrange("p (i h) -> p i h", h=8)
        self.tt('dve', lg3, sp3[:, :, 0:8], self.fbias[:, 0:8].rearrange("p (o c) -> p o c", o=1).broadcast_to([128, NT, 8]), ALU.add, ['smallproj', 'fbias'], ['lg'])
        self.act(lg, lg, AF.Exp, ['lg'], ['lg'], scale=-1.0)
        self.act(lg, lg, AF.Ln, ['lg'], ['lg'], bias=1.0)
        self.memset('dve', pre[:, 0:8], 0.0, ['pre'])
        for i in range(1, NT):
            self.tt('dve', pre[:, i * 8:(i + 1) * 8], pre[:, (i - 1) * 8:i * 8], lg[:, (i - 1) * 8:i * 8], ALU.add, ['lg', 'pre'], ['pre'])
        ps, pkey = self.rot('psA')
        for i in range(NT):
            self.mm(ps[:, i * 8:(i + 1) * 8], self.triu, lg[:, i * 8:(i + 1) * 8], True, False, ['const', 'lg'], [pkey])
            self.mm(ps[:, i * 8:(i + 1) * 8], self.onesf, pre[:, i * 8:(i + 1) * 8], False, True, ['const', 'pre'], [pkey])
        self.cp('dve', cpos, ps[:, 0:128], [pkey], ['cpos'])
        ps2, pkey2 = self.rot('psA')
        self.mm(ps2[:, 0:128], self.e64, cpos, True, True, ['const', 'cpos'], [pkey2])
        self.cp('dve', cmid, ps2[:, 0:128], [pkey2], ['cmid'])
        for which, dstT, nm in ((0, qT, 'cqT'), (1, kT, 'ckT')):
            for cbk in range(2):
                wv, wkey = self.load_w(dr['wC'][l, :, which * 512 + cbk * 256:which * 512 + (cbk + 1) * 256], KC, 256, scale=gp)
                for pp in range(2):
                    pr = cbk * 2 + pp

                    def epi(ps, pkey, T, pr=pr, dstT=dstT, nm=nm):
                        self.cp('act' if T % 2 == 0 else 'dve', dstT[:, pr * S_LEN + T * 512:pr * S_LEN + (T + 1) * 512], ps[:, 0:512], [pkey], [(nm, T)])
                    self.proj_fm(lambda k, wv=wv, pp=pp: wv[:, k * 256 + pp * 128:k * 256 + (pp + 1) * 128], wkey, 128, self.hT, 'hT', KC, epi)
        self.memset('pool', vC.rearrange("p (a c) -> p a c", c=65)[:, :, 64:65], 1.0, ['vC'])
        for cbk in range(2):
            wv, wkey = self.load_w(dr['wC'][l, :, 1024 + cbk * 256:1024 + (cbk + 1) * 256], KC, 256, scale=gp)

            def epi_v(ps, pkey, i, cbk=cbk):
                dst = vC[:, (i * 8 + cbk * 4) * 65:(i * 8 + cbk * 4 + 4) * 65].rearrange("p (h c) -> p h c", c=65)[:, :, 0:64]
                self.cp('act' if i % 2 == 0 else 'dve', dst, ps[:, 0:256].rearrange("p (h c) -> p h c", c=64), [pkey], ['vC'])
            self.proj_tm(lambda k, wv=wv: wv[:, k * 256:(k + 1) * 256], wkey, 256, self.hT, 'hT', KC, epi_v)
        cpos_hj = cpos.rearrange("p (j h) -> p h j", h=8)
        for i in range(NT):
            T = i // 4
            qt = i % 4
            yt = ytm[T % 2]
            ykey = f'ytm{T % 2}'
            bi, bikey = self.rot('biasp')
            bi3 = bi[:, 0:128].rearrange("p (h j) -> p h j", j=16)
            self.tt('dve', bi3[:, :, 0:i + 1], cpos_hj[:, :, 0:i + 1],
                    cmid[:, i * 8:(i + 1) * 8].rearrange("p (h c) -> p h c", c=1).broadcast_to([128, 8, i + 1]), ALU.subtract, ['cpos', 'cmid'], [bikey])
            pvs = [self.rot('psB') for _ in range(2)]
            for h in range(8):
                pr, half = h // 2, h % 2
                rows = slice(half * 64, half * 64 + 64)
                pv, pvkey = pvs[h // 4]
                hh = h % 4
                for jb in range(0, i + 1, 4):
                    js = list(range(jb, min(jb + 4, i + 1)))
                    ps, pkey = self.rot('psA')
                    for jj, j in enumerate(js):
                        self.mm(ps[:, jj * 128:(jj + 1) * 128], kT[rows, pr * S_LEN + j * 128:pr * S_LEN + (j + 1) * 128],
                                qT[rows, pr * S_LEN + i * 128:pr * S_LEN + (i + 1) * 128], True, j != i, [('ckT', j // 4), ('cqT', T)], [pkey])
                        if j == i:
                            self.mm(ps[:, jj * 128:(jj + 1) * 128], self.identb, self.causalneg, False, True, ['const'], [pkey])
                    pt, ptkey = self.rot('tmpb')
                    for jj, j in enumerate(js):
                        self.act(pt[:, jj * 128:(jj + 1) * 128], ps[:, jj * 128:(jj + 1) * 128], AF.Exp, [pkey, bikey], [ptkey], scale=0.125,
                                 bias=bi[:, h * 16 + j:h * 16 + j + 1])
                    for jj, j in enumerate(js):
                        self.mm(pv[:, hh * 65:(hh + 1) * 65], pt[:, jj * 128:(jj + 1) * 128], vC[:, (j * 8 + h) * 65:(j * 8 + h + 1) * 65],
                                j == 0, j == i, [ptkey, 'vC'], [pvkey])
            for hb in range(2):
                ydst = yt[:, qt * 512 + hb * 256:qt * 512 + (hb + 1) * 256].rearrange("p (h c) -> p h c", c=64)
                self.pv_evac(pvs[hb][0], pvs[hb][1], 4, 64, ydst, ykey)
            if qt == 3:
                for cbk in range(2):
                    wzv, wzkey = self.load_w(dr['wC'][l, :, 1536 + cbk * 256:1536 + (cbk + 1) * 256], KC, 256, scale=gp)
                    self.finish_chunk_sub(T, yt, ykey, 512, [cbk * 2, cbk * 2 + 1], cbk * 256,
                                          lambda k, wi, wzv=wzv: wzv[:, k * 256 + wi * 128:k * 256 + (wi + 1) * 128], wzkey)

    def branch_A(self, l):
        dr = self.dr
        ar = self.arena
        o = 0
        guzT = ar[:, o:o + 4 * S_LEN]; o += 4 * S_LEN
        wav = ar[:, o:o + KC * 512]; o += KC * 512
        wsT = ar[:, o:o + 512]; o += 512
        lng = ar[:, o:o + 1024].bitcast(F32); o += 1024
        lnb = ar[:, o:o + 1024].bitcast(F32); o += 1024
        bsrow = ar[0:1, o:o + 1024].bitcast(F32); o += 1024
        gp = (self.gpre, l * KC, 'gvec')
        for cbk in range(2):
            wv, wkey = self.load_w(dr['wA'][l, :, cbk * 256:(cbk + 1) * 256], KC, 256, scale=gp)
            for pp in range(2):
                wc = cbk * 2 + pp

                def epi(ps, pkey, T, wc=wc):
                    self.gelu2(ps, pkey, guzT[:, wc * S_LEN + T * 512:wc * S_LEN + (T + 1) * 512], ('guz', wc, T), 512)
                self.proj_fm(lambda k, wv=wv, pp=pp: wv[:, k * 256 + pp * 128:k * 256 + (pp + 1) * 128], wkey, 128, self.hT, 'hT', KC, epi)
        for cbk in range(2):
            wv, wkey = self.load_w(dr['wA'][l, :, 512 + cbk * 256:512 + (cbk + 1) * 256], KC, 256, scale=gp)
            for pp in range(2):
                wc = cbk * 2 + pp

                def epi(ps, pkey, T, wc=wc):
                    zg, zgkey = self.rot('tmpb')
                    self.silu2(ps, pkey, zg[:, 0:512], zgkey, 512)
                    d = guzT[:, wc * S_LEN + T * 512:wc * S_LEN + (T + 1) * 512]
                    self.tt('pool', d, d, zg[:, 0:512], ALU.mult, [zgkey, ('guz', wc, T)], [('guz', wc, T)])
                self.proj_fm(lambda k, wv=wv, pp=pp: wv[:, k * 256 + pp * 128:k * 256 + (pp + 1) * 128], wkey, 128, self.hT, 'hT', KC, epi)
        for cbk in range(2):
            self.load_w(dr['wA'][l, :, 1024 + cbk * 256:1024 + (cbk + 1) * 256], KC, 256, scale=gp,
                        dst_fn=lambda k, cbk=cbk: wav[:, k * 512 + cbk * 256:k * 512 + (cbk + 1) * 256], dstkey='wav')
        stg, skey = self.rot('stage')
        self.DMA(stg[:, 0:512], dr['awsT'][l].rearrange("s g t -> s (g t)"), w=[skey])
        self.tt('pool', wsT.rearrange("p (g t) -> p g t", g=4), stg[:, 0:512].rearrange("p (g t) -> p g t", g=4),
                self.triu.rearrange("p (o t) -> p o t", o=1).broadcast_to([128, 4, 128]), ALU.mult, [skey, 'const'], ['wsT'])
        self.DMA(lng, dr['lng'][l], w=['lng'])
        self.DMA(lnb, dr['lnb'][l], w=['lnb'])
        self.DMA(bsrow, dr['abs'][l], w=['bsrow'])
        gst = ar[:, o:o + NT * 512]; o += NT * 512
        assert o <= ARENA_EL
        st = self.cvecs
        ssum, ssq, mean, m2, var, rstd = (st[:, a * 16:(a + 1) * 16] for a in range(6))
        for c in range(NT):
            T = c // 4
            ps, pkey = self.rot('psA')
            for k in range(KC):
                self.mm(ps[:, 0:512], self.hT[:, k * S_LEN + c * 128:k * S_LEN + (c + 1) * 128], wav[:, k * 512:(k + 1) * 512], k == 0, k == KC - 1, ['wav', ('hT', T)], [pkey])
            g2, g2key = self.rot('tmpf')
            self.gelu2(ps, pkey, g2[:, 0:512], g2key, 512)
            self.S.op('dve', lambda e, c=c, g2=g2: e.tensor_reduce(out=ssum[:, c:c + 1], in_=g2[:, 0:512], axis=AX.X, op=ALU.add), [g2key], ['astat'])
            junk, jkey = self.rot('tmpb')
            self.act(junk[:, 0:512], g2[:, 0:512], AF.Square, [g2key], [jkey, 'astat'], accum_out=ssq[:, c:c + 1])
            self.cp('pool', gst[:, c * 512:(c + 1) * 512], g2[:, 0:512], [g2key], [('gst', c)])
        self.ts('dve', mean, ssum, 1.0 / 512, None, ALU.mult, None, ['astat'], ['astat'])
        self.tt('dve', m2, mean, mean, ALU.mult, ['astat'], ['astat'])
        self.stt('dve', var, ssq, 1.0 / 512, m2, ALU.mult, ALU.subtract, ['astat'], ['astat'])
        self.act(var, var, AF.Sqrt, ['astat', 'epsb'], ['astat'], bias=self.epsb[:, 1:2])
        self.recip(rstd, var, ['astat'], ['astat'])
        for c in range(NT):
            T = c // 4
            g2, g2key = self.rot('tmpf')
            self.ts('dve', g2[:, 0:512], gst[:, c * 512:(c + 1) * 512], mean[:, c:c + 1], rstd[:, c:c + 1], ALU.subtract, ALU.mult, [('gst', c), 'astat'], [g2key])
            self.tt('pool', g2[:, 0:512], g2[:, 0:512], lng, ALU.mult, [g2key, 'lng'], [g2key])
            vln, vlkey = self.rot('tmpb')
            self.tt('dve', vln[:, 0:512], g2[:, 0:512], lnb, ALU.add, [g2key, 'lnb'], [vlkey])
            ps2, pkey2 = self.rot('psA')
            for g in range(4):
                self.mm(ps2[:, g * 128:(g + 1) * 128], vln[:, g * 128:(g + 1) * 128], wsT[:, g * 128:(g + 1) * 128], True, False, [vlkey, 'wsT'], [pkey2])
                self.mm(ps2[:, g * 128:(g + 1) * 128], self.onesf[0:1, 0:128], bsrow[0:1, g * 128:(g + 1) * 128], False, True, ['const', 'bsrow'], [pkey2])
            ydst = self.yT[:].rearrange("p (g t) -> p g t", g=4)[:, :, c * 128:(c + 1) * 128]
            gsrc = guzT.rearrange("p (g t) -> p g t", g=4)[:, :, c * 128:(c + 1) * 128]
            self.stt('dve', ydst, ps2[:, 0:512].rearrange("p (g t) -> p g t", g=4), 0.5, gsrc, ALU.mult, ALU.mult,
                     [pkey2] + [('guz', wc, T) for wc in range(4)], [('yT', T)])

    def branch_B(self, l):
        dr = self.dr
        ar = self.arena
        S = self.S
        gp = (self.gpre, l * KC, 'gvec')
        ALL5 = ('pe', 'act', 'dve', 'pool', 'sp')
        cosF = ar[:, 0:4096].bitcast(F32)
        sinF = ar[:, 4096:8192].bitcast(F32)
        qTa = ar[:, 8192:16384]
        ksTa = ar[:, 16384:18432]
        kwT = ar[:, 18432:20480]
        kcT = ar[:, 20480:22528]
        vcT = ar[:, 22528:24576]
        vs = ar[:, 24576:24576 + 1040]
        vw = ar[:, 25616:25616 + 1040]
        kcmpT = ar[:, 26656:26656 + 128]
        vcmpx = ar[:, 26784:26784 + 97]
        w2kb = ar[:, 26884:26884 + 64]
        w2vb = ar[:, 26948:26948 + 64]
        posb = ar[:, 27012:27012 + 64]
        w1kb = ar[:, 0:2048]
        w1vb = ar[:, 2048:4096]
        ytm = [ar[:, 0:1024], ar[:, 1024:2048]]
        ocmp = ar[:, 2048:4096].bitcast(F32)
        cmpneg = ar[:, 4096:6144]
        keepadd = ar[:, 6144:8192].bitcast(F32)
        impacc = ar[:, 20480:20736].bitcast(F32)
        imp2 = ar[:, 20736:20992].bitcast(F32)
        selneg = ar[:, 20992:21120]
        bgs = self.cvecs[:, 0:384]
        S.barrier()
        sp3 = self.smallproj[:].rearrange("p (i c) -> p i c", c=32)
        bgs3 = bgs.rearrange("p (i c) -> p i c", c=24)
        self.act(bgs3, sp3[:, :, 8:32], AF.Tanh, ['smallproj'], ['bgs'], scale=0.5)
        self.ts('dve', bgs, bgs, 0.5, 0.5, ALU.mult, ALU.add, ['bgs'], ['bgs'])
        for g in range(2):
            wB = dr['wB'][l, g]
            S.barrier(ALL5)
            self.DMA(cosF, dr['cosF'], w=['cosF'])
            self.DMA(sinF, dr['sinF'], w=['sinF'])
            self.DMA(ksTa[64:96, :], dr['onehot'], w=[('ksTa', T) for T in range(4)])
            self.memset('pool', vs.rearrange("p (a c) -> p a c", c=65)[:, :, 64:65], 1.0, ['vs'])
            self.memset('pool', vw.rearrange("p (a c) -> p a c", c=65)[:, :, 64:65], 1.0, ['vw'])
            rope_dsts = [((qTa, 0, 'qTa'), (qTa, 1, 'qTa')), ((qTa, 2, 'qTa'), (qTa, 3, 'qTa')),
                         ((ksTa, 0, 'ksTa'), (kwT, 0, 'kwT')), ((kcT, 0, 'kcT'), (vcT, 0, 'vcT'))]
            for ti, (dA, dB) in enumerate(rope_dsts):
                wv, wkey = self.load_w(wB[:, ti * 256:(ti + 1) * 256], KC, 256, scale=gp)
                for T in range(NCH):
                    pa, pakey = self.rot('psA')
                    for k in range(KC):
                        self.mm(pa[:, 0:512], wv[:, k * 256:k * 256 + 128], self.hT[:, k * S_LEN + T * 512:k * S_LEN + (T + 1) * 512], k == 0, k == KC - 1, [wkey, ('hT', T)], [pakey])
                    pb, pbkey = self.rot('psA')
                    for k in range(KC):
                        self.mm(pb[:, 0:512], wv[:, k * 256 + 128:k * 256 + 256], self.hT[:, k * S_LEN + T * 512:k * S_LEN + (T + 1) * 512], k == 0, k == KC - 1, [wkey, ('hT', T)], [pbkey])
                    t1, t1key = self.rot('tmpf')
                    t2, t2key = self.rot('tmpf')
                    cs = slice(T * 512, (T + 1) * 512)
                    novc = (ti == 3)
                    np_ = 64 if novc else 128
                    self.tt('dve', t1[0:np_, 0:512], pa[0:np_, 0:512], cosF[0:np_, cs], ALU.mult, [pakey, 'cosF'], [t1key])
                    self.tt('dve', t2[0:np_, 0:512], pb[0:np_, 0:512], sinF[0:np_, cs], ALU.mult, [pbkey, 'sinF'], [t2key])
                    (ta, ha, ka), (tb, hb, kb) = dA, dB
                    self.tt('pool', ta[0:64, ha * S_LEN + T * 512:ha * S_LEN + (T + 1) * 512], t1[0:64, 0:512], t2[0:64, 0:512], ALU.add, [t1key, t2key], [(ka, T)])
                    if novc:
                        self.cp('act', tb[0:64, hb * S_LEN + T * 512:hb * S_LEN + (T + 1) * 512], pa[64:128, 0:512], [pakey], [(kb, T)])
                    else:
                        self.tt('dve', tb[0:64, hb * S_LEN + T * 512:hb * S_LEN + (T + 1) * 512], t1[64:128, 0:512], t2[64:128, 0:512], ALU.add, [t1key, t2key], [(kb, T)])
            wv, wkey = self.load_w(wB[:, 1024:1152], KC, 128, scale=gp)

            def epi_v(ps, pkey, i):
                self.cp('act', vs[:, i * 65:i * 65 + 64], ps[:, 0:64], [pkey], ['vs'])
                self.cp('dve', vw[:, i * 65:i * 65 + 64], ps[:, 64:128], [pkey], ['vw'])
            self.proj_tm(lambda k, wv=wv: wv[:, k * 128:(k + 1) * 128], wkey, 128, self.hT, 'hT', KC, epi_v)
            if int(os.environ.get("K_BSTOP", "99")) < 1:
                continue
            S.barrier(ALL5)
            self.load_w(dr['w1k'][l].rearrange("d l m -> d (l m)"), 1, 2048, dst_fn=lambda k: w1kb[0:64, :], dstkey='w1kb', rows=64)
            self.load_w(dr['w1v'][l].rearrange("d l m -> d (l m)"), 1, 2048, dst_fn=lambda k: w1vb[0:64, :], dstkey='w1vb', rows=64)
            self.load_w(dr['w2k'][l], 1, 64, dst_fn=lambda k: w2kb[0:64, :], dstkey='w2kb', rows=64, mulc=0.5)
            self.load_w(dr['w2v'][l], 1, 64, dst_fn=lambda k: w2vb[0:64, :], dstkey='w2vb', rows=64, mulc=0.5)
            self.load_w(dr['posk'][l], 1, 32, dst_fn=lambda k: posb[0:64, 0:32], dstkey='posb', rows=64)
            self.load_w(dr['posv'][l], 1, 32, dst_fn=lambda k: posb[0:64, 32:64], dstkey='posb', rows=64)
            cstop = int(os.environ.get("K_CSTOP", "99"))
            if cstop >= 1:
                self.cp('pool', vcmpx[:, 64:97], self.ovl[:, 0:33], ['const'], ['vcmpx'])
            for kv, (srcT, skeyn, w1b, w1key, w2b, w2key, po) in enumerate(((kcT, 'kcT', w1kb, 'w1kb', w2kb, 'w2kb', 0), (vcT, 'vcT', w1vb, 'w1vb', w2vb, 'w2vb', 32))):
                if cstop < 2:
                    break
                ph, phkey = self.rot('psA')
                for ll in range(32):
                    self.mm(ph[0:64, 0:127], w1b[0:64, ll * 64:(ll + 1) * 64], srcT[0:64, ll:ll + 2017:16], ll == 0, ll == 31, [w1key] + [(skeyn, T) for T in range(4)], [phkey])
                if cstop < 3:
                    break
                pb2, pb2key = self.rot('psA')
                for ll in range(32):
                    self.mm(pb2[0:64, 0:1], w1b[0:64, ll * 64:(ll + 1) * 64], posb[0:64, po + ll:po + ll + 1], ll == 0, ll == 31, [w1key, 'posb'], [pb2key])
                if cstop < 4:
                    break
                hb_, hbkey = self.rot('small')
                self.cp('dve', hb_[0:64, 0:1], pb2[0:64, 0:1], [pb2key], [hbkey])
                self.ts('dve', hb_[0:64, 1:2], hb_[0:64, 0:1], 0.5, None, ALU.mult, None, [hbkey], [hbkey])
                th, thkey = self.rot('tmpf')
                self.act(th[0:64, 0:127], ph[0:64, 0:127], AF.Tanh, [phkey, hbkey], [thkey], scale=0.5, bias=hb_[0:64, 1:2])
                if cstop < 5:
                    break
                xb, xbkey = self.rot('tmpf')
                var = os.environ.get("K_VAR", "")
                if 'a' not in var:
                    self.ts('dve', xb[0:64, 0:127], ph[0:64, 0:127], hb_[0:64, 0:1], None, ALU.add, None, [phkey, hbkey], [xbkey])
                a1, a1key = self.rot('tmpb')
                if 'b' not in var:
                    self.stt('dve', a1[0:64, 0:127], th[0:64, 0:127], 1.0, xb[0:64, 0:127], ALU.add, ALU.mult, [thkey, xbkey], [a1key])
                if cstop < 6:
                    break
                po2, po2key = self.rot('psA')
                if kv == 0:
                    self.mm(po2[0:64, 0:127], w2b[0:64, 0:64], a1[0:64, 0:127], True, True, [w2key, a1key], [po2key])
                    self.cp('dve', kcmpT[0:64, 0:127], po2[0:64, 0:127], [po2key], ['kcmpT'])
                else:
                    self.mm(po2[0:127, 0:64], a1[0:64, 0:127], w2b[0:64, 0:64], True, True, [w2key, a1key], [po2key])
                    self.cp('dve', vcmpx[0:127, 0:64], po2[0:127, 0:64], [po2key], ['vcmpx'])
            if int(os.environ.get("K_BSTOP", "99")) < 2:
                continue
            S.barrier(ALL5)
            self.DMA(cmpneg, dr['cmpneg'], w=['cmpneg'])
            self.DMA(keepadd, dr['keepadd'], w=['keepadd'])
            ka4 = keepadd.rearrange("p (a i c) -> p a i c", a=2, c=32)
            oc4 = ocmp.rearrange("p (r q c) -> p r q c", r=4, c=64)
            for T in range(NCH):
                yt = ytm[T % 2]
                ykey = f'ytmB{T % 2}'
                bgT = bgs3[:, 4 * T:4 * T + 4, :]
                for r in range(4):
                    h = 4 * g + r
                    ps, pkey = self.rot('psA')
                    self.mm(ps[0:127, 0:512], kcmpT[0:64, 0:127], qTa[0:64, r * S_LEN + T * 512:r * S_LEN + (T + 1) * 512], True, False, ['kcmpT', ('qTa', T)], [pkey])
                    self.mm(ps[0:127, 0:512], self.identb[0:127, 0:127], cmpneg[0:127, T * 512:(T + 1) * 512], False, True, ['const', 'cmpneg'], [pkey])
                    et, etkey = self.rot('tmpb')
                    self.act(et[0:127, 0:512], ps[0:127, 0:512], AF.Exp, [pkey], [etkey], scale=0.125)
                    R, Rkey = self.rot('psB')
                    for qt in range(4):
                        self.mm(R[:, qt * 97:(qt + 1) * 97], et[0:127, qt * 128:(qt + 1) * 128], vcmpx[0:127, 0:97], True, True, [etkey, 'vcmpx'], [Rkey])
                    R3 = R[:, 0:388].rearrange("p (q c) -> p q c", c=97)
                    rz, rzkey = self.rot('small')
                    rz3 = lambda a: rz[:, a:a + 4].rearrange("p (q c) -> p q c", c=1)
                    self.ts('dve', rz3(0), R3[:, :, 64:65], 1e-30, None, ALU.max, None, [Rkey], [rzkey])
                    self.recip(rz[:, 4:8], rz[:, 0:4], [rzkey], [rzkey])
                    if r == 0:
                        self.tt('dve', impacc.rearrange("p (q c) -> p q c", c=32), R3[:, :, 65:97], rz3(4).broadcast_to([128, 4, 32]), ALU.mult, [Rkey, rzkey], ['impacc'])
                    else:
                        self.tt('dve', imp2.rearrange("p (q c) -> p q c", c=32), R3[:, :, 65:97], rz3(4).broadcast_to([128, 4, 32]), ALU.mult, [Rkey, rzkey], ['imp2'])
                        self.tt('pool', impacc, impacc, imp2, ALU.add, ['imp2', 'impacc'], ['impacc'])
                    self.tt('dve', rz3(8), rz3(4), bgT[:, :, h * 3:h * 3 + 1], ALU.mult, [rzkey, 'bgs'], [rzkey])
                    self.tt('dve', oc4[:, r], R3[:, :, 0:64], rz3(8).broadcast_to([128, 4, 64]), ALU.mult, [Rkey, rzkey], [('ocmp', r)])
                if int(os.environ.get("K_BSTOP", "99")) < 3:
                    continue
                i3 = imp2.rearrange("p (q c) -> p q c", c=32)
                self.tt('dve', i3, impacc.rearrange("p (q c) -> p q c", c=32), ka4[:, 0, 4 * T:4 * T + 4, :], ALU.mult, ['impacc', 'keepadd', 'imp2'], ['imp2'])
                self.tt('dve', i3, i3, ka4[:, 1, 4 * T:4 * T + 4, :], ALU.add, ['imp2', 'keepadd'], ['imp2'])
                m8, m8key = self.rot('small')
                for qt in range(4):
                    self.S.op('dve', lambda e, qt=qt, m8=m8: e.max(out=m8[:, qt * 8:(qt + 1) * 8], in_=imp2[:, qt * 32:(qt + 1) * 32]), ['imp2'], [m8key])
                    self.ts('dve', selneg[:, qt * 32:(qt + 1) * 32], imp2[:, qt * 32:(qt + 1) * 32], m8[:, qt * 8 + 7:qt * 8 + 8], NEG, ALU.is_lt, ALU.mult, ['imp2', m8key], ['selneg'])
                pt, ptkey = self.rot('psT')
                for qt in range(4):
                    self.tr(pt[0:32, qt * 128:(qt + 1) * 128], selneg[:, qt * 32:(qt + 1) * 32], ['selneg'], [ptkey])
                for r in range(4):
                    self.cp('act' if r % 2 == 0 else 'dve', qTa[64:96, r * S_LEN + T * 512:r * S_LEN + (T + 1) * 512], pt[0:32, 0:512], [ptkey], [('qTa', T)])
                if int(os.environ.get("K_BSTOP", "99")) < 4:
                    continue
                for r in range(4):
                    h = 4 * g + r
                    acs, acskey = self.rot('psB')
                    first_s = True
                    for j in range(0, 4 * T + 4):
                        lo = max(128 * j, 512 * T)
                        w = 512 * (T + 1) - lo
                        diag = j >= 4 * T
                        ps, pkey = self.rot('psA')
                        self.mm(ps[:, 0:w], ksTa[0:96, j * 128:(j + 1) * 128], qTa[0:96, r * S_LEN + lo:r * S_LEN + lo + w], True, not diag, [('ksTa', j // 4), ('qTa', T)], [pkey])
                        if diag:
                            self.mm(ps[:, 0:128], self.identb, self.causalneg, False, True, ['const'], [pkey])
                        pT, pTkey = self.rot('tmpb')
                        self.act(pT[:, 0:w], ps[:, 0:w], AF.Exp, [pkey], [pTkey], scale=0.125)
                        for qt in range(4):
                            i = 4 * T + qt
                            if i < j:
                                continue
                            off = i * 128 - lo
                            self.mm(acs[:, qt * 65:(qt + 1) * 65], pT[:, off:off + 128], vs[:, j * 65:(j + 1) * 65], first_s, j == i, [pTkey, 'vs'], [acskey], skip=True)
                            first_s = False
                    acw, acwkey = self.rot('psB')
                    first_w = True
                    for j in range(max(0, 4 * T - 2), 4 * T + 4):
                        i_lo = max(j, 4 * T)
                        i_hi = min(j + 2, 4 * T + 3)
                        lo = i_lo * 128
                        w = (i_hi + 1) * 128 - lo
                        masks = []
                        if i_lo == j:
                            masks.append((0, self.causalneg))
                        if i_hi == j + 2:
                            masks.append(((j + 2) * 128 - lo, self.anticausalneg))
                        ps, pkey = self.rot('psA')
                        self.mm(ps[:, 0:w], kwT[0:64, j * 128:(j + 1) * 128], qTa[0:64, r * S_LEN + lo:r * S_LEN + lo + w], True, len(masks) == 0, [('kwT', j // 4), ('qTa', T)], [pkey])
                        for mi, (mo, mk) in enumerate(masks):
                            self.mm(ps[:, mo:mo + 128], self.identb, mk, False, mi == len(masks) - 1, ['const'], [pkey])
                        pT, pTkey = self.rot('tmpb')
                        self.act(pT[:, 0:w], ps[:, 0:w], AF.Exp, [pkey], [pTkey], scale=0.125)
                        for i in range(i_lo, i_hi + 1):
                            qt = i - 4 * T
                            off = i * 128 - lo
                            self.mm(acw[:, qt * 65:(qt + 1) * 65], pT[:, off:off + 128], vw[:, j * 65:(j + 1) * 65], first_w, j == i, [pTkey, 'vw'], [acwkey], skip=True)
                            first_w = False
                    s3 = acs[:, 0:260].rearrange("p (q c) -> p q c", c=65)
                    w3 = acw[:, 0:260].rearrange("p (q c) -> p q c", c=65)
                    rz, rzkey = self.rot('small')
                    rz3 = lambda a: rz[:, a:a + 4].rearrange("p (q c) -> p q c", c=1)
                    self.ts('dve', rz3(0), s3[:, :, 64:65], 1e-30, None, ALU.max, None, [acskey], [rzkey])
                    self.ts('dve', rz3(4), w3[:, :, 64:65], 1e-30, None, ALU.max, None, [acwkey], [rzkey])
                    self.recip(rz[:, 8:16], rz[:, 0:8], [rzkey], [rzkey])
                    self.tt('dve', rz3(16), rz3(8), bgT[:, :, h * 3 + 1:h * 3 + 2], ALU.mult, [rzkey, 'bgs'], [rzkey])
                    self.tt('dve', rz3(20), rz3(12), bgT[:, :, h * 3 + 2:h * 3 + 3], ALU.mult, [rzkey, 'bgs'], [rzkey])
                    ta, takey = self.rot('tmpf')
                    tb, tbkey = self.rot('tmpf')
                    ta3 = ta[:, 0:256].rearrange("p (q c) -> p q c", c=64)
                    tb3 = tb[:, 0:256].rearrange("p (q c) -> p q c", c=64)
                    self.tt('dve', ta3, s3[:, :, 0:64], rz3(16).broadcast_to([128, 4, 64]), ALU.mult, [acskey, rzkey], [takey])
                    self.tt('pool', ta3, ta3, oc4[:, r], ALU.add, [takey, ('ocmp', r)], [takey])
                    self.tt('dve', tb3, w3[:, :, 0:64], rz3(20).broadcast_to([128, 4, 64]), ALU.mult, [acwkey, rzkey], [tbkey])
                    ydst = yt.rearrange("p (q c) -> p q c", c=256)[:, :, r * 64:(r + 1) * 64]
                    self.tt('dve', ydst, ta3, tb3, ALU.add, [takey, tbkey], [ykey])
                wzv, wzkey = self.load_w(wB[:, 1152:1408], KC, 256, scale=gp)
                self.finish_chunk_sub(T, yt, ykey, 256, [2 * g, 2 * g + 1], 0,
                                      lambda k, wi, wzv=wzv: wzv[:, k * 256 + wi * 128:k * 256 + (wi + 1) * 128], wzkey)
        S.barrier(ALL5)


_CACHE = {}


def _prep(inputs):
    blobs = _host_layer_blobs(inputs)
    consts = _host_consts()
    return blobs, consts


def kernel(**inputs):
    inputs = {k: np.asarray(v) for k, v in inputs.items()}
    blobs, consts = _prep(inputs)
    dtm = lambda a: BF16 if a.dtype == ml_dtypes.bfloat16 else F32
    blob_shapes = {k: (v.shape, dtm(v)) for k, v in blobs.items()}
    const_shapes = {k: (v.shape, dtm(v)) for k, v in consts.items()}
    b = Builder(blob_shapes, const_shapes)
    nc = b.build()
    in_maps = []
    for c in range(8):
        m = {'x': np.ascontiguousarray(inputs['x'][c]), 'mem': np.ascontiguousarray(inputs['mem'][c])}
        m.update(blobs)
        m.update(consts)
        in_maps.append(m)
    res = run_bass_kernel_spmd(nc, in_maps, core_ids=list(range(8)))
    return np.stack([r['out'] for r in res.results], 0).astype(np.float32)
```

```python
import math
from contextlib import ExitStack
import numpy as np
import ml_dtypes
import concourse.bass as bass
import concourse.mybir as mybir
from concourse.bass_utils import run_bass_kernel_spmd

F32 = mybir.dt.float32
BF16 = mybir.dt.bfloat16
AF = mybir.ActivationFunctionType
ALU = mybir.AluOpType
AX = mybir.AxisListType

S_LEN = 2048
D = 1024
NT = 16
NCH = 4
KC = 8
MEM = 256
NEG = -1.0e30
EPS = 1e-6
GC1 = math.sqrt(2.0 / math.pi)
GC2 = GC1 * 0.044715

ENGS = ['pe', 'act', 'dve', 'pool', 'sp']
SAME_ENG_SYNC = {'pe': False, 'act': True, 'dve': True, 'pool': True, 'sp': False}
N_DMA_SEMS = 12


class Sched:
    def __init__(self):
        self.ops = []
        self.per_eng = {e: [] for e in ENGS}
        self.last_w = {}
        self.readers = {}
        self.dma_count = {e: 0 for e in ENGS}

    def op(self, eng, fn, reads=(), writes=(), dma=False):
        oid = len(self.ops)
        deps = set()
        for k in reads:
            w = self.last_w.get(k)
            if w is not None:
                deps.add(w)
            if isinstance(k, str) and k.startswith('ps'):
                for r in self.readers.get(k, ()):
                    if self.ops[r]['eng'] != eng:
                        deps.add(r)
        for k in writes:
            w = self.last_w.get(k)
            if w is not None:
                deps.add(w)
            for r in self.readers.get(k, ()):
                deps.add(r)
        for k in reads:
            self.readers.setdefault(k, []).append(oid)
        for k in writes:
            self.last_w[k] = oid
            self.readers[k] = []
        deps.discard(oid)
        o = dict(id=oid, eng=eng, fn=fn, deps=sorted(deps), dma=dma)
        if dma:
            n = self.dma_count[eng]
            self.dma_count[eng] = n + 1
            o['dsem'] = (eng, n % N_DMA_SEMS)
            o['dval'] = 16 * (n // N_DMA_SEMS + 1)
        self.ops.append(o)
        self.per_eng[eng].append(oid)
        return oid

    def barrier(self, engs=('pe', 'act', 'dve', 'pool')):
        last = {}
        for e in engs:
            if self.per_eng[e]:
                last[e] = self.per_eng[e][-1]
        for e in engs:
            deps = [v for k, v in last.items() if k != e]
            oid = len(self.ops)
            self.ops.append(dict(id=oid, eng=e, fn=None, deps=sorted(deps), dma=False))
            self.per_eng[e].append(oid)

    def finish(self, eng, dep_ops):
        oid = len(self.ops)
        self.ops.append(dict(id=oid, eng=eng, fn=None, deps=sorted(dep_ops), dma=False))
        self.per_eng[eng].append(oid)

    def emit(self, block, sems, dsems):
        ops = self.ops
        for o in ops:
            o['sig'] = False
        for o in ops:
            for d in o['deps']:
                od = ops[d]
                if od['dma']:
                    continue
                if od['fn'] is None:
                    continue
                if od['eng'] != o['eng']:
                    od['sig'] = True
        cnt = {e: 0 for e in ENGS}
        for o in ops:
            if o['sig']:
                cnt[o['eng']] += 1
                o['sidx'] = cnt[o['eng']]
        known = {e: {} for e in ENGS}
        pos = {}
        for e in ENGS:
            for i_, oid_ in enumerate(self.per_eng[e]):
                pos[oid_] = i_
        last_drain = {e: -1 for e in ENGS}
        for o in ops:
            e = o['eng']
            kn = known[e]
            wd = {}
            o['drain'] = False
            if SAME_ENG_SYNC[e] and o['fn'] is not None:
                for d in o['deps']:
                    od = ops[d]
                    if od['eng'] == e and od['fn'] is not None and not od['dma'] and pos[d] > last_drain[e]:
                        o['drain'] = True
                if o['drain']:
                    last_drain[e] = pos[o['id']] - 1
            for d in o['deps']:
                od = ops[d]
                if od['fn'] is None:
                    for k2, v2 in od['snap'].items():
                        if od['eng'] == e and kn.get(k2, 0) < v2:
                            kn[k2] = v2
                    continue
                if od['dma']:
                    key = ('d',) + od['dsem']
                    val = od['dval']
                else:
                    if od['eng'] == e:
                        continue
                    key = od['eng']
                    val = od['sidx']
                if kn.get(key, 0) >= val:
                    continue
                wd[key] = max(wd.get(key, 0), val)
                kn[key] = val
                for k2, v2 in od['snap'].items():
                    if kn.get(k2, 0) < v2:
                        kn[k2] = v2
            o['waits'] = wd
            o['snap'] = dict(kn)
        engobj = {'pe': block.tensor, 'act': block.scalar, 'dve': block.vector,
                  'pool': block.gpsimd, 'sp': block.sync}

        def make(e):
            def body(eh):
                for oid in self.per_eng[e]:
                    o = ops[oid]
                    for key, val in o['waits'].items():
                        if isinstance(key, tuple):
                            eh.wait_ge(dsems[(key[1], key[2])], val)
                        else:
                            eh.wait_ge(sems[key], val)
                    if o['fn'] is None:
                        continue
                    if o['drain']:
                        eh.drain()
                    ins = o['fn'](eh)
                    if o['dma']:
                        ins.then_inc(dsems[o['dsem']], 16)
                    elif o['sig']:
                        ins.then_inc(sems[e], 1)
            return body

        for e in ENGS:
            if self.per_eng[e]:
                engobj[e](make(e))


O_AU, O_AV, O_AZ = 0, 512, 1024
O_BQ, O_BKC, O_BVC, O_BKS, O_BVS, O_BKW, O_BVW, O_BG, O_BZ = 1536, 2048, 2176, 2304, 2432, 2560, 2688, 2816, 2840
O_CQ, O_CK, O_CV, O_CF, O_CZ = 3352, 3864, 4376, 4888, 4896
O_MQ, O_MZ = 5408, 5920
WB_COLS = 1408


def _swap_halves(w, hd=64):
    sh = w.shape
    w4 = w.reshape(sh[:-1] + (sh[-1] // hd, 2, hd // 2))
    return w4[..., ::-1, :].reshape(sh)


def _host_consts():
    c = {}
    half = 32
    freqs = 10000.0 ** (-np.arange(half, dtype=np.float32) / half)
    ang = np.arange(S_LEN, dtype=np.float32)[:, None] * freqs[None, :]
    cos, sin = np.cos(ang).astype(np.float32).T, np.sin(ang).astype(np.float32).T
    cos64 = np.concatenate([cos, cos], 0)
    sin64 = np.concatenate([-sin, sin], 0)
    c['cosF'] = np.ascontiguousarray(np.concatenate([cos64, cos64], 0))
    c['sinF'] = np.ascontiguousarray(np.concatenate([sin64, sin64], 0))
    p = np.arange(128)
    bf = ml_dtypes.bfloat16
    ident = (p[:, None] == p[None, :]).astype(np.float32)
    causalneg = np.where(p[:, None] > p[None, :], NEG, 0.0).astype(np.float32)
    anticausalneg = np.where(p[:, None] <= p[None, :], NEG, 0.0).astype(np.float32)
    cb = np.zeros((128, 128 * 3), np.float32)
    cb[:, 0:128] = ident
    cb[:, 128:256] = causalneg
    cb[:, 256:384] = anticausalneg
    t = np.arange(S_LEN)
    cmpneg = np.where((p[:, None] * 16 + 31) > t[None, :], NEG, 0.0).astype(np.float32)
    c['cb'] = cb.astype(bf)
    c['cmpneg'] = cmpneg.astype(bf)
    onehot = ((t[None, :] // 64) == np.arange(32)[:, None]).astype(np.float32)
    c['onehot'] = onehot.astype(bf)
    ci = np.arange(127) * 16
    sj = np.arange(32) * 64
    ovl = ((ci[:, None] <= sj[None, :] + 63) & (ci[:, None] + 31 >= sj[None, :])).astype(np.float32)
    ov = np.zeros((128, 33), np.float32)
    ov[:127, 0] = 1.0
    ov[:127, 1:] = ovl
    c['ovl'] = ov.astype(bf)
    cur = t // 64
    blk = np.arange(32)
    forced = (blk[None, :] == 0) | (blk[None, :] == cur[:, None]) | (blk[None, :] == cur[:, None] - 1)
    future = blk[None, :] > cur[:, None]
    keep = (~(forced | future)).astype(np.float32)
    addc = np.where(forced, 1e9, np.where(future, NEG, 0.0)).astype(np.float32)
    ka = np.zeros((128, 2, NT, 32), np.float32)
    ka[:, 0] = keep.reshape(NT, 128, 32).transpose(1, 0, 2)
    ka[:, 1] = addc.reshape(NT, 128, 32).transpose(1, 0, 2)
    c['keepadd'] = ka.reshape(128, -1)
    cf = np.zeros((128, 4 * 128), np.float32)
    cf[:, 0:128] = ident
    cf[:, 128:256] = (p[:, None] <= p[None, :]).astype(np.float32)
    cf[:, 256:384] = 1.0
    cf[64, 384:512] = 1.0
    c['cf'] = cf
    return c


def _host_layer_blobs(inp):
    out = {}
    w_in = inp['w_in']
    L = w_in.shape[0]
    f32 = np.float32
    sl = lambda o, n: w_in[:, :, o:o + n]
    out['wM'] = np.ascontiguousarray(np.concatenate([sl(O_MQ, 512), sl(O_MZ, 512)], -1))
    out['wA'] = np.ascontiguousarray(np.concatenate([sl(O_AU, 512), sl(O_AZ, 512), sl(O_AV, 512)], -1))
    out['wC'] = np.ascontiguousarray(np.concatenate([sl(O_CQ, 512), sl(O_CK, 512), sl(O_CV, 512), sl(O_CZ, 512)], -1))
    out['wsm'] = np.ascontiguousarray(np.concatenate([sl(O_CF, 8), sl(O_BG, 24)], -1))
    wB = np.zeros((L, 2, D, WB_COLS), f32)
    for g in range(2):
        parts = []
        for pr in range(2):
            a = sl(O_BQ + g * 256 + pr * 128, 128)
            parts += [a, _swap_halves(a)]
        ksw = np.concatenate([sl(O_BKS + g * 64, 64), sl(O_BKW + g * 64, 64)], -1)
        parts += [ksw, _swap_halves(ksw)]
        kcvc = np.concatenate([sl(O_BKC + g * 64, 64), sl(O_BVC + g * 64, 64)], -1)
        parts += [kcvc, _swap_halves(kcvc)]
        parts += [np.concatenate([sl(O_BVS + g * 64, 64), sl(O_BVW + g * 64, 64)], -1)]
        parts += [sl(O_BZ + g * 256, 256)]
        wB[:, g] = np.concatenate(parts, -1)
    out['wB'] = wB
    out['wmem'] = np.ascontiguousarray(inp['w_mem_kv'])
    out['wbr'] = np.ascontiguousarray(inp['w_br'])
    out['wgate'] = np.ascontiguousarray(inp['w_gate'].reshape(L, D, 4 * D))
    out['wo'] = np.ascontiguousarray(inp['w_o'])
    fm = lambda v: np.ascontiguousarray(v.reshape(L, KC, 128).transpose(2, 0, 1))
    out['gpre'] = fm(inp['g_pre']).reshape(128, -1)
    out['gmem'] = fm(inp['g_mem']).reshape(128, -1)
    rep = lambda v: np.ascontiguousarray(np.broadcast_to(v[:, None, :], (L, 128, v.shape[-1])))
    out['gpost'] = rep(inp['g_post'])
    out['lng'] = rep(inp['a_ln_g'])
    out['lnb'] = rep(inp['a_ln_b'])
    out['fbias'] = rep(inp['c_fbias'])
    out['abs'] = np.ascontiguousarray(inp['a_bs'].reshape(L, 1, 512))
    out['awsT'] = np.ascontiguousarray(inp['a_ws'].transpose(0, 3, 1, 2))
    out['posk'] = np.ascontiguousarray(inp['b_cmp_pos_k'].transpose(0, 2, 1))
    out['posv'] = np.ascontiguousarray(inp['b_cmp_pos_v'].transpose(0, 2, 1))
    out['w1k'] = np.ascontiguousarray(inp['b_cmp_w1_k'].reshape(L, 32, 64, 64).transpose(0, 2, 1, 3))
    out['w1v'] = np.ascontiguousarray(inp['b_cmp_w1_v'].reshape(L, 32, 64, 64).transpose(0, 2, 1, 3))
    out['w2k'] = np.ascontiguousarray(inp['b_cmp_w2_k'])
    out['w2v'] = np.ascontiguousarray(inp['b_cmp_w2_v'])
    return out


import os
ARENA_EL = 29184


class Builder:
    def __init__(self, blob_shapes, const_shapes, debug=None, nlayers=2, branches="MACB"):
        self.debug = debug or []
        self.nlayers = nlayers
        self.branches = branches
        nc = self.nc = bass.Bass("TRN2", target_bir_lowering=False)
        self.S = Sched()
        self.dr = {}
        self.dr['x'] = nc.dram_tensor("x", [S_LEN, D], F32, kind="ExternalInput").ap()
        self.dr['mem'] = nc.dram_tensor("mem", [MEM, D], F32, kind="ExternalInput").ap()
        for k, (shp, dt) in {**blob_shapes, **const_shapes}.items():
            self.dr[k] = nc.dram_tensor(k, list(shp), dt, kind="ExternalInput").ap()
        self.dr['out'] = nc.dram_tensor("out", [S_LEN, D], F32, kind="ExternalOutput").ap()
        self.dbg_out = {}
        self.rr = {}

    def sb(self, name, shape, dt):
        return self.es.enter_context(self.nc.sbuf_tensor("sb_" + name, shape, dt))

    def mm(self, out, lhsT, rhs, start, stop, r, w, skip=False):
        if skip:
            return self.S.op('pe', lambda e: e.matmul(out, lhsT=lhsT, rhs=rhs, start=start, stop=stop, skip_group_check=True), r, w)
        return self.S.op('pe', lambda e: e.matmul(out, lhsT=lhsT, rhs=rhs, start=start, stop=stop), r, w)

    def tr(self, out, in_, r, w):
        ident = self.identb
        return self.S.op('pe', lambda e: e.transpose(out, in_, ident), list(r) + ['const'], w)

    def act(self, out, in_, func, r, w, **kw):
        return self.S.op('act', lambda e: e.activation(out=out, in_=in_, func=func, **kw), r, w)

    def ts(self, eng, out, in0, s1, s2, op0, op1, r, w):
        def f(e):
            if s2 is None:
                return e.tensor_scalar(out=out, in0=in0, scalar1=s1, scalar2=None, op0=op0)
            return e.tensor_scalar(out=out, in0=in0, scalar1=s1, scalar2=s2, op0=op0, op1=op1)
        return self.S.op(eng, f, r, w)

    def tt(self, eng, out, in0, in1, op, r, w):
        return self.S.op(eng, lambda e: e.tensor_tensor(out=out, in0=in0, in1=in1, op=op), r, w)

    def stt(self, eng, out, in0, scalar, in1, op0, op1, r, w):
        return self.S.op(eng, lambda e: e.scalar_tensor_tensor(out=out, in0=in0, scalar=scalar, in1=in1, op0=op0, op1=op1), r, w)

    def cp(self, eng, out, in_, r, w):
        if eng == 'act':
            return self.act(out, in_, AF.Copy, r, w)
        return self.S.op(eng, lambda e: e.tensor_copy(out=out, in_=in_), r, w)

    def recip(self, out, in_, r, w):
        return self.S.op('dve', lambda e: e.reciprocal(out=out, in_=in_), r, w)

    def memset(self, eng, ap, val, w):
        return self.S.op(eng, lambda e: e.memset(ap, val), (), w)

    def DMA(self, out, in_, r=(), w=(), q='sp'):
        return self.S.op(q, lambda e: e.dma_start(out=out, in_=in_), r, w, dma=True)

    def rot(self, pool):
        lst = self.pools[pool]
        i = self.rr.get(pool, 0)
        self.rr[pool] = i + 1
        return lst[i % len(lst)]

    def dbg(self, name, ap, shape, dt, keys):
        if name not in self.debug:
            return
        t = self.nc.dram_tensor("dbg_" + name, list(shape), dt, kind="ExternalOutput").ap()
        self.dbg_out[name] = t
        self.DMA(t, ap, r=keys)

    def load_w(self, dram2d, kc, n, scale=None, dst_fn=None, dstkey=None, mulc=None, rows=128):
        assert kc * n <= 2048
        stg, skey = self.rot('stage')
        sv = stg[0:rows, 0:kc * n]
        src = dram2d.rearrange("(k p) c -> p k c", p=rows)
        self.DMA(sv.rearrange("p (k c) -> p k c", k=kc), src, w=[skey])
        if dst_fn is None:
            wb, wkey = self.rot('wb')
            dv = wb[0:rows, 0:kc * n]
            dst_fn = lambda k: dv[:, k * n:(k + 1) * n]
        else:
            dv, wkey = None, dstkey
        whole = dv is not None
        if whole:
            sv3 = sv.rearrange("p (k c) -> p k c", k=kc)
            dv3 = dv.rearrange("p (k c) -> p k c", k=kc)
        if scale is not None:
            sc_t, sc_off, sc_key = scale
            if whole:
                scb = sc_t[0:rows, sc_off:sc_off + kc].rearrange("p (k c) -> p k c", c=1).broadcast_to([rows, kc, n])
                if mulc is None:
                    self.tt('dve', dv3, sv3, scb, ALU.mult, [skey, sc_key], [wkey])
                else:
                    self.stt('dve', dv3, sv3, float(mulc), scb, ALU.mult, ALU.mult, [skey, sc_key], [wkey])
            else:
                for k in range(kc):
                    if mulc is None:
                        self.ts('dve', dst_fn(k), sv[:, k * n:(k + 1) * n], sc_t[0:rows, sc_off + k:sc_off + k + 1], None, ALU.mult, None, [skey, sc_key], [wkey])
                    else:
                        self.ts('dve', dst_fn(k), sv[:, k * n:(k + 1) * n], sc_t[0:rows, sc_off + k:sc_off + k + 1], float(mulc), ALU.mult, ALU.mult, [skey, sc_key], [wkey])
        elif mulc is not None:
            if whole:
                self.act(dv, sv, AF.Copy, [skey], [wkey], scale=float(mulc))
            else:
                for k in range(kc):
                    self.act(dst_fn(k), sv[:, k * n:(k + 1) * n], AF.Copy, [skey], [wkey], scale=float(mulc))
        else:
            if whole:
                self.cp('act', dv, sv, [skey], [wkey])
            else:
                for k in range(kc):
                    self.cp('act', dst_fn(k), sv[:, k * n:(k + 1) * n], [skey], [wkey])
        return dv, wkey

    def proj_fm(self, wfn, wkey, ncol, src, srckey, kc, epi, tchunks=range(NCH), srclen=S_LEN):
        for T in tchunks:
            ps, pkey = self.rot('psA')
            for k in range(kc):
                self.mm(ps[0:ncol, 0:512], wfn(k), src[:, k * srclen + T * 512:k * srclen + (T + 1) * 512], k == 0, k == kc - 1,
                        [wkey, (srckey, T)], [pkey])
            epi(ps, pkey, T)

    def proj_tm(self, wfn, wkey, ncol, src, srckey, kc, epi, tiles=range(NT), srclen=S_LEN):
        for i in tiles:
            ps, pkey = self.rot('psA')
            for k in range(kc):
                self.mm(ps[:, 0:ncol], src[:, k * srclen + i * 128:k * srclen + (i + 1) * 128], wfn(k), k == 0, k == kc - 1,
                        [wkey, (srckey, i // 4)], [pkey])
            epi(ps, pkey, i)

    def norm_transpose(self, xt, xkey, dstT, dkey, dlen, col0):
        junk, jkey = self.rot('tmpb1k')
        ss, sskey = self.rot('small')
        self.act(junk[:, 0:1024], xt, AF.Square, [xkey], [jkey, sskey], accum_out=ss[:, 0:1])
        self.act(ss[:, 1:2], ss[:, 0:1], AF.Sqrt, [sskey, 'epsb'], [sskey], scale=1.0 / D, bias=self.epsb[:, 0:1])
        self.recip(ss[:, 2:3], ss[:, 1:2], [sskey], [sskey])
        xn, xnkey = self.rot('tmpb1k')
        self.ts('dve', xn[:, 0:1024], xt, ss[:, 2:3], None, ALU.mult, None, [xkey, sskey], [xnkey])
        pt, ptkey = self.rot('psT')
        for k in range(KC):
            self.tr(pt[:, k * 128:(k + 1) * 128], xn[:, k * 128:(k + 1) * 128], [xnkey], [ptkey])
        dv = dstT.rearrange("p (k c) -> p k c", k=KC)[:, :, col0:col0 + 128]
        self.act(dv, pt[:, 0:1024].rearrange("p (k c) -> p k c", k=KC), AF.Copy, [ptkey], [dkey])

    def silu2(self, ps, pkey, dst, dkey, n):
        th, thkey = self.rot('tmpf')
        self.act(th[:, 0:n], ps[:, 0:n], AF.Tanh, [pkey], [thkey], scale=0.5)
        self.stt('dve', dst, th[:, 0:n], 1.0, ps[:, 0:n], ALU.add, ALU.mult, [thkey, pkey], [dkey])

    def gelu2(self, ps, pkey, dst, dkey, n):
        sq, sqkey = self.rot('tmpf')
        self.act(sq[:, 0:n], ps[:, 0:n], AF.Square, [pkey], [sqkey])
        self.ts('dve', sq[:, 0:n], sq[:, 0:n], GC2, GC1, ALU.mult, ALU.add, [sqkey], [sqkey])
        self.tt('dve', sq[:, 0:n], sq[:, 0:n], ps[:, 0:n], ALU.mult, [sqkey, pkey], [sqkey])
        self.act(sq[:, 0:n], sq[:, 0:n], AF.Tanh, [sqkey], [sqkey])
        self.stt('dve', dst, sq[:, 0:n], 1.0, ps[:, 0:n], ALU.add, ALU.mult, [sqkey, pkey], [dkey])

    def finish_chunk_sub(self, T, yt, ykey, ystride, wcs, ycol0, wzfn, wzkey):
        for wi, wc in enumerate(wcs):
            zg, zgkey = self.rot('tmpb')

            def epi(ps, pkey, T_, zg=zg, zgkey=zgkey):
                self.silu2(ps, pkey, zg[:, 0:512], zgkey, 512)
            self.proj_fm(lambda k, wi=wi: wzfn(k, wi), wzkey, 128, self.hT, 'hT', KC, epi, tchunks=[T])
            pt, ptkey = self.rot('psT')
            for qt in range(4):
                c0 = qt * ystride + ycol0 + wi * 128
                self.tr(pt[:, qt * 128:(qt + 1) * 128], yt[:, c0:c0 + 128], [ykey], [ptkey])
            dst = self.yT[:, wc * S_LEN + T * 512:wc * S_LEN + (T + 1) * 512]
            self.tt('dve', dst, pt[:, 0:512], zg[:, 0:512], ALU.mult, [ptkey, zgkey], [('yT', T)])

    def pv_evac(self, pv, pvkey, nq, hd, ydst, ykey, stride=None):
        stride = stride or (hd + 1)
        rz, rzkey = self.rot('small')
        pv3 = pv[:, 0:nq * stride].rearrange("p (q c) -> p q c", c=stride)
        rz3 = lambda a: rz[:, a:a + nq].rearrange("p (q c) -> p q c", c=1)
        self.ts('dve', rz3(0), pv3[:, :, hd:hd + 1], 1e-30, None, ALU.max, None, [pvkey], [rzkey])
        self.recip(rz[:, 8:8 + nq], rz[:, 0:nq], [rzkey], [rzkey])
        self.tt('dve', ydst, pv3[:, :, 0:hd], rz3(8).broadcast_to([128, nq, hd]), ALU.mult, [pvkey, rzkey], [ykey])

    def branch_end(self, l, n, first):
        dr = self.dr
        for eb in range(4):
            wbr, wbrkey = self.load_w(dr['wbr'][l, n, :, eb * 256:(eb + 1) * 256], 4, 256, mulc=0.5)
            wg, wgkey = self.load_w(dr['wgate'][l, :, n * D + eb * 256:n * D + (eb + 1) * 256], KC, 256, scale=(self.gpre, l * KC, 'gvec'))
            for ec in range(2):
                e_idx = eb * 2 + ec
                for T in range(NCH):
                    pu, pukey = self.rot('psA')
                    for k in range(4):
                        self.mm(pu[:, 0:512], wbr[:, k * 256 + ec * 128:k * 256 + (ec + 1) * 128], self.yT[:, k * S_LEN + T * 512:k * S_LEN + (T + 1) * 512],
                                k == 0, k == 3, [wbrkey, ('yT', T)], [pukey])
                    pg, pgkey = self.rot('psA')
                    for k in range(KC):
                        self.mm(pg[:, 0:512], wg[:, k * 256 + ec * 128:k * 256 + (ec + 1) * 128], self.hT[:, k * S_LEN + T * 512:k * S_LEN + (T + 1) * 512],
                                k == 0, k == KC - 1, [wgkey, ('hT', T)], [pgkey])
                    th, thkey = self.rot('tmpf')
                    self.act(th[:, 0:512], pg[:, 0:512], AF.Tanh, [pgkey], [thkey], scale=0.5)
                    mdst = self.merged[:, e_idx * S_LEN + T * 512:e_idx * S_LEN + (T + 1) * 512]
                    mkey = ('merged', e_idx, T)
                    if first:
                        self.stt('dve', mdst, th[:, 0:512], 1.0, pu[:, 0:512], ALU.add, ALU.mult, [thkey, pukey], [mkey])
                    else:
                        self.stt('dve', th[:, 0:512], th[:, 0:512], 1.0, pu[:, 0:512], ALU.add, ALU.mult, [thkey, pukey], [thkey])
                        self.tt('pool', mdst, mdst, th[:, 0:512], ALU.add, [thkey, mkey], [mkey])

    def build(self):
        nc, S, dr = self.nc, self.S, self.dr
        with ExitStack() as es:
            self.es = es
            sb = self.sb
            self.hT = sb("hT", [128, KC * S_LEN], BF16)
            self.merged = sb("merged", [128, KC * S_LEN], BF16)
            self.yT = sb("yT", [128, 4 * S_LEN], BF16)
            self.cb = sb("cb", [128, 384], BF16)
            self.identb = self.cb[:, 0:128]
            self.causalneg = self.cb[:, 128:256]
            self.anticausalneg = self.cb[:, 256:384]
            self.cf = sb("cf", [128, 512], F32)
            self.identf = self.cf[:, 0:128]
            self.triu = self.cf[:, 128:256]
            self.onesf = self.cf[:, 256:384]
            self.e64 = self.cf[:, 384:512]
            self.ovl = sb("ovl", [128, 33], BF16)
            self.gpre = sb("gpre", [128, 2 * KC], F32)
            self.gmem = sb("gmem", [128, 2 * KC], F32)
            self.fbias = sb("fbias", [128, 8], F32)
            self.epsb = sb("epsb", [128, 2], F32)
            self.smallproj = sb("smallproj", [128, NT * 32], F32)
            self.cvecs = sb("cvecs", [128, 4 * 128], F32)
            self.arena = sb("arena", [128, ARENA_EL], BF16)
            stage = [(sb(f"stage{i}", [128, 2048], F32), f"stage{i}") for i in range(2)]
            wbs = [(sb(f"wb{i}", [128, 2048], BF16), f"wb{i}") for i in range(3)]
            xts = [(sb(f"xt{i}", [128, 1024], F32), f"xt{i}") for i in range(int(os.environ.get("K_XT", "2")))]
            tmpf = [(sb(f"tmpf{i}", [128, 512], F32), f"tmpf{i}") for i in range(3)]
            tmpb = [(sb(f"tmpb{i}", [128, 512], BF16), f"tmpb{i}") for i in range(6)]
            tmpb1k = [(sb(f"tmpb1k{i}", [128, 1024], BF16), f"tmpb1k{i}") for i in range(2)]
            small = [(sb(f"small{i}", [128, 32], F32), f"small{i}") for i in range(8)]
            biasp = [(sb(f"biasp{i}", [128, 128], F32), f"biasp{i}") for i in range(2)]
            psA = [(es.enter_context(nc.psum_tensor(f"psA{i}", [128, 512], F32)), f"psA{i}") for i in range(4)]
            psB = [(es.enter_context(nc.psum_tensor(f"psB{i}", [128, 512], F32)), f"psB{i}") for i in range(2)]
            psT = [(es.enter_context(nc.psum_tensor(f"psT{i}", [128, 1024], BF16)), f"psT{i}") for i in range(2)]
            self.pools = dict(stage=stage, wb=wbs, xt=xts, tmpf=tmpf, tmpb=tmpb, tmpb1k=tmpb1k, small=small, psA=psA, psB=psB, psT=psT, biasp=biasp)
            sems = {e: es.enter_context(nc.semaphore("s_" + e)) for e in ENGS}
            dsems = {(e, i): es.enter_context(nc.semaphore(f"d_{e}_{i}")) for e in ('sp',) for i in range(N_DMA_SEMS)}
            block = es.enter_context(nc.Block())

            self.memset('dve', self.epsb[:, 0:1], EPS, ['epsb'])
            self.memset('dve', self.epsb[:, 1:2], 4.0 * EPS, ['epsb'])
            for nm, t in [('cb', self.cb), ('cf', self.cf), ('ovl', self.ovl), ('gpre', self.gpre), ('gmem', self.gmem)]:
                self.DMA(t[:], dr[nm], w=['const' if nm not in ('gpre', 'gmem') else 'gvec'])

            for i in range(NT):
                xt, xkey = self.rot('xt')
                self.DMA(xt[:], dr['x'][i * 128:(i + 1) * 128, :], w=[xkey])
                self.norm_transpose(xt[:], xkey, self.hT[:], ('hT', i // 4), S_LEN, i * 128)
            self.dbg('hT0', self.hT[:], [128, KC * S_LEN], BF16, [('hT', T) for T in range(4)])
            self.stop = int(os.environ.get("K_STOP", "99"))

            for l in range(self.nlayers if self.stop > 0 else 0):
                first = True
                for br in self.branches:
                    S.barrier()
                    getattr(self, 'branch_' + br)(l)
                    self.dbg(f'yT_{br}{l}', self.yT[:], [128, 4 * S_LEN], BF16, [('yT', T) for T in range(4)])
                    if self.stop == 1:
                        break
                    self.branch_end(l, 'ABCM'.index(br), first)
                    first = False
                    if self.stop == 2:
                        break
                if self.stop < 3:
                    break
                self.dbg(f'merged{l}', self.merged[:], [128, KC * S_LEN], BF16, [('merged', e, T) for e in range(8) for T in range(4)])
                S.barrier()
                self.final_phase(l)

            S.finish('sp', [o['id'] for o in S.ops if o['dma']])
            S.emit(block, sems, dsems)
        return nc

    def final_phase(self, l):
        dr = self.dr
        last = (l == self.nlayers - 1)
        ar = self.arena
        wo = ar[:, 0:KC * 1024]
        gpost = ar[:, KC * 1024:KC * 1024 + 2048].bitcast(F32)
        self.DMA(gpost, dr['gpost'][l], w=['gpost'])
        wo3 = wo.rearrange("p (k c) -> p k c", k=KC)
        for cbk in range(4):
            self.load_w(dr['wo'][l, :, cbk * 256:(cbk + 1) * 256], KC, 256, dst_fn=lambda k, cbk=cbk: wo[:, k * 1024 + cbk * 256:k * 1024 + (cbk + 1) * 256], dstkey='wo')
        src_x = dr['x'] if l == 0 else dr['out']
        fstop = int(os.environ.get("K_FSTOP", "99"))
        for i in range(NT if fstop > 0 else 0):
            if fstop in (1, 2, 3) and i > 0:
                break
            if i >= int(os.environ.get("K_FTILES", "99")):
                break
            T = i // 4
            xt, xkey = self.rot('xt')
            self.DMA(xt[:], src_x[i * 128:(i + 1) * 128, :], r=[('outdram', i)] if l > 0 else [], w=[xkey])
            pss = []
            for hf in range(2):
                ps, pkey = self.rot('psA')
                for k in range(KC):
                    self.mm(ps[:, 0:512], self.merged[:, k * S_LEN + i * 128:k * S_LEN + (i + 1) * 128], wo[:, k * 1024 + hf * 512:k * 1024 + (hf + 1) * 512],
                            k == 0, k == KC - 1, ['wo', ('merged', k, T)], [pkey])
                pss.append((ps, pkey))
            if fstop == 1:
                break
            ss, sskey = self.rot('small')
            junk, jkey = self.rot('tmpb')
            for hf in range(2):
                self.act(junk[:, 0:512], pss[hf][0][:, 0:512], AF.Square, [pss[hf][1]], [jkey, sskey], accum_out=ss[:, hf:hf + 1])
            self.tt('dve', ss[:, 2:3], ss[:, 0:1], ss[:, 1:2], ALU.add, [sskey], [sskey])
            self.act(ss[:, 3:4], ss[:, 2:3], AF.Sqrt, [sskey, 'epsb'], [sskey], scale=0.25 / D, bias=self.epsb[:, 0:1])
            self.recip(ss[:, 5:6], ss[:, 3:4], [sskey], [sskey])
            self.ts('dve', ss[:, 4:5], ss[:, 5:6], 0.5, None, ALU.mult, None, [sskey], [sskey])
            if fstop == 2:
                break
            for hf in range(2):
                dl, dlkey = self.rot('tmpf')
                self.stt('dve', dl[:, 0:512], pss[hf][0][:, 0:512], ss[:, 4:5], gpost[:, hf * 512:(hf + 1) * 512], ALU.mult, ALU.mult, [pss[hf][1], sskey, 'gpost'], [dlkey])
                self.tt('pool', xt[:, hf * 512:(hf + 1) * 512], xt[:, hf * 512:(hf + 1) * 512], dl[:, 0:512], ALU.add, [dlkey, xkey], [xkey])
            self.DMA(dr['out'][i * 128:(i + 1) * 128, :], xt[:], r=[xkey], w=[('outdram', i)])
            if not last:
                self.norm_transpose(xt[:], xkey, self.hT[:], ('hT', T), S_LEN, i * 128)

    def branch_M(self, l):
        dr = self.dr
        ar = self.arena
        o = 0
        qmT = ar[:, o:o + 4 * S_LEN]; o += 4 * S_LEN
        memT = ar[:, o:o + KC * MEM]; o += KC * MEM
        mkT = ar[:, o:o + 4 * MEM]; o += 4 * MEM
        MVW = 130
        mv = ar[:, o:o + 2 * 4 * MVW]; o += 2 * 4 * MVW
        ytm = [ar[:, o + i * 2048:o + (i + 1) * 2048] for i in range(2)]; o += 4096
        for mt in range(2):
            xt, xkey = self.rot('xt')
            self.DMA(xt[:], dr['mem'][mt * 128:(mt + 1) * 128, :], w=[xkey])
            self.norm_transpose(xt[:], xkey, memT, 'memT', MEM, mt * 128)
        self.memset('pool', mv.rearrange("p (a c) -> p a c", c=MVW)[:, :, 128:129], 1.0, ['mv'])
        gm = (self.gmem, l * KC, 'gvec')
        gp = (self.gpre, l * KC, 'gvec')
        for cbk in range(4):
            wv, wkey = self.load_w(dr['wmem'][l, :, cbk * 256:(cbk + 1) * 256], KC, 256, scale=gm)
            if cbk < 2:
                for hh in range(2):
                    h = cbk * 2 + hh
                    ps, pkey = self.rot('psA')
                    for k in range(KC):
                        self.mm(ps[:, 0:MEM], wv[:, k * 256 + hh * 128:k * 256 + (hh + 1) * 128], memT[:, k * MEM:(k + 1) * MEM], k == 0, k == KC - 1, [wkey, 'memT'], [pkey])
                    self.cp('act', mkT[:, h * MEM:(h + 1) * MEM], ps[:, 0:MEM], [pkey], ['mkT'])
            else:
                for mt in range(2):
                    ps, pkey = self.rot('psA')
                    for k in range(KC):
                        self.mm(ps[:, 0:256], memT[:, k * MEM + mt * 128:k * MEM + (mt + 1) * 128], wv[:, k * 256:(k + 1) * 256], k == 0, k == KC - 1, [wkey, 'memT'], [pkey])
                    h0 = (cbk - 2) * 2
                    dst = mv[:, (mt * 4 + h0) * MVW:(mt * 4 + h0 + 2) * MVW].rearrange("p (h c) -> p h c", c=MVW)[:, :, 0:128]
                    self.cp('act', dst, ps[:, 0:256].rearrange("p (h c) -> p h c", c=128), [pkey], ['mv'])
        for cbk in range(2):
            wv, wkey = self.load_w(dr['wM'][l, :, cbk * 256:(cbk + 1) * 256], KC, 256, scale=gp)
            for hh in range(2):
                h = cbk * 2 + hh

                def epi(ps, pkey, T, h=h):
                    self.cp('act', qmT[:, h * S_LEN + T * 512:h * S_LEN + (T + 1) * 512], ps[:, 0:512], [pkey], [('qmT', T)])
                self.proj_fm(lambda k, wv=wv, hh=hh: wv[:, k * 256 + hh * 128:k * 256 + (hh + 1) * 128], wkey, 128, self.hT, 'hT', KC, epi)
        sc = 128.0 ** -0.5
        for T in range(NCH):
            yt = ytm[T % 2]
            ykey = f'ytm{T % 2}'
            for h in range(4):
                pts = []
                for mt in range(2):
                    ps, pkey = self.rot('psA')
                    self.mm(ps[:, 0:512], mkT[:, h * MEM + mt * 128:h * MEM + (mt + 1) * 128], qmT[:, h * S_LEN + T * 512:h * S_LEN + (T + 1) * 512], True, True,
                            ['mkT', ('qmT', T)], [pkey])
                    pt, ptkey = self.rot('tmpb')
                    self.act(pt[:, 0:512], ps[:, 0:512], AF.Exp, [pkey], [ptkey], scale=sc)
                    pts.append((pt, ptkey))
                for q2 in range(2):
                    pv, pvkey = self.rot('psB')
                    for qq in range(2):
                        qt = q2 * 2 + qq
                        for mt in range(2):
                            self.mm(pv[:, qq * 129:(qq + 1) * 129], pts[mt][0][:, qt * 128:(qt + 1) * 128], mv[:, (mt * 4 + h) * MVW:(mt * 4 + h) * MVW + 129],
                                    mt == 0, mt == 1, [pts[mt][1], 'mv'], [pvkey])
                    ydst = yt[:, q2 * 1024:(q2 + 1) * 1024].rearrange("p (q c) -> p q c", c=512)[:, :, h * 128:(h + 1) * 128]
                    self.pv_evac(pv, pvkey, 2, 128, ydst, ykey)
            for cbk in range(2):
                wzv, wzkey = self.load_w(dr['wM'][l, :, 512 + cbk * 256:512 + (cbk + 1) * 256], KC, 256, scale=gp)
                self.finish_chunk_sub(T, yt, ykey, 512, [cbk * 2, cbk * 2 + 1], cbk * 256,
                                      lambda k, wi, wzv=wzv: wzv[:, k * 256 + wi * 128:k * 256 + (wi + 1) * 128], wzkey)

    def branch_C(self, l):
        dr = self.dr
        ar = self.arena
        o = 0
        qT = ar[:, o:o + 4 * S_LEN]; o += 4 * S_LEN
        kT = ar[:, o:o + 4 * S_LEN]; o += 4 * S_LEN
        vC = ar[:, o:o + NT * 8 * 65]; o += NT * 8 * 65
        ytm = [ar[:, o + i * 2048:o + (i + 1) * 2048] for i in range(2)]; o += 4096
        assert o <= ARENA_EL
        gp = (self.gpre, l * KC, 'gvec')
        cv = self.cvecs
        lg = cv[:, 0:128]
        pre = cv[:, 128:256]
        cpos = cv[:, 256:384]
        cmid = cv[:, 384:512]
        wv, wkey = self.load_w(dr['wsm'][l], KC, 32, scale=gp)

        def epi_s(ps, pkey, i):
            self.cp('act', self.smallproj[:, i * 32:(i + 1) * 32], ps[:, 0:32], [pkey], ['smallproj'])
        self.proj_tm(lambda k: wv[:, k * 32:(k + 1) * 32], wkey, 32, self.hT, 'hT', KC, epi_s)
        self.DMA(self.fbias[:], dr['fbias'][l], w=['fbias'])
        sp3 = self.smallproj[:].rearrange("p (i c) -> p i c", c=32)
        lg3 = lg.rearrange("p (i h) -> p i h", h=8)
        self.tt('dve', lg3, sp3[:, :, 0:8], self.fbias[:, 0:8].rearrange("p (o c) -> p o c", o=1).broadcast_to([128, NT, 8]), ALU.add, ['smallproj', 'fbias'], ['lg'])
        self.act(lg, lg, AF.Exp, ['lg'], ['lg'], scale=-1.0)
        self.act(lg, lg, AF.Ln, ['lg'], ['lg'], bias=1.0)
        self.memset('dve', pre[:, 0:8], 0.0, ['pre'])
        for i in range(1, NT):
            self.tt('dve', pre[:, i * 8:(i + 1) * 8], pre[:, (i - 1) * 8:i * 8], lg[:, (i - 1) * 8:i * 8], ALU.add, ['lg', 'pre'], ['pre'])
        ps, pkey = self.rot('psA')
        for i in range(NT):
            self.mm(ps[:, i * 8:(i + 1) * 8], self.triu, lg[:, i * 8:(i + 1) * 8], True, False, ['const', 'lg'], [pkey])
            self.mm(ps[:, i * 8:(i + 1) * 8], self.onesf, pre[:, i * 8:(i + 1) * 8], False, True, ['const', 'pre'], [pkey])
        self.cp('dve', cpos, ps[:, 0:128], [pkey], ['cpos'])
        ps2, pkey2 = self.rot('psA')
        self.mm(ps2[:, 0:128], self.e64, cpos, True, True, ['const', 'cpos'], [pkey2])
        self.cp('dve', cmid, ps2[:, 0:128], [pkey2], ['cmid'])
        for which, dstT, nm in ((0, qT, 'cqT'), (1, kT, 'ckT')):
            for cbk in range(2):
                wv, wkey = self.load_w(dr['wC'][l, :, which * 512 + cbk * 256:which * 512 + (cbk + 1) * 256], KC, 256, scale=gp)
                for pp in range(2):
                    pr = cbk * 2 + pp

                    def epi(ps, pkey, T, pr=pr, dstT=dstT, nm=nm):
                        self.cp('act' if T % 2 == 0 else 'dve', dstT[:, pr * S_LEN + T * 512:pr * S_LEN + (T + 1) * 512], ps[:, 0:512], [pkey], [(nm, T)])
                    self.proj_fm(lambda k, wv=wv, pp=pp: wv[:, k * 256 + pp * 128:k * 256 + (pp + 1) * 128], wkey, 128, self.hT, 'hT', KC, epi)
        self.memset('pool', vC.rearrange("p (a c) -> p a c", c=65)[:, :, 64:65], 1.0, ['vC'])
        for cbk in range(2):
            wv, wkey = self.load_w(dr['wC'][l, :, 1024 + cbk * 256:1024 + (cbk + 1) * 256], KC, 256, scale=gp)

            def epi_v(ps, pkey, i, cbk=cbk):
                dst = vC[:, (i * 8 + cbk * 4) * 65:(i * 8 + cbk * 4 + 4) * 65].rearrange("p (h c) -> p h c", c=65)[:, :, 0:64]
                self.cp('act' if i % 2 == 0 else 'dve', dst, ps[:, 0:256].rearrange("p (h c) -> p h c", c=64), [pkey], ['vC'])
            self.proj_tm(lambda k, wv=wv: wv[:, k * 256:(k + 1) * 256], wkey, 256, self.hT, 'hT', KC, epi_v)
        cpos_hj = cpos.rearrange("p (j h) -> p h j", h=8)
        for i in range(NT):
            T = i // 4
            qt = i % 4
            yt = ytm[T % 2]
            ykey = f'ytm{T % 2}'
            bi, bikey = self.rot('biasp')
            bi3 = bi[:, 0:128].rearrange("p (h j) -> p h j", j=16)
            self.tt('dve', bi3[:, :, 0:i + 1], cpos_hj[:, :, 0:i + 1],
                    cmid[:, i * 8:(i + 1) * 8].rearrange("p (h c) -> p h c", c=1).broadcast_to([128, 8, i + 1]), ALU.subtract, ['cpos', 'cmid'], [bikey])
            pvs = [self.rot('psB') for _ in range(2)]
            for h in range(8):
                pr, half = h // 2, h % 2
                rows = slice(half * 64, half * 64 + 64)
                pv, pvkey = pvs[h // 4]
                hh = h % 4
                for jb in range(0, i + 1, 4):
                    js = list(range(jb, min(jb + 4, i + 1)))
                    ps, pkey = self.rot('psA')
                    for jj, j in enumerate(js):
                        self.mm(ps[:, jj * 128:(jj + 1) * 128], kT[rows, pr * S_LEN + j * 128:pr * S_LEN + (j + 1) * 128],
                                qT[rows, pr * S_LEN + i * 128:pr * S_LEN + (i + 1) * 128], True, j != i, [('ckT', j // 4), ('cqT', T)], [pkey])
                        if j == i:
                            self.mm(ps[:, jj * 128:(jj + 1) * 128], self.identb, self.causalneg, False, True, ['const'], [pkey])
                    pt, ptkey = self.rot('tmpb')
                    for jj, j in enumerate(js):
                        self.act(pt[:, jj * 128:(jj + 1) * 128], ps[:, jj * 128:(jj + 1) * 128], AF.Exp, [pkey, bikey], [ptkey], scale=0.125,
                                 bias=bi[:, h * 16 + j:h * 16 + j + 1])
                    for jj, j in enumerate(js):
                        self.mm(pv[:, hh * 65:(hh + 1) * 65], pt[:, jj * 128:(jj + 1) * 128], vC[:, (j * 8 + h) * 65:(j * 8 + h + 1) * 65],
                                j == 0, j == i, [ptkey, 'vC'], [pvkey])
            for hb in range(2):
                ydst = yt[:, qt * 512 + hb * 256:qt * 512 + (hb + 1) * 256].rearrange("p (h c) -> p h c", c=64)
                self.pv_evac(pvs[hb][0], pvs[hb][1], 4, 64, ydst, ykey)
            if qt == 3:
                for cbk in range(2):
                    wzv, wzkey = self.load_w(dr['wC'][l, :, 1536 + cbk * 256:1536 + (cbk + 1) * 256], KC, 256, scale=gp)
                    self.finish_chunk_sub(T, yt, ykey, 512, [cbk * 2, cbk * 2 + 1], cbk * 256,
                                          lambda k, wi, wzv=wzv: wzv[:, k * 256 + wi * 128:k * 256 + (wi + 1) * 128], wzkey)

    def branch_A(self, l):
        dr = self.dr
        ar = self.arena
        o = 0
        guzT = ar[:, o:o + 4 * S_LEN]; o += 4 * S_LEN
        wav = ar[:, o:o + KC * 512]; o += KC * 512
        wsT = ar[:, o:o + 512]; o += 512
        lng = ar[:, o:o + 1024].bitcast(F32); o += 1024
        lnb = ar[:, o:o + 1024].bitcast(F32); o += 1024
        bsrow = ar[0:1, o:o + 1024].bitcast(F32); o += 1024
        gp = (self.gpre, l * KC, 'gvec')
        for cbk in range(2):
            wv, wkey = self.load_w(dr['wA'][l, :, cbk * 256:(cbk + 1) * 256], KC, 256, scale=gp)
            for pp in range(2):
                wc = cbk * 2 + pp

                def epi(ps, pkey, T, wc=wc):
                    self.gelu2(ps, pkey, guzT[:, wc * S_LEN + T * 512:wc * S_LEN + (T + 1) * 512], ('guz', wc, T), 512)
                self.proj_fm(lambda k, wv=wv, pp=pp: wv[:, k * 256 + pp * 128:k * 256 + (pp + 1) * 128], wkey, 128, self.hT, 'hT', KC, epi)
        for cbk in range(2):
            wv, wkey = self.load_w(dr['wA'][l, :, 512 + cbk * 256:512 + (cbk + 1) * 256], KC, 256, scale=gp)
            for pp in range(2):
                wc = cbk * 2 + pp

                def epi(ps, pkey, T, wc=wc):
                    zg, zgkey = self.rot('tmpb')
                    self.silu2(ps, pkey, zg[:, 0:512], zgkey, 512)
                    d = guzT[:, wc * S_LEN + T * 512:wc * S_LEN + (T + 1) * 512]
                    self.tt('pool', d, d, zg[:, 0:512], ALU.mult, [zgkey, ('guz', wc, T)], [('guz', wc, T)])
                self.proj_fm(lambda k, wv=wv, pp=pp: wv[:, k * 256 + pp * 128:k * 256 + (pp + 1) * 128], wkey, 128, self.hT, 'hT', KC, epi)
        for cbk in range(2):
            self.load_w(dr['wA'][l, :, 1024 + cbk * 256:1024 + (cbk + 1) * 256], KC, 256, scale=gp,
                        dst_fn=lambda k, cbk=cbk: wav[:, k * 512 + cbk * 256:k * 512 + (cbk + 1) * 256], dstkey='wav')
        stg, skey = self.rot('stage')
        self.DMA(stg[:, 0:512], dr['awsT'][l].rearrange("s g t -> s (g t)"), w=[skey])
        self.tt('pool', wsT.rearrange("p (g t) -> p g t", g=4), stg[:, 0:512].rearrange("p (g t) -> p g t", g=4),
                self.triu.rearrange("p (o t) -> p o t", o=1).broadcast_to([128, 4, 128]), ALU.mult, [skey, 'const'], ['wsT'])
        self.DMA(lng, dr['lng'][l], w=['lng'])
        self.DMA(lnb, dr['lnb'][l], w=['lnb'])
        self.DMA(bsrow, dr['abs'][l], w=['bsrow'])
        gst = ar[:, o:o + NT * 512]; o += NT * 512
        assert o <= ARENA_EL
        st = self.cvecs
        ssum, ssq, mean, m2, var, rstd = (st[:, a * 16:(a + 1) * 16] for a in range(6))
        for c in range(NT):
            T = c // 4
            ps, pkey = self.rot('psA')
            for k in range(KC):
                self.mm(ps[:, 0:512], self.hT[:, k * S_LEN + c * 128:k * S_LEN + (c + 1) * 128], wav[:, k * 512:(k + 1) * 512], k == 0, k == KC - 1, ['wav', ('hT', T)], [pkey])
            g2, g2key = self.rot('tmpf')
            self.gelu2(ps, pkey, g2[:, 0:512], g2key, 512)
            self.S.op('dve', lambda e, c=c, g2=g2: e.tensor_reduce(out=ssum[:, c:c + 1], in_=g2[:, 0:512], axis=AX.X, op=ALU.add), [g2key], ['astat'])
            junk, jkey = self.rot('tmpb')
            self.act(junk[:, 0:512], g2[:, 0:512], AF.Square, [g2key], [jkey, 'astat'], accum_out=ssq[:, c:c + 1])
            self.cp('pool', gst[:, c * 512:(c + 1) * 512], g2[:, 0:512], [g2key], [('gst', c)])
        self.ts('dve', mean, ssum, 1.0 / 512, None, ALU.mult, None, ['astat'], ['astat'])
        self.tt('dve', m2, mean, mean, ALU.mult, ['astat'], ['astat'])
        self.stt('dve', var, ssq, 1.0 / 512, m2, ALU.mult, ALU.subtract, ['astat'], ['astat'])
        self.act(var, var, AF.Sqrt, ['astat', 'epsb'], ['astat'], bias=self.epsb[:, 1:2])
        self.recip(rstd, var, ['astat'], ['astat'])
        for c in range(NT):
            T = c // 4
            g2, g2key = self.rot('tmpf')
            self.ts('dve', g2[:, 0:512], gst[:, c * 512:(c + 1) * 512], mean[:, c:c + 1], rstd[:, c:c + 1], ALU.subtract, ALU.mult, [('gst', c), 'astat'], [g2key])
            self.tt('pool', g2[:, 0:512], g2[:, 0:512], lng, ALU.mult, [g2key, 'lng'], [g2key])
            vln, vlkey = self.rot('tmpb')
            self.tt('dve', vln[:, 0:512], g2[:, 0:512], lnb, ALU.add, [g2key, 'lnb'], [vlkey])
            ps2, pkey2 = self.rot('psA')
            for g in range(4):
                self.mm(ps2[:, g * 128:(g + 1) * 128], vln[:, g * 128:(g + 1) * 128], wsT[:, g * 128:(g + 1) * 128], True, False, [vlkey, 'wsT'], [pkey2])
                self.mm(ps2[:, g * 128:(g + 1) * 128], self.onesf[0:1, 0:128], bsrow[0:1, g * 128:(g + 1) * 128], False, True, ['const', 'bsrow'], [pkey2])
            ydst = self.yT[:].rearrange("p (g t) -> p g t", g=4)[:, :, c * 128:(c + 1) * 128]
            gsrc = guzT.rearrange("p (g t) -> p g t", g=4)[:, :, c * 128:(c + 1) * 128]
            self.stt('dve', ydst, ps2[:, 0:512].rearrange("p (g t) -> p g t", g=4), 0.5, gsrc, ALU.mult, ALU.mult,
                     [pkey2] + [('guz', wc, T) for wc in range(4)], [('yT', T)])

    def branch_B(self, l):
        dr = self.dr
        ar = self.arena
        S = self.S
        gp = (self.gpre, l * KC, 'gvec')
        ALL5 = ('pe', 'act', 'dve', 'pool', 'sp')
        cosF = ar[:, 0:4096].bitcast(F32)
        sinF = ar[:, 4096:8192].bitcast(F32)
        qTa = ar[:, 8192:16384]
        ksTa = ar[:, 16384:18432]
        kwT = ar[:, 18432:20480]
        kcT = ar[:, 20480:22528]
        vcT = ar[:, 22528:24576]
        vs = ar[:, 24576:24576 + 1040]
        vw = ar[:, 25616:25616 + 1040]
        kcmpT = ar[:, 26656:26656 + 128]
        vcmpx = ar[:, 26784:26784 + 97]
        w2kb = ar[:, 26884:26884 + 64]
        w2vb = ar[:, 26948:26948 + 64]
        posb = ar[:, 27012:27012 + 64]
        w1kb = ar[:, 0:2048]
        w1vb = ar[:, 2048:4096]
        ytm = [ar[:, 0:1024], ar[:, 1024:2048]]
        ocmp = ar[:, 2048:4096].bitcast(F32)
        cmpneg = ar[:, 4096:6144]
        keepadd = ar[:, 6144:8192].bitcast(F32)
        impacc = ar[:, 20480:20736].bitcast(F32)
        imp2 = ar[:, 20736:20992].bitcast(F32)
        selneg = ar[:, 20992:21120]
        bgs = self.cvecs[:, 0:384]
        S.barrier()
        sp3 = self.smallproj[:].rearrange("p (i c) -> p i c", c=32)
        bgs3 = bgs.rearrange("p (i c) -> p i c", c=24)
        self.act(bgs3, sp3[:, :, 8:32], AF.Tanh, ['smallproj'], ['bgs'], scale=0.5)
        self.ts('dve', bgs, bgs, 0.5, 0.5, ALU.mult, ALU.add, ['bgs'], ['bgs'])
        for g in range(2):
            wB = dr['wB'][l, g]
            S.barrier(ALL5)
            self.DMA(cosF, dr['cosF'], w=['cosF'])
            self.DMA(sinF, dr['sinF'], w=['sinF'])
            self.DMA(ksTa[64:96, :], dr['onehot'], w=[('ksTa', T) for T in range(4)])
            self.memset('pool', vs.rearrange("p (a c) -> p a c", c=65)[:, :, 64:65], 1.0, ['vs'])
            self.memset('pool', vw.rearrange("p (a c) -> p a c", c=65)[:, :, 64:65], 1.0, ['vw'])
            rope_dsts = [((qTa, 0, 'qTa'), (qTa, 1, 'qTa')), ((qTa, 2, 'qTa'), (qTa, 3, 'qTa')),
                         ((ksTa, 0, 'ksTa'), (kwT, 0, 'kwT')), ((kcT, 0, 'kcT'), (vcT, 0, 'vcT'))]
            for ti, (dA, dB) in enumerate(rope_dsts):
                wv, wkey = self.load_w(wB[:, ti * 256:(ti + 1) * 256], KC, 256, scale=gp)
                for T in range(NCH):
                    pa, pakey = self.rot('psA')
                    for k in range(KC):
                        self.mm(pa[:, 0:512], wv[:, k * 256:k * 256 + 128], self.hT[:, k * S_LEN + T * 512:k * S_LEN + (T + 1) * 512], k == 0, k == KC - 1, [wkey, ('hT', T)], [pakey])
                    pb, pbkey = self.rot('psA')
                    for k in range(KC):
                        self.mm(pb[:, 0:512], wv[:, k * 256 + 128:k * 256 + 256], self.hT[:, k * S_LEN + T * 512:k * S_LEN + (T + 1) * 512], k == 0, k == KC - 1, [wkey, ('hT', T)], [pbkey])
                    t1, t1key = self.rot('tmpf')
                    t2, t2key = self.rot('tmpf')
                    cs = slice(T * 512, (T + 1) * 512)
                    novc = (ti == 3)
                    np_ = 64 if novc else 128
                    self.tt('dve', t1[0:np_, 0:512], pa[0:np_, 0:512], cosF[0:np_, cs], ALU.mult, [pakey, 'cosF'], [t1key])
                    self.tt('dve', t2[0:np_, 0:512], pb[0:np_, 0:512], sinF[0:np_, cs], ALU.mult, [pbkey, 'sinF'], [t2key])
                    (ta, ha, ka), (tb, hb, kb) = dA, dB
                    self.tt('pool', ta[0:64, ha * S_LEN + T * 512:ha * S_LEN + (T + 1) * 512], t1[0:64, 0:512], t2[0:64, 0:512], ALU.add, [t1key, t2key], [(ka, T)])
                    if novc:
                        self.cp('act', tb[0:64, hb * S_LEN + T * 512:hb * S_LEN + (T + 1) * 512], pa[64:128, 0:512], [pakey], [(kb, T)])
                    else:
                        self.tt('dve', tb[0:64, hb * S_LEN + T * 512:hb * S_LEN + (T + 1) * 512], t1[64:128, 0:512], t2[64:128, 0:512], ALU.add, [t1key, t2key], [(kb, T)])
            wv, wkey = self.load_w(wB[:, 1024:1152], KC, 128, scale=gp)

            def epi_v(ps, pkey, i):
                self.cp('act', vs[:, i * 65:i * 65 + 64], ps[:, 0:64], [pkey], ['vs'])
                self.cp('dve', vw[:, i * 65:i * 65 + 64], ps[:, 64:128], [pkey], ['vw'])
            self.proj_tm(lambda k, wv=wv: wv[:, k * 128:(k + 1) * 128], wkey, 128, self.hT, 'hT', KC, epi_v)
            if int(os.environ.get("K_BSTOP", "99")) < 1:
                continue
            S.barrier(ALL5)
            self.load_w(dr['w1k'][l].rearrange("d l m -> d (l m)"), 1, 2048, dst_fn=lambda k: w1kb[0:64, :], dstkey='w1kb', rows=64)
            self.load_w(dr['w1v'][l].rearrange("d l m -> d (l m)"), 1, 2048, dst_fn=lambda k: w1vb[0:64, :], dstkey='w1vb', rows=64)
            self.load_w(dr['w2k'][l], 1, 64, dst_fn=lambda k: w2kb[0:64, :], dstkey='w2kb', rows=64, mulc=0.5)
            self.load_w(dr['w2v'][l], 1, 64, dst_fn=lambda k: w2vb[0:64, :], dstkey='w2vb', rows=64, mulc=0.5)
            self.load_w(dr['posk'][l], 1, 32, dst_fn=lambda k: posb[0:64, 0:32], dstkey='posb', rows=64)
            self.load_w(dr['posv'][l], 1, 32, dst_fn=lambda k: posb[0:64, 32:64], dstkey='posb', rows=64)
            cstop = int(os.environ.get("K_CSTOP", "99"))
            if cstop >= 1:
                self.cp('pool', vcmpx[:, 64:97], self.ovl[:, 0:33], ['const'], ['vcmpx'])
            for kv, (srcT, skeyn, w1b, w1key, w2b, w2key, po) in enumerate(((kcT, 'kcT', w1kb, 'w1kb', w2kb, 'w2kb', 0), (vcT, 'vcT', w1vb, 'w1vb', w2vb, 'w2vb', 32))):
                if cstop < 2:
                    break
                ph, phkey = self.rot('psA')
                for ll in range(32):
                    self.mm(ph[0:64, 0:127], w1b[0:64, ll * 64:(ll + 1) * 64], srcT[0:64, ll:ll + 2017:16], ll == 0, ll == 31, [w1key] + [(skeyn, T) for T in range(4)], [phkey])
                if cstop < 3:
                    break
                pb2, pb2key = self.rot('psA')
                for ll in range(32):
                    self.mm(pb2[0:64, 0:1], w1b[0:64, ll * 64:(ll + 1) * 64], posb[0:64, po + ll:po + ll + 1], ll == 0, ll == 31, [w1key, 'posb'], [pb2key])
                if cstop < 4:
                    break
                hb_, hbkey = self.rot('small')
                self.cp('dve', hb_[0:64, 0:1], pb2[0:64, 0:1], [pb2key], [hbkey])
                self.ts('dve', hb_[0:64, 1:2], hb_[0:64, 0:1], 0.5, None, ALU.mult, None, [hbkey], [hbkey])
                th, thkey = self.rot('tmpf')
                self.act(th[0:64, 0:127], ph[0:64, 0:127], AF.Tanh, [phkey, hbkey], [thkey], scale=0.5, bias=hb_[0:64, 1:2])
                if cstop < 5:
                    break
                xb, xbkey = self.rot('tmpf')
                var = os.environ.get("K_VAR", "")
                if 'a' not in var:
                    self.ts('dve', xb[0:64, 0:127], ph[0:64, 0:127], hb_[0:64, 0:1], None, ALU.add, None, [phkey, hbkey], [xbkey])
                a1, a1key = self.rot('tmpb')
                if 'b' not in var:
                    self.stt('dve', a1[0:64, 0:127], th[0:64, 0:127], 1.0, xb[0:64, 0:127], ALU.add, ALU.mult, [thkey, xbkey], [a1key])
                if cstop < 6:
                    break
                po2, po2key = self.rot('psA')
                if kv == 0:
                    self.mm(po2[0:64, 0:127], w2b[0:64, 0:64], a1[0:64, 0:127], True, True, [w2key, a1key], [po2key])
                    self.cp('dve', kcmpT[0:64, 0:127], po2[0:64, 0:127], [po2key], ['kcmpT'])
                else:
                    self.mm(po2[0:127, 0:64], a1[0:64, 0:127], w2b[0:64, 0:64], True, True, [w2key, a1key], [po2key])
                    self.cp('dve', vcmpx[0:127, 0:64], po2[0:127, 0:64], [po2key], ['vcmpx'])
            if int(os.environ.get("K_BSTOP", "99")) < 2:
                continue
            S.barrier(ALL5)
            self.DMA(cmpneg, dr['cmpneg'], w=['cmpneg'])
            self.DMA(keepadd, dr['keepadd'], w=['keepadd'])
            ka4 = keepadd.rearrange("p (a i c) -> p a i c", a=2, c=32)
            oc4 = ocmp.rearrange("p (r q c) -> p r q c", r=4, c=64)
            for T in range(NCH):
                yt = ytm[T % 2]
                ykey = f'ytmB{T % 2}'
                bgT = bgs3[:, 4 * T:4 * T + 4, :]
                for r in range(4):
                    h = 4 * g + r
                    ps, pkey = self.rot('psA')
                    self.mm(ps[0:127, 0:512], kcmpT[0:64, 0:127], qTa[0:64, r * S_LEN + T * 512:r * S_LEN + (T + 1) * 512], True, False, ['kcmpT', ('qTa', T)], [pkey])
                    self.mm(ps[0:127, 0:512], self.identb[0:127, 0:127], cmpneg[0:127, T * 512:(T + 1) * 512], False, True, ['const', 'cmpneg'], [pkey])
                    et, etkey = self.rot('tmpb')
                    self.act(et[0:127, 0:512], ps[0:127, 0:512], AF.Exp, [pkey], [etkey], scale=0.125)
                    R, Rkey = self.rot('psB')
                    for qt in range(4):
                        self.mm(R[:, qt * 97:(qt + 1) * 97], et[0:127, qt * 128:(qt + 1) * 128], vcmpx[0:127, 0:97], True, True, [etkey, 'vcmpx'], [Rkey])
                    R3 = R[:, 0:388].rearrange("p (q c) -> p q c", c=97)
                    rz, rzkey = self.rot('small')
                    rz3 = lambda a: rz[:, a:a + 4].rearrange("p (q c) -> p q c", c=1)
                    self.ts('dve', rz3(0), R3[:, :, 64:65], 1e-30, None, ALU.max, None, [Rkey], [rzkey])
                    self.recip(rz[:, 4:8], rz[:, 0:4], [rzkey], [rzkey])
                    if r == 0:
                        self.tt('dve', impacc.rearrange("p (q c) -> p q c", c=32), R3[:, :, 65:97], rz3(4).broadcast_to([128, 4, 32]), ALU.mult, [Rkey, rzkey], ['impacc'])
                    else:
                        self.tt('dve', imp2.rearrange("p (q c) -> p q c", c=32), R3[:, :, 65:97], rz3(4).broadcast_to([128, 4, 32]), ALU.mult, [Rkey, rzkey], ['imp2'])
                        self.tt('pool', impacc, impacc, imp2, ALU.add, ['imp2', 'impacc'], ['impacc'])
                    self.tt('dve', rz3(8), rz3(4), bgT[:, :, h * 3:h * 3 + 1], ALU.mult, [rzkey, 'bgs'], [rzkey])
                    self.tt('dve', oc4[:, r], R3[:, :, 0:64], rz3(8).broadcast_to([128, 4, 64]), ALU.mult, [Rkey, rzkey], [('ocmp', r)])
                if int(os.environ.get("K_BSTOP", "99")) < 3:
                    continue
                i3 = imp2.rearrange("p (q c) -> p q c", c=32)
                self.tt('dve', i3, impacc.rearrange("p (q c) -> p q c", c=32), ka4[:, 0, 4 * T:4 * T + 4, :], ALU.mult, ['impacc', 'keepadd', 'imp2'], ['imp2'])
                self.tt('dve', i3, i3, ka4[:, 1, 4 * T:4 * T + 4, :], ALU.add, ['imp2', 'keepadd'], ['imp2'])
                m8, m8key = self.rot('small')
                for qt in range(4):
                    self.S.op('dve', lambda e, qt=qt, m8=m8: e.max(out=m8[:, qt * 8:(qt + 1) * 8], in_=imp2[:, qt * 32:(qt + 1) * 32]), ['imp2'], [m8key])
                    self.ts('dve', selneg[:, qt * 32:(qt + 1) * 32], imp2[:, qt * 32:(qt + 1) * 32], m8[:, qt * 8 + 7:qt * 8 + 8], NEG, ALU.is_lt, ALU.mult, ['imp2', m8key], ['selneg'])
                pt, ptkey = self.rot('psT')
                for qt in range(4):
                    self.tr(pt[0:32, qt * 128:(qt + 1) * 128], selneg[:, qt * 32:(qt + 1) * 32], ['selneg'], [ptkey])
                for r in range(4):
                    self.cp('act' if r % 2 == 0 else 'dve', qTa[64:96, r * S_LEN + T * 512:r * S_LEN + (T + 1) * 512], pt[0:32, 0:512], [ptkey], [('qTa', T)])
                if int(os.environ.get("K_BSTOP", "99")) < 4:
                    continue
                for r in range(4):
                    h = 4 * g + r
                    acs, acskey = self.rot('psB')
                    first_s = True
                    for j in range(0, 4 * T + 4):
                        lo = max(128 * j, 512 * T)
                        w = 512 * (T + 1) - lo
                        diag = j >= 4 * T
                        ps, pkey = self.rot('psA')
                        self.mm(ps[:, 0:w], ksTa[0:96, j * 128:(j + 1) * 128], qTa[0:96, r * S_LEN + lo:r * S_LEN + lo + w], True, not diag, [('ksTa', j // 4), ('qTa', T)], [pkey])
                        if diag:
                            self.mm(ps[:, 0:128], self.identb, self.causalneg, False, True, ['const'], [pkey])
                        pT, pTkey = self.rot('tmpb')
                        self.act(pT[:, 0:w], ps[:, 0:w], AF.Exp, [pkey], [pTkey], scale=0.125)
                        for qt in range(4):
                            i = 4 * T + qt
                            if i < j:
                                continue
                            off = i * 128 - lo
                            self.mm(acs[:, qt * 65:(qt + 1) * 65], pT[:, off:off + 128], vs[:, j * 65:(j + 1) * 65], first_s, j == i, [pTkey, 'vs'], [acskey], skip=True)
                            first_s = False
                    acw, acwkey = self.rot('psB')
                    first_w = True
                    for j in range(max(0, 4 * T - 2), 4 * T + 4):
                        i_lo = max(j, 4 * T)
                        i_hi = min(j + 2, 4 * T + 3)
                        lo = i_lo * 128
                        w = (i_hi + 1) * 128 - lo
                        masks = []
                        if i_lo == j:
                            masks.append((0, self.causalneg))
                        if i_hi == j + 2:
                            masks.append(((j + 2) * 128 - lo, self.anticausalneg))
                        ps, pkey = self.rot('psA')
                        self.mm(ps[:, 0:w], kwT[0:64, j * 128:(j + 1) * 128], qTa[0:64, r * S_LEN + lo:r * S_LEN + lo + w], True, len(masks) == 0, [('kwT', j // 4), ('qTa', T)], [pkey])
                        for mi, (mo, mk) in enumerate(masks):
                            self.mm(ps[:, mo:mo + 128], self.identb, mk, False, mi == len(masks) - 1, ['const'], [pkey])
                        pT, pTkey = self.rot('tmpb')
                        self.act(pT[:, 0:w], ps[:, 0:w], AF.Exp, [pkey], [pTkey], scale=0.125)
                        for i in range(i_lo, i_hi + 1):
                            qt = i - 4 * T
                            off = i * 128 - lo
                            self.mm(acw[:, qt * 65:(qt + 1) * 65], pT[:, off:off + 128], vw[:, j * 65:(j + 1) * 65], first_w, j == i, [pTkey, 'vw'], [acwkey], skip=True)
                            first_w = False
                    s3 = acs[:, 0:260].rearrange("p (q c) -> p q c", c=65)
                    w3 = acw[:, 0:260].rearrange("p (q c) -> p q c", c=65)
                    rz, rzkey = self.rot('small')
                    rz3 = lambda a: rz[:, a:a + 4].rearrange("p (q c) -> p q c", c=1)
                    self.ts('dve', rz3(0), s3[:, :, 64:65], 1e-30, None, ALU.max, None, [acskey], [rzkey])
                    self.ts('dve', rz3(4), w3[:, :, 64:65], 1e-30, None, ALU.max, None, [acwkey], [rzkey])
                    self.recip(rz[:, 8:16], rz[:, 0:8], [rzkey], [rzkey])
                    self.tt('dve', rz3(16), rz3(8), bgT[:, :, h * 3 + 1:h * 3 + 2], ALU.mult, [rzkey, 'bgs'], [rzkey])
                    self.tt('dve', rz3(20), rz3(12), bgT[:, :, h * 3 + 2:h * 3 + 3], ALU.mult, [rzkey, 'bgs'], [rzkey])
                    ta, takey = self.rot('tmpf')
                    tb, tbkey = self.rot('tmpf')
                    ta3 = ta[:, 0:256].rearrange("p (q c) -> p q c", c=64)
                    tb3 = tb[:, 0:256].rearrange("p (q c) -> p q c", c=64)
                    self.tt('dve', ta3, s3[:, :, 0:64], rz3(16).broadcast_to([128, 4, 64]), ALU.mult, [acskey, rzkey], [takey])
                    self.tt('pool', ta3, ta3, oc4[:, r], ALU.add, [takey, ('ocmp', r)], [takey])
                    self.tt('dve', tb3, w3[:, :, 0:64], rz3(20).broadcast_to([128, 4, 64]), ALU.mult, [acwkey, rzkey], [tbkey])
                    ydst = yt.rearrange("p (q c) -> p q c", c=256)[:, :, r * 64:(r + 1) * 64]
                    self.tt('dve', ydst, ta3, tb3, ALU.add, [takey, tbkey], [ykey])
                wzv, wzkey = self.load_w(wB[:, 1152:1408], KC, 256, scale=gp)
                self.finish_chunk_sub(T, yt, ykey, 256, [2 * g, 2 * g + 1], 0,
                                      lambda k, wi, wzv=wzv: wzv[:, k * 256 + wi * 128:k * 256 + (wi + 1) * 128], wzkey)
        S.barrier(ALL5)


_CACHE = {}


def _prep(inputs):
    blobs = _host_layer_blobs(inputs)
    consts = _host_consts()
    return blobs, consts


def kernel(**inputs):
    inputs = {k: np.asarray(v) for k, v in inputs.items()}
    blobs, consts = _prep(inputs)
    dtm = lambda a: BF16 if a.dtype == ml_dtypes.bfloat16 else F32
    blob_shapes = {k: (v.shape, dtm(v)) for k, v in blobs.items()}
    const_shapes = {k: (v.shape, dtm(v)) for k, v in consts.items()}
    b = Builder(blob_shapes, const_shapes)
    nc = b.build()
    in_maps = []
    for c in range(8):
        m = {'x': np.ascontiguousarray(inputs['x'][c]), 'mem': np.ascontiguousarray(inputs['mem'][c])}
        m.update(blobs)
        m.update(consts)
        in_maps.append(m)
    res = run_bass_kernel_spmd(nc, in_maps, core_ids=list(range(8)))
    return np.stack([r['out'] for r in res.results], 0).astype(np.float32)
```

```python
import math
from contextlib import ExitStack
import numpy as np
import ml_dtypes
import concourse.bass as bass
import concourse.mybir as mybir
from concourse.bass_utils import run_bass_kernel_spmd

F32 = mybir.dt.float32
BF16 = mybir.dt.bfloat16
AF = mybir.ActivationFunctionType
ALU = mybir.AluOpType
AX = mybir.AxisListType

S_LEN = 2048
D = 1024
NT = 16
NCH = 4
KC = 8
MEM = 256
NEG = -1.0e30
EPS = 1e-6
GC1 = math.sqrt(2.0 / math.pi)
GC2 = GC1 * 0.044715

ENGS = ['pe', 'act', 'dve', 'pool', 'sp']
SAME_ENG_SYNC = {'pe': False, 'act': True, 'dve': True, 'pool': True, 'sp': False}
N_DMA_SEMS = 12


class Sched:
    def __init__(self):
        self.ops = []
        self.per_eng = {e: [] for e in ENGS}
        self.last_w = {}
        self.readers = {}
        self.dma_count = {e: 0 for e in ENGS}

    def op(self, eng, fn, reads=(), writes=(), dma=False, nodrain=False):
        oid = len(self.ops)
        deps = set()
        for k in reads:
            w = self.last_w.get(k)
            if w is not None:
                deps.add(w)
            if isinstance(k, str) and k.startswith('ps'):
                for r in self.readers.get(k, ()):
                    if self.ops[r]['eng'] != eng:
                        deps.add(r)
        for k in writes:
            w = self.last_w.get(k)
            if w is not None:
                deps.add(w)
            for r in self.readers.get(k, ()):
                deps.add(r)
        for k in reads:
            self.readers.setdefault(k, []).append(oid)
        for k in writes:
            self.last_w[k] = oid
            self.readers[k] = []
        deps.discard(oid)
        o = dict(id=oid, eng=eng, fn=fn, deps=sorted(deps), dma=dma, nodrain=nodrain)
        if dma:
            n = self.dma_count[eng]
            self.dma_count[eng] = n + 1
            o['dsem'] = (eng, n % N_DMA_SEMS)
            o['dval'] = 16 * (n // N_DMA_SEMS + 1)
        self.ops.append(o)
        self.per_eng[eng].append(oid)
        return oid

    def barrier(self, engs=('pe', 'act', 'dve', 'pool')):
        last = {}
        for e in engs:
            if self.per_eng[e]:
                last[e] = self.per_eng[e][-1]
        for e in engs:
            deps = [v for k, v in last.items() if k != e]
            oid = len(self.ops)
            self.ops.append(dict(id=oid, eng=e, fn=None, deps=sorted(deps), dma=False))
            self.per_eng[e].append(oid)

    def finish(self, eng, dep_ops):
        oid = len(self.ops)
        self.ops.append(dict(id=oid, eng=eng, fn=None, deps=sorted(dep_ops), dma=False))
        self.per_eng[eng].append(oid)

    def emit(self, block, sems, dsems):
        ops = self.ops
        for o in ops:
            o['sig'] = False
        for o in ops:
            for d in o['deps']:
                od = ops[d]
                if od['dma']:
                    continue
                if od['fn'] is None:
                    continue
                if od['eng'] != o['eng']:
                    od['sig'] = True
        cnt = {e: 0 for e in ENGS}
        for o in ops:
            if o['sig']:
                cnt[o['eng']] += 1
                o['sidx'] = cnt[o['eng']]
        known = {e: {} for e in ENGS}
        pos = {}
        for e in ENGS:
            for i_, oid_ in enumerate(self.per_eng[e]):
                pos[oid_] = i_
        last_drain = {e: -1 for e in ENGS}
        for o in ops:
            e = o['eng']
            kn = known[e]
            wd = {}
            o['drain'] = False
            if SAME_ENG_SYNC[e] and o['fn'] is not None and not o.get('nodrain'):
                for d in o['deps']:
                    od = ops[d]
                    if od['eng'] == e and od['fn'] is not None and not od['dma'] and pos[d] > last_drain[e]:
                        o['drain'] = True
                if o['drain']:
                    last_drain[e] = pos[o['id']] - 1
            for d in o['deps']:
                od = ops[d]
                if od['fn'] is None:
                    for k2, v2 in od['snap'].items():
                        if od['eng'] == e and kn.get(k2, 0) < v2:
                            kn[k2] = v2
                    continue
                if od['dma']:
                    key = ('d',) + od['dsem']
                    val = od['dval']
                else:
                    if od['eng'] == e:
                        continue
                    key = od['eng']
                    val = od['sidx']
                if kn.get(key, 0) >= val:
                    continue
                wd[key] = max(wd.get(key, 0), val)
                kn[key] = val
                for k2, v2 in od['snap'].items():
                    if kn.get(k2, 0) < v2:
                        kn[k2] = v2
            o['waits'] = wd
            o['snap'] = dict(kn)
        engobj = {'pe': block.tensor, 'act': block.scalar, 'dve': block.vector,
                  'pool': block.gpsimd, 'sp': block.sync}

        def make(e):
            def body(eh):
                for oid in self.per_eng[e]:
                    o = ops[oid]
                    for key, val in o['waits'].items():
                        if isinstance(key, tuple):
                            eh.wait_ge(dsems[(key[1], key[2])], val)
                        else:
                            eh.wait_ge(sems[key], val)
                    if o['fn'] is None:
                        continue
                    if o['drain']:
                        eh.drain()
                    ins = o['fn'](eh)
                    if o['dma']:
                        ins.then_inc(dsems[o['dsem']], 16)
                    elif o['sig']:
                        ins.then_inc(sems[e], 1)
            return body

        for e in ENGS:
            if self.per_eng[e]:
                engobj[e](make(e))


O_AU, O_AV, O_AZ = 0, 512, 1024
O_BQ, O_BKC, O_BVC, O_BKS, O_BVS, O_BKW, O_BVW, O_BG, O_BZ = 1536, 2048, 2176, 2304, 2432, 2560, 2688, 2816, 2840
O_CQ, O_CK, O_CV, O_CF, O_CZ = 3352, 3864, 4376, 4888, 4896
O_MQ, O_MZ = 5408, 5920
WB_COLS = 1408


def _swap_halves(w, hd=64):
    sh = w.shape
    w4 = w.reshape(sh[:-1] + (sh[-1] // hd, 2, hd // 2))
    return w4[..., ::-1, :].reshape(sh)


def _host_consts():
    c = {}
    half = 32
    freqs = 10000.0 ** (-np.arange(half, dtype=np.float32) / half)
    ang = np.arange(S_LEN, dtype=np.float32)[:, None] * freqs[None, :]
    cos, sin = np.cos(ang).astype(np.float32).T, np.sin(ang).astype(np.float32).T
    cos64 = np.concatenate([cos, cos], 0)
    sin64 = np.concatenate([-sin, sin], 0)
    c['cosF'] = np.ascontiguousarray(np.concatenate([cos64, cos64], 0))
    c['sinF'] = np.ascontiguousarray(np.concatenate([sin64, sin64], 0))
    p = np.arange(128)
    bf = ml_dtypes.bfloat16
    ident = (p[:, None] == p[None, :]).astype(np.float32)
    causalneg = np.where(p[:, None] > p[None, :], NEG, 0.0).astype(np.float32)
    anticausalneg = np.where(p[:, None] <= p[None, :], NEG, 0.0).astype(np.float32)
    cb = np.zeros((128, 128 * 3), np.float32)
    cb[:, 0:128] = ident
    cb[:, 128:256] = causalneg
    cb[:, 256:384] = anticausalneg
    t = np.arange(S_LEN)
    cmpneg = np.where((p[:, None] * 16 + 31) > t[None, :], NEG, 0.0).astype(np.float32)
    c['cb'] = cb.astype(bf)
    c['cmpneg'] = cmpneg.astype(bf)
    onehot = ((t[None, :] // 64) == np.arange(32)[:, None]).astype(np.float32)
    c['onehot'] = onehot.astype(bf)
    ci = np.arange(127) * 16
    sj = np.arange(32) * 64
    ovl = ((ci[:, None] <= sj[None, :] + 63) & (ci[:, None] + 31 >= sj[None, :])).astype(np.float32)
    ov = np.zeros((128, 33), np.float32)
    ov[:127, 0] = 1.0
    ov[:127, 1:] = ovl
    c['ovl'] = ov.astype(bf)
    cur = t // 64
    blk = np.arange(32)
    forced = (blk[None, :] == 0) | (blk[None, :] == cur[:, None]) | (blk[None, :] == cur[:, None] - 1)
    future = blk[None, :] > cur[:, None]
    keep = (~(forced | future)).astype(np.float32)
    addc = np.where(forced, 1e9, np.where(future, NEG, 0.0)).astype(np.float32)
    ka = np.zeros((128, 2, NT, 32), np.float32)
    ka[:, 0] = keep.reshape(NT, 128, 32).transpose(1, 0, 2)
    ka[:, 1] = addc.reshape(NT, 128, 32).transpose(1, 0, 2)
    c['keepadd'] = ka.reshape(128, -1)
    cf = np.zeros((128, 4 * 128), np.float32)
    cf[:, 0:128] = ident
    cf[:, 128:256] = (p[:, None] <= p[None, :]).astype(np.float32)
    cf[:, 256:384] = 1.0
    cf[64, 384:512] = 1.0
    c['cf'] = cf
    return c


def _host_layer_blobs(inp):
    out = {}
    w_in = inp['w_in']
    L = w_in.shape[0]
    f32 = np.float32
    sl = lambda o, n: w_in[:, :, o:o + n]
    out['wM'] = np.ascontiguousarray(np.concatenate([sl(O_MQ, 512), sl(O_MZ, 512)], -1))
    out['wA'] = np.ascontiguousarray(np.concatenate([sl(O_AU, 512), sl(O_AZ, 512), sl(O_AV, 512)], -1))
    out['wC'] = np.ascontiguousarray(np.concatenate([sl(O_CQ, 512), sl(O_CK, 512), sl(O_CV, 512), sl(O_CZ, 512)], -1))
    out['wsm'] = np.ascontiguousarray(np.concatenate([sl(O_CF, 8), sl(O_BG, 24)], -1))
    wB = np.zeros((L, 2, D, WB_COLS), f32)
    for g in range(2):
        parts = []
        for pr in range(2):
            a = sl(O_BQ + g * 256 + pr * 128, 128)
            parts += [a, _swap_halves(a)]
        ksw = np.concatenate([sl(O_BKS + g * 64, 64), sl(O_BKW + g * 64, 64)], -1)
        parts += [ksw, _swap_halves(ksw)]
        kcvc = np.concatenate([sl(O_BKC + g * 64, 64), sl(O_BVC + g * 64, 64)], -1)
        parts += [kcvc, _swap_halves(kcvc)]
        parts += [np.concatenate([sl(O_BVS + g * 64, 64), sl(O_BVW + g * 64, 64)], -1)]
        parts += [sl(O_BZ + g * 256, 256)]
        wB[:, g] = np.concatenate(parts, -1)
    out['wB'] = wB
    out['wmem'] = np.ascontiguousarray(inp['w_mem_kv'])
    out['wbr'] = np.ascontiguousarray(inp['w_br'])
    out['wgate'] = np.ascontiguousarray(inp['w_gate'].reshape(L, D, 4 * D))
    out['wo'] = np.ascontiguousarray(inp['w_o'])
    fm = lambda v: np.ascontiguousarray(v.reshape(L, KC, 128).transpose(2, 0, 1))
    out['gpre'] = fm(inp['g_pre']).reshape(128, -1)
    out['gmem'] = fm(inp['g_mem']).reshape(128, -1)
    rep = lambda v: np.ascontiguousarray(np.broadcast_to(v[:, None, :], (L, 128, v.shape[-1])))
    out['gpost'] = rep(inp['g_post'])
    out['lng'] = rep(inp['a_ln_g'])
    out['lnb'] = rep(inp['a_ln_b'])
    out['fbias'] = rep(inp['c_fbias'])
    out['abs'] = np.ascontiguousarray(inp['a_bs'].reshape(L, 1, 512))
    out['awsT'] = np.ascontiguousarray(inp['a_ws'].transpose(0, 3, 1, 2))
    out['posk'] = np.ascontiguousarray(inp['b_cmp_pos_k'].transpose(0, 2, 1))
    out['posv'] = np.ascontiguousarray(inp['b_cmp_pos_v'].transpose(0, 2, 1))
    out['w1k'] = np.ascontiguousarray(inp['b_cmp_w1_k'].reshape(L, 32, 64, 64).transpose(0, 2, 1, 3))
    out['w1v'] = np.ascontiguousarray(inp['b_cmp_w1_v'].reshape(L, 32, 64, 64).transpose(0, 2, 1, 3))
    out['w2k'] = np.ascontiguousarray(inp['b_cmp_w2_k'])
    out['w2v'] = np.ascontiguousarray(inp['b_cmp_w2_v'])
    return out


import os
ARENA_EL = 29184


class Builder:
    def __init__(self, blob_shapes, const_shapes, debug=None, nlayers=2, branches="MACB"):
        self.debug = debug or []
        self.nlayers = nlayers
        self.branches = branches
        nc = self.nc = bass.Bass("TRN2", target_bir_lowering=False)
        self.S = Sched()
        self.dr = {}
        self.dr['x'] = nc.dram_tensor("x", [S_LEN, D], F32, kind="ExternalInput").ap()
        self.dr['mem'] = nc.dram_tensor("mem", [MEM, D], F32, kind="ExternalInput").ap()
        for k, (shp, dt) in {**blob_shapes, **const_shapes}.items():
            self.dr[k] = nc.dram_tensor(k, list(shp), dt, kind="ExternalInput").ap()
        self.dr['out'] = nc.dram_tensor("out", [S_LEN, D], F32, kind="ExternalOutput").ap()
        self.dbg_out = {}
        self.rr = {}

    def sb(self, name, shape, dt):
        return self.es.enter_context(self.nc.sbuf_tensor("sb_" + name, shape, dt))

    def mm(self, out, lhsT, rhs, start, stop, r, w, skip=False):
        if skip:
            return self.S.op('pe', lambda e: e.matmul(out, lhsT=lhsT, rhs=rhs, start=start, stop=stop, skip_group_check=True), r, w)
        return self.S.op('pe', lambda e: e.matmul(out, lhsT=lhsT, rhs=rhs, start=start, stop=stop), r, w)

    def tr(self, out, in_, r, w):
        ident = self.identb
        return self.S.op('pe', lambda e: e.transpose(out, in_, ident), list(r) + ['const'], w)

    def act(self, out, in_, func, r, w, nodrain=False, **kw):
        return self.S.op('act', lambda e: e.activation(out=out, in_=in_, func=func, **kw), r, w, nodrain=nodrain)

    def ts(self, eng, out, in0, s1, s2, op0, op1, r, w):
        def f(e):
            if s2 is None:
                return e.tensor_scalar(out=out, in0=in0, scalar1=s1, scalar2=None, op0=op0)
            return e.tensor_scalar(out=out, in0=in0, scalar1=s1, scalar2=s2, op0=op0, op1=op1)
        return self.S.op(eng, f, r, w)

    def tt(self, eng, out, in0, in1, op, r, w):
        return self.S.op(eng, lambda e: e.tensor_tensor(out=out, in0=in0, in1=in1, op=op), r, w)

    def stt(self, eng, out, in0, scalar, in1, op0, op1, r, w):
        return self.S.op(eng, lambda e: e.scalar_tensor_tensor(out=out, in0=in0, scalar=scalar, in1=in1, op0=op0, op1=op1), r, w)

    def cp(self, eng, out, in_, r, w):
        if eng == 'act':
            return self.act(out, in_, AF.Copy, r, w)
        return self.S.op(eng, lambda e: e.tensor_copy(out=out, in_=in_), r, w)

    def recip(self, out, in_, r, w):
        return self.S.op('dve', lambda e: e.reciprocal(out=out, in_=in_), r, w)

    def memset(self, eng, ap, val, w):
        return self.S.op(eng, lambda e: e.memset(ap, val), (), w)

    def DMA(self, out, in_, r=(), w=(), q='sp'):
        return self.S.op(q, lambda e: e.dma_start(out=out, in_=in_), r, w, dma=True)

    def rot(self, pool):
        lst = self.pools[pool]
        i = self.rr.get(pool, 0)
        self.rr[pool] = i + 1
        return lst[i % len(lst)]

    def dbg(self, name, ap, shape, dt, keys):
        if name not in self.debug:
            return
        t = self.nc.dram_tensor("dbg_" + name, list(shape), dt, kind="ExternalOutput").ap()
        self.dbg_out[name] = t
        self.DMA(t, ap, r=keys)

    def load_w(self, dram2d, kc, n, scale=None, dst_fn=None, dstkey=None, mulc=None, rows=128):
        assert kc * n <= 2048
        stg, skey = self.rot('stage')
        sv = stg[0:rows, 0:kc * n]
        src = dram2d.rearrange("(k p) c -> p k c", p=rows)
        self.DMA(sv.rearrange("p (k c) -> p k c", k=kc), src, w=[skey])
        if dst_fn is None:
            wb, wkey = self.rot('wb')
            dv = wb[0:rows, 0:kc * n]
            dst_fn = lambda k: dv[:, k * n:(k + 1) * n]
        else:
            dv, wkey = None, dstkey
        whole = dv is not None
        if whole:
            sv3 = sv.rearrange("p (k c) -> p k c", k=kc)
            dv3 = dv.rearrange("p (k c) -> p k c", k=kc)
        if scale is not None:
            sc_t, sc_off, sc_key = scale
            if whole:
                scb = sc_t[0:rows, sc_off:sc_off + kc].rearrange("p (k c) -> p k c", c=1).broadcast_to([rows, kc, n])
                if mulc is None:
                    self.tt('dve', dv3, sv3, scb, ALU.mult, [skey, sc_key], [wkey])
                else:
                    self.stt('dve', dv3, sv3, float(mulc), scb, ALU.mult, ALU.mult, [skey, sc_key], [wkey])
            else:
                for k in range(kc):
                    if mulc is None:
                        self.ts('dve', dst_fn(k), sv[:, k * n:(k + 1) * n], sc_t[0:rows, sc_off + k:sc_off + k + 1], None, ALU.mult, None, [skey, sc_key], [wkey])
                    else:
                        self.ts('dve', dst_fn(k), sv[:, k * n:(k + 1) * n], sc_t[0:rows, sc_off + k:sc_off + k + 1], float(mulc), ALU.mult, ALU.mult, [skey, sc_key], [wkey])
        elif mulc is not None:
            if whole:
                self.act(dv, sv, AF.Copy, [skey], [wkey], scale=float(mulc))
            else:
                for k in range(kc):
                    self.act(dst_fn(k), sv[:, k * n:(k + 1) * n], AF.Copy, [skey], [wkey], scale=float(mulc))
        else:
            if whole:
                self.cp('act', dv, sv, [skey], [wkey])
            else:
                for k in range(kc):
                    self.cp('act', dst_fn(k), sv[:, k * n:(k + 1) * n], [skey], [wkey])
        return dv, wkey

    def proj_fm(self, wfn, wkey, ncol, src, srckey, kc, epi, tchunks=range(NCH), srclen=S_LEN):
        for T in tchunks:
            ps, pkey = self.rot('psA')
            for k in range(kc):
                self.mm(ps[0:ncol, 0:512], wfn(k), src[:, k * srclen + T * 512:k * srclen + (T + 1) * 512], k == 0, k == kc - 1,
                        [wkey, (srckey, T)], [pkey])
            epi(ps, pkey, T)

    def proj_tm(self, wfn, wkey, ncol, src, srckey, kc, epi, tiles=range(NT), srclen=S_LEN):
        for i in tiles:
            ps, pkey = self.rot('psA')
            for k in range(kc):
                self.mm(ps[:, 0:ncol], src[:, k * srclen + i * 128:k * srclen + (i + 1) * 128], wfn(k), k == 0, k == kc - 1,
                        [wkey, (srckey, i // 4)], [pkey])
            epi(ps, pkey, i)

    def norm_transpose(self, xt, xkey, dstT, dkey, dlen, col0):
        junk, jkey = self.rot('tmpb1k')
        ss, sskey = self.rot('small')
        self.act(junk[:, 0:1024], xt, AF.Square, [xkey], [jkey, sskey], accum_out=ss[:, 0:1])
        self.S.op('act', lambda e: e.drain(), [sskey], [sskey])
        self.act(ss[:, 1:2], ss[:, 0:1], AF.Sqrt, [sskey, 'epsb'], [sskey], scale=1.0 / D, bias=self.epsb[:, 0:1])
        self.recip(ss[:, 2:3], ss[:, 1:2], [sskey], [sskey])
        xn, xnkey = self.rot('tmpb1k')
        self.ts('dve', xn[:, 0:1024], xt, ss[:, 2:3], None, ALU.mult, None, [xkey, sskey], [xnkey])
        pt, ptkey = self.rot('psT')
        for k in range(KC):
            self.tr(pt[:, k * 128:(k + 1) * 128], xn[:, k * 128:(k + 1) * 128], [xnkey], [ptkey])
        dv = dstT.rearrange("p (k c) -> p k c", k=KC)[:, :, col0:col0 + 128]
        self.act(dv, pt[:, 0:1024].rearrange("p (k c) -> p k c", k=KC), AF.Copy, [ptkey], [dkey])

    def silu2(self, ps, pkey, dst, dkey, n):
        th, thkey = self.rot('tmpf')
        self.act(th[:, 0:n], ps[:, 0:n], AF.Tanh, [pkey], [thkey], scale=0.5)
        self.stt('dve', dst, th[:, 0:n], 1.0, ps[:, 0:n], ALU.add, ALU.mult, [thkey, pkey], [dkey])

    def gelu2(self, ps, pkey, dst, dkey, n):
        sq, sqkey = self.rot('tmpf')
        self.act(sq[:, 0:n], ps[:, 0:n], AF.Square, [pkey], [sqkey])
        self.ts('dve', sq[:, 0:n], sq[:, 0:n], GC2, GC1, ALU.mult, ALU.add, [sqkey], [sqkey])
        self.tt('dve', sq[:, 0:n], sq[:, 0:n], ps[:, 0:n], ALU.mult, [sqkey, pkey], [sqkey])
        self.act(sq[:, 0:n], sq[:, 0:n], AF.Tanh, [sqkey], [sqkey])
        self.stt('dve', dst, sq[:, 0:n], 1.0, ps[:, 0:n], ALU.add, ALU.mult, [sqkey, pkey], [dkey])

    def finish_chunk_sub(self, T, yt, ykey, ystride, wcs, ycol0, wzfn, wzkey):
        for wi, wc in enumerate(wcs):
            zg, zgkey = self.rot('tmpb')

            def epi(ps, pkey, T_, zg=zg, zgkey=zgkey):
                self.silu2(ps, pkey, zg[:, 0:512], zgkey, 512)
            self.proj_fm(lambda k, wi=wi: wzfn(k, wi), wzkey, 128, self.hT, 'hT', KC, epi, tchunks=[T])
            pt, ptkey = self.rot('psT')
            for qt in range(4):
                c0 = qt * ystride + ycol0 + wi * 128
                self.tr(pt[:, qt * 128:(qt + 1) * 128], yt[:, c0:c0 + 128], [ykey], [ptkey])
            dst = self.yT[:, wc * S_LEN + T * 512:wc * S_LEN + (T + 1) * 512]
            self.tt('dve', dst, pt[:, 0:512], zg[:, 0:512], ALU.mult, [ptkey, zgkey], [('yT', T)])

    def pv_evac(self, pv, pvkey, nq, hd, ydst, ykey, stride=None):
        stride = stride or (hd + 1)
        rz, rzkey = self.rot('small')
        pv3 = pv[:, 0:nq * stride].rearrange("p (q c) -> p q c", c=stride)
        rz3 = lambda a: rz[:, a:a + nq].rearrange("p (q c) -> p q c", c=1)
        self.ts('dve', rz3(0), pv3[:, :, hd:hd + 1], 1e-30, None, ALU.max, None, [pvkey], [rzkey])
        self.recip(rz[:, 8:8 + nq], rz[:, 0:nq], [rzkey], [rzkey])
        self.tt('dve', ydst, pv3[:, :, 0:hd], rz3(8).broadcast_to([128, nq, hd]), ALU.mult, [pvkey, rzkey], [ykey])

    def branch_end(self, l, n, first):
        dr = self.dr
        for eb in range(4):
            wbr, wbrkey = self.load_w(dr['wbr'][l, n, :, eb * 256:(eb + 1) * 256], 4, 256, mulc=0.5)
            wg, wgkey = self.load_w(dr['wgate'][l, :, n * D + eb * 256:n * D + (eb + 1) * 256], KC, 256, scale=(self.gpre, l * KC, 'gvec'))
            for ec in range(2):
                e_idx = eb * 2 + ec
                for T in range(NCH):
                    pu, pukey = self.rot('psA')
                    for k in range(4):
                        self.mm(pu[:, 0:512], wbr[:, k * 256 + ec * 128:k * 256 + (ec + 1) * 128], self.yT[:, k * S_LEN + T * 512:k * S_LEN + (T + 1) * 512],
                                k == 0, k == 3, [wbrkey, ('yT', T)], [pukey])
                    pg, pgkey = self.rot('psA')
                    for k in range(KC):
                        self.mm(pg[:, 0:512], wg[:, k * 256 + ec * 128:k * 256 + (ec + 1) * 128], self.hT[:, k * S_LEN + T * 512:k * S_LEN + (T + 1) * 512],
                                k == 0, k == KC - 1, [wgkey, ('hT', T)], [pgkey])
                    th, thkey = self.rot('tmpf')
                    self.act(th[:, 0:512], pg[:, 0:512], AF.Tanh, [pgkey], [thkey], scale=0.5)
                    mdst = self.merged[:, e_idx * S_LEN + T * 512:e_idx * S_LEN + (T + 1) * 512]
                    mkey = ('merged', e_idx, T)
                    if first:
                        self.stt('dve', mdst, th[:, 0:512], 1.0, pu[:, 0:512], ALU.add, ALU.mult, [thkey, pukey], [mkey])
                    else:
                        self.stt('dve', th[:, 0:512], th[:, 0:512], 1.0, pu[:, 0:512], ALU.add, ALU.mult, [thkey, pukey], [thkey])
                        self.tt('pool', mdst, mdst, th[:, 0:512], ALU.add, [thkey, mkey], [mkey])

    def build(self):
        nc, S, dr = self.nc, self.S, self.dr
        with ExitStack() as es:
            self.es = es
            sb = self.sb
            self.hT = sb("hT", [128, KC * S_LEN], BF16)
            self.merged = sb("merged", [128, KC * S_LEN], BF16)
            self.yT = sb("yT", [128, 4 * S_LEN], BF16)
            self.cb = sb("cb", [128, 384], BF16)
            self.identb = self.cb[:, 0:128]
            self.causalneg = self.cb[:, 128:256]
            self.anticausalneg = self.cb[:, 256:384]
            self.cf = sb("cf", [128, 512], F32)
            self.identf = self.cf[:, 0:128]
            self.triu = self.cf[:, 128:256]
            self.onesf = self.cf[:, 256:384]
            self.e64 = self.cf[:, 384:512]
            self.ovl = sb("ovl", [128, 33], BF16)
            self.gpre = sb("gpre", [128, 2 * KC], F32)
            self.gmem = sb("gmem", [128, 2 * KC], F32)
            self.fbias = sb("fbias", [128, 8], F32)
            self.epsb = sb("epsb", [128, 2], F32)
            self.smallproj = sb("smallproj", [128, NT * 32], F32)
            self.cvecs = sb("cvecs", [128, 4 * 128], F32)
            self.arena = sb("arena", [128, ARENA_EL], BF16)
            stage = [(sb(f"stage{i}", [128, 2048], F32), f"stage{i}") for i in range(2)]
            wbs = [(sb(f"wb{i}", [128, 2048], BF16), f"wb{i}") for i in range(3)]
            xts = [(sb(f"xt{i}", [128, 1024], F32), f"xt{i}") for i in range(int(os.environ.get("K_XT", "2")))]
            tmpf = [(sb(f"tmpf{i}", [128, 512], F32), f"tmpf{i}") for i in range(3)]
            tmpb = [(sb(f"tmpb{i}", [128, 512], BF16), f"tmpb{i}") for i in range(6)]
            tmpb1k = [(sb(f"tmpb1k{i}", [128, 1024], BF16), f"tmpb1k{i}") for i in range(2)]
            small = [(sb(f"small{i}", [128, 32], F32), f"small{i}") for i in range(8)]
            biasp = [(sb(f"biasp{i}", [128, 128], F32), f"biasp{i}") for i in range(2)]
            psA = [(es.enter_context(nc.psum_tensor(f"psA{i}", [128, 512], F32)), f"psA{i}") for i in range(4)]
            psB = [(es.enter_context(nc.psum_tensor(f"psB{i}", [128, 512], F32)), f"psB{i}") for i in range(2)]
            psT = [(es.enter_context(nc.psum_tensor(f"psT{i}", [128, 1024], BF16)), f"psT{i}") for i in range(2)]
            self.pools = dict(stage=stage, wb=wbs, xt=xts, tmpf=tmpf, tmpb=tmpb, tmpb1k=tmpb1k, small=small, psA=psA, psB=psB, psT=psT, biasp=biasp)
            sems = {e: es.enter_context(nc.semaphore("s_" + e)) for e in ENGS}
            dsems = {(e, i): es.enter_context(nc.semaphore(f"d_{e}_{i}")) for e in ('sp',) for i in range(N_DMA_SEMS)}
            block = es.enter_context(nc.Block())

            self.memset('dve', self.epsb[:, 0:1], EPS, ['epsb'])
            self.memset('dve', self.epsb[:, 1:2], 4.0 * EPS, ['epsb'])
            for nm, t in [('cb', self.cb), ('cf', self.cf), ('ovl', self.ovl), ('gpre', self.gpre), ('gmem', self.gmem)]:
                self.DMA(t[:], dr[nm], w=['const' if nm not in ('gpre', 'gmem') else 'gvec'])

            for i in range(NT):
                xt, xkey = self.rot('xt')
                self.DMA(xt[:], dr['x'][i * 128:(i + 1) * 128, :], w=[xkey])
                self.norm_transpose(xt[:], xkey, self.hT[:], ('hT', i // 4), S_LEN, i * 128)
            self.dbg('hT0', self.hT[:], [128, KC * S_LEN], BF16, [('hT', T) for T in range(4)])
            self.stop = int(os.environ.get("K_STOP", "99"))

            for l in range(self.nlayers if self.stop > 0 else 0):
                first = True
                for br in self.branches:
                    S.barrier()
                    getattr(self, 'branch_' + br)(l)
                    self.dbg(f'yT_{br}{l}', self.yT[:], [128, 4 * S_LEN], BF16, [('yT', T) for T in range(4)])
                    if self.stop == 1:
                        break
                    self.branch_end(l, 'ABCM'.index(br), first)
                    first = False
                    if self.stop == 2:
                        break
                if self.stop < 3:
                    break
                self.dbg(f'merged{l}', self.merged[:], [128, KC * S_LEN], BF16, [('merged', e, T) for e in range(8) for T in range(4)])
                S.barrier()
                self.final_phase(l)

            S.finish('sp', [o['id'] for o in S.ops if o['dma']])
            S.emit(block, sems, dsems)
        return nc

    def final_phase(self, l):
        dr = self.dr
        last = (l == self.nlayers - 1)
        ar = self.arena
        wo = ar[:, 0:KC * 1024]
        gpost = ar[:, KC * 1024:KC * 1024 + 2048].bitcast(F32)
        self.DMA(gpost, dr['gpost'][l], w=['gpost'])
        wo3 = wo.rearrange("p (k c) -> p k c", k=KC)
        for cbk in range(4):
            self.load_w(dr['wo'][l, :, cbk * 256:(cbk + 1) * 256], KC, 256, dst_fn=lambda k, cbk=cbk: wo[:, k * 1024 + cbk * 256:k * 1024 + (cbk + 1) * 256], dstkey='wo')
        src_x = dr['x'] if l == 0 else dr['out']
        fstop = int(os.environ.get("K_FSTOP", "99"))
        for i in range(NT if fstop > 0 else 0):
            if fstop in (1, 2, 3) and i > 0:
                break
            if i >= int(os.environ.get("K_FTILES", "99")):
                break
            T = i // 4
            xt, xkey = self.rot('xt')
            self.DMA(xt[:], src_x[i * 128:(i + 1) * 128, :], r=[('outdram', i)] if l > 0 else [], w=[xkey])
            pss = []
            for hf in range(2):
                ps, pkey = self.rot('psA')
                for k in range(KC):
                    self.mm(ps[:, 0:512], self.merged[:, k * S_LEN + i * 128:k * S_LEN + (i + 1) * 128], wo[:, k * 1024 + hf * 512:k * 1024 + (hf + 1) * 512],
                            k == 0, k == KC - 1, ['wo', ('merged', k, T)], [pkey])
                pss.append((ps, pkey))
            if fstop == 1:
                break
            ss, sskey = self.rot('small')
            junk, jkey = self.rot('tmpb')
            for hf in range(2):
                self.act(junk[:, 0:512], pss[hf][0][:, 0:512], AF.Square, [pss[hf][1]], [jkey, sskey], accum_out=ss[:, hf:hf + 1])
            self.tt('dve', ss[:, 2:3], ss[:, 0:1], ss[:, 1:2], ALU.add, [sskey], [sskey])
            self.act(ss[:, 3:4], ss[:, 2:3], AF.Sqrt, [sskey, 'epsb'], [sskey], scale=0.25 / D, bias=self.epsb[:, 0:1])
            self.recip(ss[:, 5:6], ss[:, 3:4], [sskey], [sskey])
            self.ts('dve', ss[:, 4:5], ss[:, 5:6], 0.5, None, ALU.mult, None, [sskey], [sskey])
            if fstop == 2:
                break
            for hf in range(2):
                dl, dlkey = self.rot('tmpf')
                self.stt('dve', dl[:, 0:512], pss[hf][0][:, 0:512], ss[:, 4:5], gpost[:, hf * 512:(hf + 1) * 512], ALU.mult, ALU.mult, [pss[hf][1], sskey, 'gpost'], [dlkey])
                self.tt('pool', xt[:, hf * 512:(hf + 1) * 512], xt[:, hf * 512:(hf + 1) * 512], dl[:, 0:512], ALU.add, [dlkey, xkey], [xkey])
            self.DMA(dr['out'][i * 128:(i + 1) * 128, :], xt[:], r=[xkey], w=[('outdram', i)])
            if not last:
                self.norm_transpose(xt[:], xkey, self.hT[:], ('hT', T), S_LEN, i * 128)

    def branch_M(self, l):
        dr = self.dr
        ar = self.arena
        o = 0
        qmT = ar[:, o:o + 4 * S_LEN]; o += 4 * S_LEN
        memT = ar[:, o:o + KC * MEM]; o += KC * MEM
        mkT = ar[:, o:o + 4 * MEM]; o += 4 * MEM
        MVW = 130
        mv = ar[:, o:o + 2 * 4 * MVW]; o += 2 * 4 * MVW
        ytm = [ar[:, o + i * 2048:o + (i + 1) * 2048] for i in range(2)]; o += 4096
        for mt in range(2):
            xt, xkey = self.rot('xt')
            self.DMA(xt[:], dr['mem'][mt * 128:(mt + 1) * 128, :], w=[xkey])
            self.norm_transpose(xt[:], xkey, memT, 'memT', MEM, mt * 128)
        self.memset('pool', mv.rearrange("p (a c) -> p a c", c=MVW)[:, :, 128:129], 1.0, ['mv'])
        gm = (self.gmem, l * KC, 'gvec')
        gp = (self.gpre, l * KC, 'gvec')
        for cbk in range(4):
            wv, wkey = self.load_w(dr['wmem'][l, :, cbk * 256:(cbk + 1) * 256], KC, 256, scale=gm)
            if cbk < 2:
                for hh in range(2):
                    h = cbk * 2 + hh
                    ps, pkey = self.rot('psA')
                    for k in range(KC):
                        self.mm(ps[:, 0:MEM], wv[:, k * 256 + hh * 128:k * 256 + (hh + 1) * 128], memT[:, k * MEM:(k + 1) * MEM], k == 0, k == KC - 1, [wkey, 'memT'], [pkey])
                    self.cp('act', mkT[:, h * MEM:(h + 1) * MEM], ps[:, 0:MEM], [pkey], ['mkT'])
            else:
                for mt in range(2):
                    ps, pkey = self.rot('psA')
                    for k in range(KC):
                        self.mm(ps[:, 0:256], memT[:, k * MEM + mt * 128:k * MEM + (mt + 1) * 128], wv[:, k * 256:(k + 1) * 256], k == 0, k == KC - 1, [wkey, 'memT'], [pkey])
                    h0 = (cbk - 2) * 2
                    dst = mv[:, (mt * 4 + h0) * MVW:(mt * 4 + h0 + 2) * MVW].rearrange("p (h c) -> p h c", c=MVW)[:, :, 0:128]
                    self.cp('act', dst, ps[:, 0:256].rearrange("p (h c) -> p h c", c=128), [pkey], ['mv'])
        for cbk in range(2):
            wv, wkey = self.load_w(dr['wM'][l, :, cbk * 256:(cbk + 1) * 256], KC, 256, scale=gp)
            for hh in range(2):
                h = cbk * 2 + hh

                def epi(ps, pkey, T, h=h):
                    self.cp('act', qmT[:, h * S_LEN + T * 512:h * S_LEN + (T + 1) * 512], ps[:, 0:512], [pkey], [('qmT', T)])
                self.proj_fm(lambda k, wv=wv, hh=hh: wv[:, k * 256 + hh * 128:k * 256 + (hh + 1) * 128], wkey, 128, self.hT, 'hT', KC, epi)
        sc = 128.0 ** -0.5
        for T in range(NCH):
            yt = ytm[T % 2]
            ykey = f'ytm{T % 2}'
            for h in range(4):
                pts = []
                for mt in range(2):
                    ps, pkey = self.rot('psA')
                    self.mm(ps[:, 0:512], mkT[:, h * MEM + mt * 128:h * MEM + (mt + 1) * 128], qmT[:, h * S_LEN + T * 512:h * S_LEN + (T + 1) * 512], True, True,
                            ['mkT', ('qmT', T)], [pkey])
                    pt, ptkey = self.rot('tmpb')
                    self.act(pt[:, 0:512], ps[:, 0:512], AF.Exp, [pkey], [ptkey], scale=sc, nodrain=True)
                    pts.append((pt, ptkey))
                for q2 in range(2):
                    pv, pvkey = self.rot('psB')
                    for qq in range(2):
                        qt = q2 * 2 + qq
                        for mt in range(2):
                            self.mm(pv[:, qq * 129:(qq + 1) * 129], pts[mt][0][:, qt * 128:(qt + 1) * 128], mv[:, (mt * 4 + h) * MVW:(mt * 4 + h) * MVW + 129],
                                    mt == 0, mt == 1, [pts[mt][1], 'mv'], [pvkey])
                    ydst = yt[:, q2 * 1024:(q2 + 1) * 1024].rearrange("p (q c) -> p q c", c=512)[:, :, h * 128:(h + 1) * 128]
                    self.pv_evac(pv, pvkey, 2, 128, ydst, ykey)
            for cbk in range(2):
                wzv, wzkey = self.load_w(dr['wM'][l, :, 512 + cbk * 256:512 + (cbk + 1) * 256], KC, 256, scale=gp)
                self.finish_chunk_sub(T, yt, ykey, 512, [cbk * 2, cbk * 2 + 1], cbk * 256,
                                      lambda k, wi, wzv=wzv: wzv[:, k * 256 + wi * 128:k * 256 + (wi + 1) * 128], wzkey)

    def branch_C(self, l):
        dr = self.dr
        ar = self.arena
        o = 0
        qT = ar[:, o:o + 4 * S_LEN]; o += 4 * S_LEN
        kT = ar[:, o:o + 4 * S_LEN]; o += 4 * S_LEN
        vC = ar[:, o:o + NT * 8 * 65]; o += NT * 8 * 65
        ytm = [ar[:, o + i * 2048:o + (i + 1) * 2048] for i in range(2)]; o += 4096
        assert o <= ARENA_EL
        gp = (self.gpre, l * KC, 'gvec')
        cv = self.cvecs
        lg = cv[:, 0:128]
        pre = cv[:, 128:256]
        cpos = cv[:, 256:384]
        cmid = cv[:, 384:512]
        wv, wkey = self.load_w(dr['wsm'][l], KC, 32, scale=gp)

        def epi_s(ps, pkey, i):
            self.cp('act', self.smallproj[:, i * 32:(i + 1) * 32], ps[:, 0:32], [pkey], ['smallproj'])
        self.proj_tm(lambda k: wv[:, k * 32:(k + 1) * 32], wkey, 32, self.hT, 'hT', KC, epi_s)
        self.DMA(self.fbias[:], dr['fbias'][l], w=['fbias'])
        sp3 = self.smallproj[:].rearrange("p (i c) -> p i c", c=32)
        lg3 = lg.rearrange("p (i h) -> p i h", h=8)
        self.tt('dve', lg3, sp3[:, :, 0:8], self.fbias[:, 0:8].rearrange("p (o c) -> p o c", o=1).broadcast_to([128, NT, 8]), ALU.add, ['smallproj', 'fbias'], ['lg'])
        self.act(lg, lg, AF.Exp, ['lg'], ['lg'], scale=-1.0)
        self.act(lg, lg, AF.Ln, ['lg'], ['lg'], bias=1.0)
        self.memset('dve', pre[:, 0:8], 0.0, ['pre'])
        for i in range(1, NT):
            self.tt('dve', pre[:, i * 8:(i + 1) * 8], pre[:, (i - 1) * 8:i * 8], lg[:, (i - 1) * 8:i * 8], ALU.add, ['lg', 'pre'], ['pre'])
        ps, pkey = self.rot('psA')
        for i in range(NT):
            self.mm(ps[:, i * 8:(i + 1) * 8], self.triu, lg[:, i * 8:(i + 1) * 8], True, False, ['const', 'lg'], [pkey])
            self.mm(ps[:, i * 8:(i + 1) * 8], self.onesf, pre[:, i * 8:(i + 1) * 8], False, True, ['const', 'pre'], [pkey])
        self.cp('dve', cpos, ps[:, 0:128], [pkey], ['cpos'])
        ps2, pkey2 = self.rot('psA')
        self.mm(ps2[:, 0:128], self.e64, cpos, True, True, ['const', 'cpos'], [pkey2])
        self.cp('dve', cmid, ps2[:, 0:128], [pkey2], ['cmid'])
        for which, dstT, nm in ((0, qT, 'cqT'), (1, kT, 'ckT')):
            for cbk in range(2):
                wv, wkey = self.load_w(dr['wC'][l, :, which * 512 + cbk * 256:which * 512 + (cbk + 1) * 256], KC, 256, scale=gp)
                for pp in range(2):
                    pr = cbk * 2 + pp

                    def epi(ps, pkey, T, pr=pr, dstT=dstT, nm=nm):
                        self.cp('act' if T % 2 == 0 else 'dve', dstT[:, pr * S_LEN + T * 512:pr * S_LEN + (T + 1) * 512], ps[:, 0:512], [pkey], [(nm, T)])
                    self.proj_fm(lambda k, wv=wv, pp=pp: wv[:, k * 256 + pp * 128:k * 256 + (pp + 1) * 128], wkey, 128, self.hT, 'hT', KC, epi)
        self.memset('pool', vC.rearrange("p (a c) -> p a c", c=65)[:, :, 64:65], 1.0, ['vC'])
        for cbk in range(2):
            wv, wkey = self.load_w(dr['wC'][l, :, 1024 + cbk * 256:1024 + (cbk + 1) * 256], KC, 256, scale=gp)

            def epi_v(ps, pkey, i, cbk=cbk):
                dst = vC[:, (i * 8 + cbk * 4) * 65:(i * 8 + cbk * 4 + 4) * 65].rearrange("p (h c) -> p h c", c=65)[:, :, 0:64]
                self.cp('act' if i % 2 == 0 else 'dve', dst, ps[:, 0:256].rearrange("p (h c) -> p h c", c=64), [pkey], ['vC'])
            self.proj_tm(lambda k, wv=wv: wv[:, k * 256:(k + 1) * 256], wkey, 256, self.hT, 'hT', KC, epi_v)
        cpos_hj = cpos.rearrange("p (j h) -> p h j", h=8)
        for i in range(NT):
            T = i // 4
            qt = i % 4
            yt = ytm[T % 2]
            ykey = f'ytm{T % 2}'
            bi, bikey = self.rot('biasp')
            bi3 = bi[:, 0:128].rearrange("p (h j) -> p h j", j=16)
            self.tt('dve', bi3[:, :, 0:i + 1], cpos_hj[:, :, 0:i + 1],
                    cmid[:, i * 8:(i + 1) * 8].rearrange("p (h c) -> p h c", c=1).broadcast_to([128, 8, i + 1]), ALU.subtract, ['cpos', 'cmid'], [bikey])
            pvs = [self.rot('psB') for _ in range(2)]
            for h in range(8):
                pr, half = h // 2, h % 2
                rows = slice(half * 64, half * 64 + 64)
                pv, pvkey = pvs[h // 4]
                hh = h % 4
                for jb in range(0, i + 1, 4):
                    js = list(range(jb, min(jb + 4, i + 1)))
                    ps, pkey = self.rot('psA')
                    for jj, j in enumerate(js):
                        self.mm(ps[:, jj * 128:(jj + 1) * 128], kT[rows, pr * S_LEN + j * 128:pr * S_LEN + (j + 1) * 128],
                                qT[rows, pr * S_LEN + i * 128:pr * S_LEN + (i + 1) * 128], True, j != i, [('ckT', j // 4), ('cqT', T)], [pkey])
                        if j == i:
                            self.mm(ps[:, jj * 128:(jj + 1) * 128], self.identb, self.causalneg, False, True, ['const'], [pkey])
                    pt, ptkey = self.rot('tmpb')
                    for jj, j in enumerate(js):
                        self.act(pt[:, jj * 128:(jj + 1) * 128], ps[:, jj * 128:(jj + 1) * 128], AF.Exp, [pkey, bikey], [ptkey], nodrain=True, scale=0.125,
                                 bias=bi[:, h * 16 + j:h * 16 + j + 1])
                    for jj, j in enumerate(js):
                        self.mm(pv[:, hh * 65:(hh + 1) * 65], pt[:, jj * 128:(jj + 1) * 128], vC[:, (j * 8 + h) * 65:(j * 8 + h + 1) * 65],
                                j == 0, j == i, [ptkey, 'vC'], [pvkey])
            for hb in range(2):
                ydst = yt[:, qt * 512 + hb * 256:qt * 512 + (hb + 1) * 256].rearrange("p (h c) -> p h c", c=64)
                self.pv_evac(pvs[hb][0], pvs[hb][1], 4, 64, ydst, ykey)
            if qt == 3:
                for cbk in range(2):
                    wzv, wzkey = self.load_w(dr['wC'][l, :, 1536 + cbk * 256:1536 + (cbk + 1) * 256], KC, 256, scale=gp)
                    self.finish_chunk_sub(T, yt, ykey, 512, [cbk * 2, cbk * 2 + 1], cbk * 256,
                                          lambda k, wi, wzv=wzv: wzv[:, k * 256 + wi * 128:k * 256 + (wi + 1) * 128], wzkey)

    def branch_A(self, l):
        dr = self.dr
        ar = self.arena
        o = 0
        guzT = ar[:, o:o + 4 * S_LEN]; o += 4 * S_LEN
        wav = ar[:, o:o + KC * 512]; o += KC * 512
        wsT = ar[:, o:o + 512]; o += 512
        lng = ar[:, o:o + 1024].bitcast(F32); o += 1024
        lnb = ar[:, o:o + 1024].bitcast(F32); o += 1024
        bsrow = ar[0:1, o:o + 1024].bitcast(F32); o += 1024
        gp = (self.gpre, l * KC, 'gvec')
        for cbk in range(2):
            wv, wkey = self.load_w(dr['wA'][l, :, cbk * 256:(cbk + 1) * 256], KC, 256, scale=gp)
            for pp in range(2):
                wc = cbk * 2 + pp

                def epi(ps, pkey, T, wc=wc):
                    self.gelu2(ps, pkey, guzT[:, wc * S_LEN + T * 512:wc * S_LEN + (T + 1) * 512], ('guz', wc, T), 512)
                self.proj_fm(lambda k, wv=wv, pp=pp: wv[:, k * 256 + pp * 128:k * 256 + (pp + 1) * 128], wkey, 128, self.hT, 'hT', KC, epi)
        for cbk in range(2):
            wv, wkey = self.load_w(dr['wA'][l, :, 512 + cbk * 256:512 + (cbk + 1) * 256], KC, 256, scale=gp)
            for pp in range(2):
                wc = cbk * 2 + pp

                def epi(ps, pkey, T, wc=wc):
                    zg, zgkey = self.rot('tmpb')
                    self.silu2(ps, pkey, zg[:, 0:512], zgkey, 512)
                    d = guzT[:, wc * S_LEN + T * 512:wc * S_LEN + (T + 1) * 512]
                    self.tt('pool', d, d, zg[:, 0:512], ALU.mult, [zgkey, ('guz', wc, T)], [('guz', wc, T)])
                self.proj_fm(lambda k, wv=wv, pp=pp: wv[:, k * 256 + pp * 128:k * 256 + (pp + 1) * 128], wkey, 128, self.hT, 'hT', KC, epi)
        for cbk in range(2):
            self.load_w(dr['wA'][l, :, 1024 + cbk * 256:1024 + (cbk + 1) * 256], KC, 256, scale=gp,
                        dst_fn=lambda k, cbk=cbk: wav[:, k * 512 + cbk * 256:k * 512 + (cbk + 1) * 256], dstkey='wav')
        stg, skey = self.rot('stage')
        self.DMA(stg[:, 0:512], dr['awsT'][l].rearrange("s g t -> s (g t)"), w=[skey])
        self.tt('pool', wsT.rearrange("p (g t) -> p g t", g=4), stg[:, 0:512].rearrange("p (g t) -> p g t", g=4),
                self.triu.rearrange("p (o t) -> p o t", o=1).broadcast_to([128, 4, 128]), ALU.mult, [skey, 'const'], ['wsT'])
        self.DMA(lng, dr['lng'][l], w=['lng'])
        self.DMA(lnb, dr['lnb'][l], w=['lnb'])
        self.DMA(bsrow, dr['abs'][l], w=['bsrow'])
        gst = ar[:, o:o + NT * 512]; o += NT * 512
        assert o <= ARENA_EL
        st = self.cvecs
        ssum, ssq, mean, m2, var, rstd = (st[:, a * 16:(a + 1) * 16] for a in range(6))
        for c in range(NT):
            T = c // 4
            ps, pkey = self.rot('psA')
            for k in range(KC):
                self.mm(ps[:, 0:512], self.hT[:, k * S_LEN + c * 128:k * S_LEN + (c + 1) * 128], wav[:, k * 512:(k + 1) * 512], k == 0, k == KC - 1, ['wav', ('hT', T)], [pkey])
            g2, g2key = self.rot('tmpf')
            self.gelu2(ps, pkey, g2[:, 0:512], g2key, 512)
            self.S.op('dve', lambda e, c=c, g2=g2: e.tensor_reduce(out=ssum[:, c:c + 1], in_=g2[:, 0:512], axis=AX.X, op=ALU.add), [g2key], ['astat'])
            junk, jkey = self.rot('tmpb')
            self.act(junk[:, 0:512], g2[:, 0:512], AF.Square, [g2key], [jkey, 'astat'], accum_out=ssq[:, c:c + 1])
            self.cp('pool', gst[:, c * 512:(c + 1) * 512], g2[:, 0:512], [g2key], [('gst', c)])
        self.ts('dve', mean, ssum, 1.0 / 512, None, ALU.mult, None, ['astat'], ['astat'])
        self.tt('dve', m2, mean, mean, ALU.mult, ['astat'], ['astat'])
        self.stt('dve', var, ssq, 1.0 / 512, m2, ALU.mult, ALU.subtract, ['astat'], ['astat'])
        self.act(var, var, AF.Sqrt, ['astat', 'epsb'], ['astat'], bias=self.epsb[:, 1:2])
        self.recip(rstd, var, ['astat'], ['astat'])
        for c in range(NT):
            T = c // 4
            g2, g2key = self.rot('tmpf')
            self.ts('dve', g2[:, 0:512], gst[:, c * 512:(c + 1) * 512], mean[:, c:c + 1], rstd[:, c:c + 1], ALU.subtract, ALU.mult, [('gst', c), 'astat'], [g2key])
            self.tt('pool', g2[:, 0:512], g2[:, 0:512], lng, ALU.mult, [g2key, 'lng'], [g2key])
            vln, vlkey = self.rot('tmpb')
            self.tt('dve', vln[:, 0:512], g2[:, 0:512], lnb, ALU.add, [g2key, 'lnb'], [vlkey])
            ps2, pkey2 = self.rot('psA')
            for g in range(4):
                self.mm(ps2[:, g * 128:(g + 1) * 128], vln[:, g * 128:(g + 1) * 128], wsT[:, g * 128:(g + 1) * 128], True, False, [vlkey, 'wsT'], [pkey2])
                self.mm(ps2[:, g * 128:(g + 1) * 128], self.onesf[0:1, 0:128], bsrow[0:1, g * 128:(g + 1) * 128], False, True, ['const', 'bsrow'], [pkey2])
            ydst = self.yT[:].rearrange("p (g t) -> p g t", g=4)[:, :, c * 128:(c + 1) * 128]
            gsrc = guzT.rearrange("p (g t) -> p g t", g=4)[:, :, c * 128:(c + 1) * 128]
            self.stt('dve', ydst, ps2[:, 0:512].rearrange("p (g t) -> p g t", g=4), 0.5, gsrc, ALU.mult, ALU.mult,
                     [pkey2] + [('guz', wc, T) for wc in range(4)], [('yT', T)])

    def branch_B(self, l):
        dr = self.dr
        ar = self.arena
        S = self.S
        gp = (self.gpre, l * KC, 'gvec')
        ALL5 = ('pe', 'act', 'dve', 'pool', 'sp')
        cosF = ar[:, 0:4096].bitcast(F32)
        sinF = ar[:, 4096:8192].bitcast(F32)
        qTa = ar[:, 8192:16384]
        ksTa = ar[:, 16384:18432]
        kwT = ar[:, 18432:20480]
        kcT = ar[:, 20480:22528]
        vcT = ar[:, 22528:24576]
        vs = ar[:, 24576:24576 + 1040]
        vw = ar[:, 25616:25616 + 1040]
        kcmpT = ar[:, 26656:26656 + 128]
        vcmpx = ar[:, 26784:26784 + 97]
        w2kb = ar[:, 26884:26884 + 64]
        w2vb = ar[:, 26948:26948 + 64]
        posb = ar[:, 27012:27012 + 64]
        w1kb = ar[:, 0:2048]
        w1vb = ar[:, 2048:4096]
        ytm = [ar[:, 0:1024], ar[:, 1024:2048]]
        ocmp = ar[:, 2048:4096].bitcast(F32)
        cmpneg = ar[:, 4096:6144]
        keepadd = ar[:, 6144:8192].bitcast(F32)
        impacc = ar[:, 20480:20736].bitcast(F32)
        imp2 = ar[:, 20736:20992].bitcast(F32)
        selneg = ar[:, 20992:21120]
        bgs = self.cvecs[:, 0:384]
        S.barrier()
        sp3 = self.smallproj[:].rearrange("p (i c) -> p i c", c=32)
        bgs3 = bgs.rearrange("p (i c) -> p i c", c=24)
        self.act(bgs3, sp3[:, :, 8:32], AF.Tanh, ['smallproj'], ['bgs'], scale=0.5)
        self.ts('dve', bgs, bgs, 0.5, 0.5, ALU.mult, ALU.add, ['bgs'], ['bgs'])
        for g in range(2):
            wB = dr['wB'][l, g]
            S.barrier(ALL5)
            self.DMA(cosF, dr['cosF'], w=['cosF'])
            self.DMA(sinF, dr['sinF'], w=['sinF'])
            self.DMA(ksTa[64:96, :], dr['onehot'], w=[('ksTa', T) for T in range(4)])
            self.memset('pool', vs.rearrange("p (a c) -> p a c", c=65)[:, :, 64:65], 1.0, ['vs'])
            self.memset('pool', vw.rearrange("p (a c) -> p a c", c=65)[:, :, 64:65], 1.0, ['vw'])
            rope_dsts = [((qTa, 0, 'qTa'), (qTa, 1, 'qTa')), ((qTa, 2, 'qTa'), (qTa, 3, 'qTa')),
                         ((ksTa, 0, 'ksTa'), (kwT, 0, 'kwT')), ((kcT, 0, 'kcT'), (vcT, 0, 'vcT'))]
            for ti, (dA, dB) in enumerate(rope_dsts):
                wv, wkey = self.load_w(wB[:, ti * 256:(ti + 1) * 256], KC, 256, scale=gp)
                for T in range(NCH):
                    pa, pakey = self.rot('psA')
                    for k in range(KC):
                        self.mm(pa[:, 0:512], wv[:, k * 256:k * 256 + 128], self.hT[:, k * S_LEN + T * 512:k * S_LEN + (T + 1) * 512], k == 0, k == KC - 1, [wkey, ('hT', T)], [pakey])
                    pb, pbkey = self.rot('psA')
                    for k in range(KC):
                        self.mm(pb[:, 0:512], wv[:, k * 256 + 128:k * 256 + 256], self.hT[:, k * S_LEN + T * 512:k * S_LEN + (T + 1) * 512], k == 0, k == KC - 1, [wkey, ('hT', T)], [pbkey])
                    t1, t1key = self.rot('tmpf')
                    t2, t2key = self.rot('tmpf')
                    cs = slice(T * 512, (T + 1) * 512)
                    novc = (ti == 3)
                    np_ = 64 if novc else 128
                    self.tt('dve', t1[0:np_, 0:512], pa[0:np_, 0:512], cosF[0:np_, cs], ALU.mult, [pakey, 'cosF'], [t1key])
                    self.tt('dve', t2[0:np_, 0:512], pb[0:np_, 0:512], sinF[0:np_, cs], ALU.mult, [pbkey, 'sinF'], [t2key])
                    (ta, ha, ka), (tb, hb, kb) = dA, dB
                    self.tt('pool', ta[0:64, ha * S_LEN + T * 512:ha * S_LEN + (T + 1) * 512], t1[0:64, 0:512], t2[0:64, 0:512], ALU.add, [t1key, t2key], [(ka, T)])
                    if novc:
                        self.cp('act', tb[0:64, hb * S_LEN + T * 512:hb * S_LEN + (T + 1) * 512], pa[64:128, 0:512], [pakey], [(kb, T)])
                    else:
                        self.tt('dve', tb[0:64, hb * S_LEN + T * 512:hb * S_LEN + (T + 1) * 512], t1[64:128, 0:512], t2[64:128, 0:512], ALU.add, [t1key, t2key], [(kb, T)])
            wv, wkey = self.load_w(wB[:, 1024:1152], KC, 128, scale=gp)

            def epi_v(ps, pkey, i):
                self.cp('act', vs[:, i * 65:i * 65 + 64], ps[:, 0:64], [pkey], ['vs'])
                self.cp('dve', vw[:, i * 65:i * 65 + 64], ps[:, 64:128], [pkey], ['vw'])
            self.proj_tm(lambda k, wv=wv: wv[:, k * 128:(k + 1) * 128], wkey, 128, self.hT, 'hT', KC, epi_v)
            if int(os.environ.get("K_BSTOP", "99")) < 1:
                continue
            S.barrier(ALL5)
            self.load_w(dr['w1k'][l].rearrange("d l m -> d (l m)"), 1, 2048, dst_fn=lambda k: w1kb[0:64, :], dstkey='w1kb', rows=64)
            self.load_w(dr['w1v'][l].rearrange("d l m -> d (l m)"), 1, 2048, dst_fn=lambda k: w1vb[0:64, :], dstkey='w1vb', rows=64)
            self.load_w(dr['w2k'][l], 1, 64, dst_fn=lambda k: w2kb[0:64, :], dstkey='w2kb', rows=64, mulc=0.5)
            self.load_w(dr['w2v'][l], 1, 64, dst_fn=lambda k: w2vb[0:64, :], dstkey='w2vb', rows=64, mulc=0.5)
            self.load_w(dr['posk'][l], 1, 32, dst_fn=lambda k: posb[0:64, 0:32], dstkey='posb', rows=64)
            self.load_w(dr['posv'][l], 1, 32, dst_fn=lambda k: posb[0:64, 32:64], dstkey='posb', rows=64)
            cstop = int(os.environ.get("K_CSTOP", "99"))
            if cstop >= 1:
                self.cp('pool', vcmpx[:, 64:97], self.ovl[:, 0:33], ['const'], ['vcmpx'])
            for kv, (srcT, skeyn, w1b, w1key, w2b, w2key, po) in enumerate(((kcT, 'kcT', w1kb, 'w1kb', w2kb, 'w2kb', 0), (vcT, 'vcT', w1vb, 'w1vb', w2vb, 'w2vb', 32))):
                if cstop < 2:
                    break
                ph, phkey = self.rot('psA')
                for ll in range(32):
                    self.mm(ph[0:64, 0:127], w1b[0:64, ll * 64:(ll + 1) * 64], srcT[0:64, ll:ll + 2017:16], ll == 0, ll == 31, [w1key] + [(skeyn, T) for T in range(4)], [phkey])
                if cstop < 3:
                    break
                pb2, pb2key = self.rot('psA')
                for ll in range(32):
                    self.mm(pb2[0:64, 0:1], w1b[0:64, ll * 64:(ll + 1) * 64], posb[0:64, po + ll:po + ll + 1], ll == 0, ll == 31, [w1key, 'posb'], [pb2key])
                if cstop < 4:
                    break
                hb_, hbkey = self.rot('small')
                self.cp('dve', hb_[0:64, 0:1], pb2[0:64, 0:1], [pb2key], [hbkey])
                self.ts('dve', hb_[0:64, 1:2], hb_[0:64, 0:1], 0.5, None, ALU.mult, None, [hbkey], [hbkey])
                th, thkey = self.rot('tmpf')
                self.act(th[0:64, 0:127], ph[0:64, 0:127], AF.Tanh, [phkey, hbkey], [thkey], scale=0.5, bias=hb_[0:64, 1:2])
                if cstop < 5:
                    break
                xb, xbkey = self.rot('tmpf')
                var = os.environ.get("K_VAR", "")
                if 'a' not in var:
                    self.ts('dve', xb[0:64, 0:127], ph[0:64, 0:127], hb_[0:64, 0:1], None, ALU.add, None, [phkey, hbkey], [xbkey])
                a1, a1key = self.rot('tmpb')
                if 'b' not in var:
                    self.stt('dve', a1[0:64, 0:127], th[0:64, 0:127], 1.0, xb[0:64, 0:127], ALU.add, ALU.mult, [thkey, xbkey], [a1key])
                if cstop < 6:
                    break
                po2, po2key = self.rot('psA')
                if kv == 0:
                    self.mm(po2[0:64, 0:127], w2b[0:64, 0:64], a1[0:64, 0:127], True, True, [w2key, a1key], [po2key])
                    self.cp('dve', kcmpT[0:64, 0:127], po2[0:64, 0:127], [po2key], ['kcmpT'])
                else:
                    self.mm(po2[0:127, 0:64], a1[0:64, 0:127], w2b[0:64, 0:64], True, True, [w2key, a1key], [po2key])
                    self.cp('dve', vcmpx[0:127, 0:64], po2[0:127, 0:64], [po2key], ['vcmpx'])
            if int(os.environ.get("K_BSTOP", "99")) < 2:
                continue
            S.barrier(ALL5)
            self.DMA(cmpneg, dr['cmpneg'], w=['cmpneg'])
            self.DMA(keepadd, dr['keepadd'], w=['keepadd'])
            ka4 = keepadd.rearrange("p (a i c) -> p a i c", a=2, c=32)
            oc4 = ocmp.rearrange("p (r q c) -> p r q c", r=4, c=64)
            for T in range(NCH):
                yt = ytm[T % 2]
                ykey = f'ytmB{T % 2}'
                bgT = bgs3[:, 4 * T:4 * T + 4, :]
                for r in range(4):
                    h = 4 * g + r
                    ps, pkey = self.rot('psA')
                    self.mm(ps[0:127, 0:512], kcmpT[0:64, 0:127], qTa[0:64, r * S_LEN + T * 512:r * S_LEN + (T + 1) * 512], True, False, ['kcmpT', ('qTa', T)], [pkey])
                    self.mm(ps[0:127, 0:512], self.identb[0:127, 0:127], cmpneg[0:127, T * 512:(T + 1) * 512], False, True, ['const', 'cmpneg'], [pkey])
                    et, etkey = self.rot('tmpb')
                    self.act(et[0:127, 0:512], ps[0:127, 0:512], AF.Exp, [pkey], [etkey], scale=0.125, nodrain=True)
                    R, Rkey = self.rot('psB')
                    for qt in range(4):
                        self.mm(R[:, qt * 97:(qt + 1) * 97], et[0:127, qt * 128:(qt + 1) * 128], vcmpx[0:127, 0:97], True, True, [etkey, 'vcmpx'], [Rkey])
                    R3 = R[:, 0:388].rearrange("p (q c) -> p q c", c=97)
                    rz, rzkey = self.rot('small')
                    rz3 = lambda a: rz[:, a:a + 4].rearrange("p (q c) -> p q c", c=1)
                    self.ts('dve', rz3(0), R3[:, :, 64:65], 1e-30, None, ALU.max, None, [Rkey], [rzkey])
                    self.recip(rz[:, 4:8], rz[:, 0:4], [rzkey], [rzkey])
                    if r == 0:
                        self.tt('dve', impacc.rearrange("p (q c) -> p q c", c=32), R3[:, :, 65:97], rz3(4).broadcast_to([128, 4, 32]), ALU.mult, [Rkey, rzkey], ['impacc'])
                    else:
                        self.tt('dve', imp2.rearrange("p (q c) -> p q c", c=32), R3[:, :, 65:97], rz3(4).broadcast_to([128, 4, 32]), ALU.mult, [Rkey, rzkey], ['imp2'])
                        self.tt('pool', impacc, impacc, imp2, ALU.add, ['imp2', 'impacc'], ['impacc'])
                    self.tt('dve', rz3(8), rz3(4), bgT[:, :, h * 3:h * 3 + 1], ALU.mult, [rzkey, 'bgs'], [rzkey])
                    self.tt('dve', oc4[:, r], R3[:, :, 0:64], rz3(8).broadcast_to([128, 4, 64]), ALU.mult, [Rkey, rzkey], [('ocmp', r)])
                if int(os.environ.get("K_BSTOP", "99")) < 3:
                    continue
                i3 = imp2.rearrange("p (q c) -> p q c", c=32)
                self.tt('dve', i3, impacc.rearrange("p (q c) -> p q c", c=32), ka4[:, 0, 4 * T:4 * T + 4, :], ALU.mult, ['impacc', 'keepadd', 'imp2'], ['imp2'])
                self.tt('dve', i3, i3, ka4[:, 1, 4 * T:4 * T + 4, :], ALU.add, ['imp2', 'keepadd'], ['imp2'])
                m8, m8key = self.rot('small')
                for qt in range(4):
                    self.S.op('dve', lambda e, qt=qt, m8=m8: e.max(out=m8[:, qt * 8:(qt + 1) * 8], in_=imp2[:, qt * 32:(qt + 1) * 32]), ['imp2'], [m8key])
                    self.ts('dve', selneg[:, qt * 32:(qt + 1) * 32], imp2[:, qt * 32:(qt + 1) * 32], m8[:, qt * 8 + 7:qt * 8 + 8], NEG, ALU.is_lt, ALU.mult, ['imp2', m8key], ['selneg'])
                pt, ptkey = self.rot('psT')
                for qt in range(4):
                    self.tr(pt[0:32, qt * 128:(qt + 1) * 128], selneg[:, qt * 32:(qt + 1) * 32], ['selneg'], [ptkey])
                for r in range(4):
                    self.cp('act' if r % 2 == 0 else 'dve', qTa[64:96, r * S_LEN + T * 512:r * S_LEN + (T + 1) * 512], pt[0:32, 0:512], [ptkey], [('qTa', T)])
                if int(os.environ.get("K_BSTOP", "99")) < 4:
                    continue
                for r in range(4):
                    h = 4 * g + r
                    acs, acskey = self.rot('psB')
                    first_s = True
                    for j in range(0, 4 * T + 4):
                        lo = max(128 * j, 512 * T)
                        w = 512 * (T + 1) - lo
                        diag = j >= 4 * T
                        ps, pkey = self.rot('psA')
                        self.mm(ps[:, 0:w], ksTa[0:96, j * 128:(j + 1) * 128], qTa[0:96, r * S_LEN + lo:r * S_LEN + lo + w], True, not diag, [('ksTa', j // 4), ('qTa', T)], [pkey])
                        if diag:
                            self.mm(ps[:, 0:128], self.identb, self.causalneg, False, True, ['const'], [pkey])
                        pT, pTkey = self.rot('tmpb')
                        self.act(pT[:, 0:w], ps[:, 0:w], AF.Exp, [pkey], [pTkey], scale=0.125, nodrain=True)
                        for qt in range(4):
                            i = 4 * T + qt
                            if i < j:
                                continue
                            off = i * 128 - lo
                            self.mm(acs[:, qt * 65:(qt + 1) * 65], pT[:, off:off + 128], vs[:, j * 65:(j + 1) * 65], first_s, j == i, [pTkey, 'vs'], [acskey], skip=True)
                            first_s = False
                    acw, acwkey = self.rot('psB')
                    first_w = True
                    for j in range(max(0, 4 * T - 2), 4 * T + 4):
                        i_lo = max(j, 4 * T)
                        i_hi = min(j + 2, 4 * T + 3)
                        lo = i_lo * 128
                        w = (i_hi + 1) * 128 - lo
                        masks = []
                        if i_lo == j:
                            masks.append((0, self.causalneg))
                        if i_hi == j + 2:
                            masks.append(((j + 2) * 128 - lo, self.anticausalneg))
                        ps, pkey = self.rot('psA')
                        self.mm(ps[:, 0:w], kwT[0:64, j * 128:(j + 1) * 128], qTa[0:64, r * S_LEN + lo:r * S_LEN + lo + w], True, len(masks) == 0, [('kwT', j // 4), ('qTa', T)], [pkey])
                        for mi, (mo, mk) in enumerate(masks):
                            self.mm(ps[:, mo:mo + 128], self.identb, mk, False, mi == len(masks) - 1, ['const'], [pkey])
                        pT, pTkey = self.rot('tmpb')
                        self.act(pT[:, 0:w], ps[:, 0:w], AF.Exp, [pkey], [pTkey], scale=0.125, nodrain=True)
                        for i in range(i_lo, i_hi + 1):
                            qt = i - 4 * T
                            off = i * 128 - lo
                            self.mm(acw[:, qt * 65:(qt + 1) * 65], pT[:, off:off + 128], vw[:, j * 65:(j + 1) * 65], first_w, j == i, [pTkey, 'vw'], [acwkey], skip=True)
                            first_w = False
                    s3 = acs[:, 0:260].rearrange("p (q c) -> p q c", c=65)
                    w3 = acw[:, 0:260].rearrange("p (q c) -> p q c", c=65)
                    rz, rzkey = self.rot('small')
                    rz3 = lambda a: rz[:, a:a + 4].rearrange("p (q c) -> p q c", c=1)
                    self.ts('dve', rz3(0), s3[:, :, 64:65], 1e-30, None, ALU.max, None, [acskey], [rzkey])
                    self.ts('dve', rz3(4), w3[:, :, 64:65], 1e-30, None, ALU.max, None, [acwkey], [rzkey])
                    self.recip(rz[:, 8:16], rz[:, 0:8], [rzkey], [rzkey])
                    self.tt('dve', rz3(16), rz3(8), bgT[:, :, h * 3 + 1:h * 3 + 2], ALU.mult, [rzkey, 'bgs'], [rzkey])
                    self.tt('dve', rz3(20), rz3(12), bgT[:, :, h * 3 + 2:h * 3 + 3], ALU.mult, [rzkey, 'bgs'], [rzkey])
                    ta, takey = self.rot('tmpf')
                    tb, tbkey = self.rot('tmpf')
                    ta3 = ta[:, 0:256].rearrange("p (q c) -> p q c", c=64)
                    tb3 = tb[:, 0:256].rearrange("p (q c) -> p q c", c=64)
                    self.tt('dve', ta3, s3[:, :, 0:64], rz3(16).broadcast_to([128, 4, 64]), ALU.mult, [acskey, rzkey], [takey])
                    self.tt('pool', ta3, ta3, oc4[:, r], ALU.add, [takey, ('ocmp', r)], [takey])
                    self.tt('dve', tb3, w3[:, :, 0:64], rz3(20).broadcast_to([128, 4, 64]), ALU.mult, [acwkey, rzkey], [tbkey])
                    ydst = yt.rearrange("p (q c) -> p q c", c=256)[:, :, r * 64:(r + 1) * 64]
                    self.tt('dve', ydst, ta3, tb3, ALU.add, [takey, tbkey], [ykey])
                wzv, wzkey = self.load_w(wB[:, 1152:1408], KC, 256, scale=gp)
                self.finish_chunk_sub(T, yt, ykey, 256, [2 * g, 2 * g + 1], 0,
                                      lambda k, wi, wzv=wzv: wzv[:, k * 256 + wi * 128:k * 256 + (wi + 1) * 128], wzkey)
        S.barrier(ALL5)


_CACHE = {}


def _prep(inputs):
    blobs = _host_layer_blobs(inputs)
    consts = _host_consts()
    return blobs, consts


def kernel(**inputs):
    inputs = {k: np.asarray(v) for k, v in inputs.items()}
    blobs, consts = _prep(inputs)
    dtm = lambda a: BF16 if a.dtype == ml_dtypes.bfloat16 else F32
    blob_shapes = {k: (v.shape, dtm(v)) for k, v in blobs.items()}
    const_shapes = {k: (v.shape, dtm(v)) for k, v in consts.items()}
    b = Builder(blob_shapes, const_shapes)
    nc = b.build()
    in_maps = []
    for c in range(8):
        m = {'x': np.ascontiguousarray(inputs['x'][c]), 'mem': np.ascontiguousarray(inputs['mem'][c])}
        m.update(blobs)
        m.update(consts)
        in_maps.append(m)
    res = run_bass_kernel_spmd(nc, in_maps, core_ids=list(range(8)))
    return np.stack([r['out'] for r in res.results], 0).astype(np.float32)
```
